# Optimizing a Trainium2 kernel written in Bass

```python
import jax, jax.numpy as jnp
from jax import lax
import numpy as np

D_MODEL = 1024
BATCH = 2
SEQ = 16384
DEPTH = 4

GRID_W = 64
CTX_LEN = 256
HEAD_DIM = 64
MIXER_WIDTH = 256
N_MIXERS = 4
MIX_WIDTH = N_MIXERS * MIXER_WIDTH
NA_HEADS = 4
NA_WIN_R = 8
NA_WIN_C = 16
GB_HEADS = 4
GB_KV = 2
GB_GROUP = GB_HEADS // GB_KV
SW_HEADS = 4
SW_KV = 2
SW_GROUP = SW_HEADS // SW_KV
WINDOW = 128
Q_BLOCK = 128
SG_GROUPS = 4
SG_GROUP_DIM = MIXER_WIDTH // SG_GROUPS
CHUNK = 128
N_EXPERTS = 16
EXPERT_FF = 2048
EC_CAPACITY_FACTOR = 2
ROPE_THETA = 10000.0
ROPE_FREQS = HEAD_DIM // 4
EPS = 1e-6
NEG_INF = -1e30
PROJ_SIZES = [MIXER_WIDTH] * 3 + [MIXER_WIDTH, GB_KV * HEAD_DIM, GB_KV * HEAD_DIM] \
    + [MIXER_WIDTH, SW_KV * HEAD_DIM, SW_KV * HEAD_DIM] + [MIXER_WIDTH, MIXER_WIDTH]
IN_WIDTH = sum(PROJ_SIZES)

kernel_name = "hybrid_headgroup_diffusion_trunk"


def rms_norm(x, g):
    xf = x.astype(jnp.float32)
    y = xf * lax.rsqrt(jnp.mean(xf * xf, axis=-1, keepdims=True) + EPS)
    return (y * g.astype(jnp.float32)).astype(x.dtype)


def modulate(h, shift, scale):
    return h * (1 + scale) + shift


def adaln(cvec, w, b):
    m = (jax.nn.silu(cvec) @ w + b)[:, None, :]
    return jnp.split(m, 6, axis=-1)


def rope_tables(n):
    t = jnp.arange(n)
    row = (t // GRID_W).astype(jnp.float32)
    col = (t % GRID_W).astype(jnp.float32)
    freqs = ROPE_THETA ** (-jnp.arange(ROPE_FREQS, dtype=jnp.float32) / ROPE_FREQS)
    ang = jnp.stack([row[:, None] * freqs, col[:, None] * freqs], axis=1)
    return jnp.cos(ang), jnp.sin(ang)


def apply_rope(x, cos, sin):
    xf = x.astype(jnp.float32).reshape(x.shape[:-1] + (2, 2, ROPE_FREQS))
    a, b = xf[..., 0, :], xf[..., 1, :]
    shp = (cos.shape[0],) + (1,) * (x.ndim - 3) + (2, ROPE_FREQS)
    cs, sn = cos.reshape(shp), sin.reshape(shp)
    out = jnp.stack([a * cs - b * sn, a * sn + b * cs], axis=-2)
    return out.reshape(x.shape).astype(x.dtype)


def softmax_parts(parts, sink=None):
    s = jnp.concatenate([p.astype(jnp.float32) for p in parts], axis=-1)
    if sink is not None:
        s = jnp.concatenate([s, jnp.broadcast_to(sink.astype(jnp.float32), s.shape[:-1] + (1,))], axis=-1)
    p = jax.nn.softmax(s, axis=-1)
    outs, off = [], 0
    for part in parts:
        n = part.shape[-1]
        outs.append(p[..., off:off + n])
        off += n
    return outs


def ctx_attention(q, k, v, sink=None):
    s = jnp.einsum('bqhgd,bkhd->bhgqk', q, k) * HEAD_DIM ** -0.5
    (p,) = softmax_parts([s], sink)
    o = jnp.einsum('bhgqk,bkhd->bqhgd', p.astype(v.dtype), v)
    return o.reshape(o.shape[0], o.shape[1], -1)


def neighbourhood_attention(q, k, v, kc, vc, rpb):
    B, N, H, dh = q.shape
    rows = N // GRID_W
    win_r = min(NA_WIN_R, rows)
    qg = q.reshape(B, rows, GRID_W, H, dh)
    kg = k.reshape(B, rows, GRID_W, H, dh)
    vg = v.reshape(B, rows, GRID_W, H, dh)
    cols = np.arange(GRID_W)
    c_start = np.clip(cols - NA_WIN_C // 2, 0, GRID_W - NA_WIN_C)
    col_idx = c_start[:, None] + np.arange(NA_WIN_C)[None, :]
    col_off = col_idx - cols[:, None] + (NA_WIN_C - 1)
    scale = dh ** -0.5

    def row_block(r):
        r_start = jnp.clip(r - win_r // 2, 0, rows - win_r)
        k_sel = lax.dynamic_slice_in_dim(kg, r_start, win_r, axis=1)[:, :, col_idx]
        v_sel = lax.dynamic_slice_in_dim(vg, r_start, win_r, axis=1)[:, :, col_idx]
        q_r = lax.dynamic_index_in_dim(qg, r, axis=1, keepdims=False)
        s_loc = jnp.einsum('bqhd,bwqjhd->bhqwj', q_r, k_sel) * scale
        row_off = r_start + jnp.arange(win_r) - r + (NA_WIN_R - 1)
        bias = rpb[:, row_off[:, None, None], col_off[None]]
        s_loc = (s_loc + bias.transpose(0, 2, 1, 3)[None]).reshape(B, H, GRID_W, win_r * NA_WIN_C)
        s_ctx = jnp.einsum('bqhd,bkhd->bhqk', q_r, kc) * scale
        p_loc, p_ctx = softmax_parts([s_loc, s_ctx])
        p_loc = p_loc.reshape(B, H, GRID_W, win_r, NA_WIN_C).astype(v.dtype)
        return (jnp.einsum('bhqwj,bwqjhd->bqhd', p_loc, v_sel)
                + jnp.einsum('bhqk,bkhd->bqhd', p_ctx.astype(v.dtype), vc))

    y = lax.map(row_block, jnp.arange(rows))
    return y.transpose(1, 0, 2, 3, 4).reshape(B, N, H * dh)


def global_attention(q, k, v, kc, vc):
    B, N = q.shape[:2]
    nb = N // Q_BLOCK
    scale = HEAD_DIM ** -0.5
    q_blocks = q.reshape((B, nb, Q_BLOCK) + q.shape[2:]).swapaxes(0, 1)

    def block(qb):
        s_lat = jnp.einsum('bqhgd,bkhd->bhgqk', qb, k) * scale
        s_ctx = jnp.einsum('bqhgd,bkhd->bhgqk', qb, kc) * scale
        p_lat, p_ctx = softmax_parts([s_lat, s_ctx])
        return (jnp.einsum('bhgqk,bkhd->bqhgd', p_lat.astype(v.dtype), v)
                + jnp.einsum('bhgqk,bkhd->bqhgd', p_ctx.astype(v.dtype), vc))

    y = lax.map(block, q_blocks)
    return y.swapaxes(0, 1).reshape(B, N, -1)


def window_attention(q, k, v, kc, vc, sink):
    B, N, HKV, G, dh = q.shape
    nb = N // Q_BLOCK
    scale = dh ** -0.5
    pad = ((0, 0), (Q_BLOCK, Q_BLOCK), (0, 0), (0, 0))
    kp = jnp.pad(k, pad).reshape(B, nb + 2, Q_BLOCK, HKV, dh)
    vp = jnp.pad(v, pad).reshape(B, nb + 2, Q_BLOCK, HKV, dh)
    k_band = jnp.concatenate([kp[:, i:i + nb] for i in range(3)], axis=2)
    v_band = jnp.concatenate([vp[:, i:i + nb] for i in range(3)], axis=2)
    a = np.arange(Q_BLOCK)[:, None]
    j = np.arange(3 * Q_BLOCK)[None, :]
    in_window = np.abs(j - Q_BLOCK - a) <= WINDOW
    kpos = (np.arange(nb)[:, None] - 1) * Q_BLOCK + np.arange(3 * Q_BLOCK)[None, :]
    in_range = (kpos >= 0) & (kpos < N)
    mask = in_window[None] & in_range[:, None, :]
    sink_b = sink.reshape(HKV, G, 1, 1)
    xs = (q.reshape(B, nb, Q_BLOCK, HKV, G, dh).swapaxes(0, 1),
          k_band.swapaxes(0, 1), v_band.swapaxes(0, 1), jnp.asarray(mask))

    def block(args):
        qb, kb, vb, m = args
        s_loc = jnp.einsum('bqhgd,bkhd->bhgqk', qb, kb).astype(jnp.float32) * scale
        s_loc = jnp.where(m, s_loc, NEG_INF)
        s_ctx = jnp.einsum('bqhgd,bkhd->bhgqk', qb, kc) * scale
        p_loc, p_ctx = softmax_parts([s_loc, s_ctx], sink_b)
        return (jnp.einsum('bhgqk,bkhd->bqhgd', p_loc.astype(v.dtype), vb)
                + jnp.einsum('bhgqk,bkhd->bqhgd', p_ctx.astype(v.dtype), vc))

    y = lax.map(block, xs)
    return y.swapaxes(0, 1).reshape(B, N, -1)


def spatial_gating(u, v, g_norm, w_s, b_s):
    B, N, _ = u.shape
    nc = N // CHUNK
    u = jax.nn.gelu(u)
    v = rms_norm(jax.nn.gelu(v), g_norm).reshape(B, nc, CHUNK, SG_GROUPS, SG_GROUP_DIM)
    mixed = jnp.einsum('gts,bcsgd->bctgd', w_s, v) + b_s.T[None, None, :, :, None]
    return u * mixed.reshape(B, N, MIXER_WIDTH)


def split_projection(p):
    return jnp.split(p, [int(s) for s in np.cumsum(PROJ_SIZES)[:-1]], axis=-1)


def merge_groups(parts, out_norm, w_out):
    y = jnp.concatenate(parts, axis=-1)
    yg = rms_norm(y.reshape(y.shape[:-1] + (N_MIXERS, MIXER_WIDTH)), out_norm.reshape(N_MIXERS, MIXER_WIDTH))
    return yg.reshape(y.shape) @ w_out


def token_mixer(h, hc, cos, sin, w_in, rpb, q_norm, k_norm, sink, sgu_norm, w_sgu, b_sgu,
                out_norm, w_out, ctx_out):
    B, N, _ = h.shape
    Nc = hc.shape[1]
    qa, ka, va, qb, kb, vb, qs, ks, vs, su, sv = split_projection(h @ w_in)
    qa_c, ka_c, va_c, qb_c, kb_c, vb_c, qs_c, ks_c, vs_c, su_c, sv_c = split_projection(hc @ w_in)
    qa, ka, va = [t.reshape(B, N, NA_HEADS, HEAD_DIM) for t in (qa, ka, va)]
    qa_c, ka_c, va_c = [t.reshape(B, Nc, NA_HEADS, HEAD_DIM) for t in (qa_c, ka_c, va_c)]
    y_a = neighbourhood_attention(qa, ka, va, ka_c, va_c, rpb)
    qb = apply_rope(rms_norm(qb.reshape(B, N, GB_KV, GB_GROUP, HEAD_DIM), q_norm), cos, sin)
    kb = apply_rope(rms_norm(kb.reshape(B, N, GB_KV, HEAD_DIM), k_norm), cos, sin)
    vb = vb.reshape(B, N, GB_KV, HEAD_DIM)
    qb_c = rms_norm(qb_c.reshape(B, Nc, GB_KV, GB_GROUP, HEAD_DIM), q_norm)
    kb_c = rms_norm(kb_c.reshape(B, Nc, GB_KV, HEAD_DIM), k_norm)
    vb_c = vb_c.reshape(B, Nc, GB_KV, HEAD_DIM)
    y_b = global_attention(qb, kb, vb, kb_c, vb_c)
    qs = apply_rope(qs.reshape(B, N, SW_KV, SW_GROUP, HEAD_DIM), cos, sin)
    ks = apply_rope(ks.reshape(B, N, SW_KV, HEAD_DIM), cos, sin)
    vs = vs.reshape(B, N, SW_KV, HEAD_DIM)
    qs_c = qs_c.reshape(B, Nc, SW_KV, SW_GROUP, HEAD_DIM)
    ks_c = ks_c.reshape(B, Nc, SW_KV, HEAD_DIM)
    vs_c = vs_c.reshape(B, Nc, SW_KV, HEAD_DIM)
    y_c = window_attention(qs, ks, vs, ks_c, vs_c, sink)
    y_d = spatial_gating(su, sv, sgu_norm, w_sgu, b_sgu)
    y = merge_groups([y_a, y_b, y_c, y_d], out_norm, w_out)
    if not ctx_out:
        return y, None
    ya_c = ctx_attention(qa_c[:, :, :, None], ka_c, va_c)
    yb_c = ctx_attention(qb_c, kb_c, vb_c)
    yc_c = ctx_attention(qs_c, ks_c, vs_c, sink.reshape(SW_KV, SW_GROUP, 1, 1))
    yd_c = spatial_gating(su_c, sv_c, sgu_norm, w_sgu, b_sgu)
    y_ctx = merge_groups([ya_c, yb_c, yc_c, yd_c], out_norm, w_out)
    return y, y_ctx


def expert_choice_moe(h, w_router, w_gate, w_up, w_down):
    B, N, D = h.shape
    cap = EC_CAPACITY_FACTOR * N // N_EXPERTS
    logits = jnp.einsum('bnd,de->ben', h, w_router).astype(jnp.float32)
    aff = jax.nn.softmax(logits, axis=1)
    gate, idx = lax.top_k(aff, cap)
    xe = jax.vmap(lambda hb, ib: hb[ib])(h, idx)
    hid = jax.nn.silu(jnp.einsum('becd,edf->becf', xe, w_gate)) * jnp.einsum('becd,edf->becf', xe, w_up)
    ye = jnp.einsum('becf,efd->becd', hid, w_down) * gate[..., None].astype(h.dtype)
    return jax.vmap(lambda yb, ib: jnp.zeros((N, D), yb.dtype).at[ib.reshape(-1)].add(yb.reshape(-1, D)))(ye, idx)


def setup_inputs(seed: int = 0) -> dict:
    key = jax.random.key(seed)
    ks = jax.random.split(key, 24)

    def nrm(k, shape, scale):
        return jax.random.normal(k, shape, jnp.float32) * scale

    return {
        "x": nrm(ks[0], (BATCH, SEQ, D_MODEL), 1.0),
        "c": nrm(ks[1], (BATCH, D_MODEL), 1.0),
        "ctx": nrm(ks[2], (BATCH, CTX_LEN, D_MODEL), 1.0),
        "c_ctx": nrm(ks[3], (D_MODEL,), 1.0),
        "w_mod": nrm(ks[4], (DEPTH, D_MODEL, 6 * D_MODEL), 0.5 * D_MODEL ** -0.5),
        "b_mod": nrm(ks[5], (DEPTH, 6 * D_MODEL), 0.02),
        "norm_mix": 1.0 + nrm(ks[6], (DEPTH, D_MODEL), 0.02),
        "norm_ffn": 1.0 + nrm(ks[7], (DEPTH, D_MODEL), 0.02),
        "w_in": nrm(ks[8], (DEPTH, D_MODEL, IN_WIDTH), D_MODEL ** -0.5),
        "rpb": nrm(ks[9], (DEPTH, NA_HEADS, 2 * NA_WIN_R - 1, 2 * NA_WIN_C - 1), 0.1),
        "q_norm": 1.0 + nrm(ks[10], (DEPTH, HEAD_DIM), 0.02),
        "k_norm": 1.0 + nrm(ks[11], (DEPTH, HEAD_DIM), 0.02),
        "sink": nrm(ks[12], (DEPTH, SW_HEADS), 0.5),
        "sgu_norm": 1.0 + nrm(ks[13], (DEPTH, MIXER_WIDTH), 0.02),
        "w_sgu": nrm(ks[14], (DEPTH, SG_GROUPS, CHUNK, CHUNK), CHUNK ** -0.5),
        "b_sgu": nrm(ks[15], (DEPTH, SG_GROUPS, CHUNK), 0.02),
        "out_norm": 1.0 + nrm(ks[16], (DEPTH, MIX_WIDTH), 0.02),
        "w_out": nrm(ks[17], (DEPTH, MIX_WIDTH, D_MODEL), MIX_WIDTH ** -0.5),
        "w_router": nrm(ks[18], (DEPTH, D_MODEL, N_EXPERTS), D_MODEL ** -0.5),
        "w_gate": nrm(ks[19], (DEPTH, N_EXPERTS, D_MODEL, EXPERT_FF), D_MODEL ** -0.5),
        "w_up": nrm(ks[20], (DEPTH, N_EXPERTS, D_MODEL, EXPERT_FF), D_MODEL ** -0.5),
        "w_down": nrm(ks[21], (DEPTH, N_EXPERTS, EXPERT_FF, D_MODEL), EXPERT_FF ** -0.5),
        "final_norm": 1.0 + nrm(ks[22], (D_MODEL,), 0.02),
    }


def reference(x, c, ctx, c_ctx, w_mod, b_mod, norm_mix, norm_ffn, w_in, rpb, q_norm, k_norm, sink,
              sgu_norm, w_sgu, b_sgu, out_norm, w_out, w_router, w_gate, w_up, w_down, final_norm):
    N = x.shape[1]
    cos, sin = rope_tables(N)
    xc = ctx
    for l in range(DEPTH):
        ctx_needed = l < DEPTH - 1
        sh1, sc1, g1, sh2, sc2, g2 = adaln(c, w_mod[l], b_mod[l])
        csh1, csc1, cg1, csh2, csc2, cg2 = adaln(c_ctx[None], w_mod[l], b_mod[l])
        h = modulate(rms_norm(x, norm_mix[l]), sh1, sc1)
        hc = modulate(rms_norm(xc, norm_mix[l]), csh1, csc1)
        y, y_ctx = token_mixer(h, hc, cos, sin, w_in[l], rpb[l], q_norm[l], k_norm[l], sink[l],
                               sgu_norm[l], w_sgu[l], b_sgu[l], out_norm[l], w_out[l], ctx_needed)
        x = x + g1 * y
        h = modulate(rms_norm(x, norm_ffn[l]), sh2, sc2)
        x = x + g2 * expert_choice_moe(h, w_router[l], w_gate[l], w_up[l], w_down[l])
        if ctx_needed:
            xc = xc + cg1 * y_ctx
            hc = modulate(rms_norm(xc, norm_ffn[l]), csh2, csc2)
            xc = xc + cg2 * expert_choice_moe(hc, w_router[l], w_gate[l], w_up[l], w_down[l])
    return rms_norm(x, final_norm)
```

```python
from contextlib import ExitStack

import numpy as np
import ml_dtypes
import concourse.bass as bass
import concourse.mybir as mybir
from concourse.bass_utils import run_bass_kernel_spmd

F32 = mybir.dt.float32
BF16 = mybir.dt.bfloat16
U32 = mybir.dt.uint32
AF = mybir.ActivationFunctionType
ALU = mybir.AluOpType
AX = mybir.AxisListType

D = 1024
DC = 8
CTX = 256
GRID_W = 64
EPS = 1e-6
NEG = -30000.0
BIG = 1.0e6
N_CORES = 8


class Buf:
    def __init__(self, name, shared=False):
        self.name = name
        self.shared = shared
        self.w = {}
        self.r = {}


class T:
    def __init__(self, t, name, shared=False):
        self.t = t
        self.b = Buf(name, shared)

    def __getitem__(self, idx):
        return self.t[idx]


class Cnt:
    def __init__(self, sem, step):
        self.sem = sem
        self.step = step
        self.n = 0


def _bufs(xs):
    return [x.b if isinstance(x, T) else x for x in xs]


class K:
    ENG = ("pe", "act", "dve", "pool", "sp")

    def __init__(self, nc, st):
        self.nc = nc
        self.st = st
        self.prog = {e: [] for e in self.ENG}
        self.q = {}
        for e in ("pe", "act", "dve", "pool"):
            self.q[e] = Cnt(st.enter_context(nc.semaphore("c_" + e)), 1)
        self.seen = {}
        self.ninst = 0

    def dmaq(self, name, step=16):
        self.q[name] = Cnt(self.st.enter_context(self.nc.semaphore("d_" + name)), step)
        return name

    def sb(self, name, shape, dt, st=None):
        self.uid = getattr(self, "uid", 0) + 1
        name = f"s{self.uid}_{name}"
        return T((st or self.st).enter_context(self.nc.sbuf_tensor(name, shape, dt)), name)

    def ps(self, name, shape, dt=F32, st=None):
        self.uid = getattr(self, "uid", 0) + 1
        name = f"p{self.uid}_{name}"
        return T((st or self.st).enter_context(self.nc.psum_tensor(name, shape, dt)), name)

    def _deps(self, r, w):
        deps = {}

        def add(m):
            for qn, s in m.items():
                if s > deps.get(qn, 0):
                    deps[qn] = s
        for b in r:
            add(b.w)
        for b in w:
            add(b.r)
            if not b.shared:
                add(b.w)
        return deps

    def _wait(self, eng, deps):
        for qn, v in deps.items():
            if qn == "pe" and eng == "pe":
                continue
            q = self.q[qn]
            if q.step == 16:
                v = q.n
            if self.seen.get((eng, qn), 0) >= v:
                continue
            self.seen[(eng, qn)] = v
            self.prog[eng].append(lambda E, sem=q.sem, val=v * q.step: E.wait_ge(sem, val))

    def _mark(self, me, r, w):
        for b in r:
            if me[1] > b.r.get(me[0], 0):
                b.r[me[0]] = me[1]
        for b in w:
            if b.shared:
                if me[1] > b.w.get(me[0], 0):
                    b.w[me[0]] = me[1]
            else:
                b.w = {me[0]: me[1]}
                b.r = {}

    def op(self, eng, fn, r=(), w=()):
        r, w = _bufs(r), _bufs(w)
        self._wait(eng, self._deps(r, w))
        q = self.q[eng]
        q.n += 1
        self.ninst += 1
        self.prog[eng].append(lambda E, sem=q.sem: fn(E).then_inc(sem, 1))
        self._mark((eng, q.n), r, w)

    def dma(self, eng, qn, out, in_, r=(), w=()):
        self.dmaop(eng, qn, lambda E: E.dma_start(out=out, in_=in_), r, w)

    def dmaop(self, eng, qn, fn, r=(), w=()):
        r, w = _bufs(r), _bufs(w)
        self._wait(eng, self._deps(r, w))
        q = self.q[qn]
        q.n += 1
        self.ninst += 1
        self.prog[eng].append(lambda E, sem=q.sem, step=q.step: fn(E).then_inc(sem, step))
        self._mark((qn, q.n), r, w)

    def ring(self, name, n, step=16):
        self.rings = getattr(self, "rings", {})
        self.rings[name] = [self.dmaq(f"{name}{i}", step) for i in range(n)]
        self.ringpos = getattr(self, "ringpos", {})
        self.ringpos[name] = 0

    def rdma(self, eng, ring, fn, r=(), w=(), slot=None, selfwait=True):
        names = self.rings[ring]
        if slot is None:
            slot = self.ringpos[ring]
            self.ringpos[ring] += 1
        qn = names[slot % len(names)]
        q = self.q[qn]
        r, w = _bufs(r), _bufs(w)
        deps = self._deps(r, w)
        if q.n and selfwait:
            deps[qn] = q.n
        self._wait(eng, deps)
        q.n += 1
        self.ninst += 1
        self.prog[eng].append(lambda E, sem=q.sem, step=q.step: fn(E).then_inc(sem, step))
        self._mark((qn, q.n), r, w)

    def pdma(self, ring, fn, r=(), w=(), slot=None, selfwait=True):
        self.rdma("pool", ring, fn, r, w, slot, selfwait)

    def barrier(self):
        allq = {qn: q.n for qn, q in self.q.items() if q.n}
        for eng in self.ENG:
            self._wait(eng, dict(allq))

    def preg(self, E, val):
        self._regs = getattr(self, "_regs", {})
        if val not in self._regs:
            self._regs[val] = E.to_reg(val)
        return self._regs[val]

    def drain(self, eng, qns):
        for qn in qns:
            q = self.q[qn]
            if q.n:
                self.prog[eng].append(lambda E, sem=q.sem, val=q.n * q.step: E.wait_ge(sem, val))

    def emit(self):
        with self.nc.Block() as block:
            @block.tensor
            def _(E):
                for f in self.prog["pe"]:
                    f(E)

            @block.scalar
            def _(E):
                for f in self.prog["act"]:
                    f(E)

            @block.vector
            def _(E):
                for f in self.prog["dve"]:
                    f(E)

            @block.gpsimd
            def _(E):
                for f in self.prog["pool"]:
                    f(E)

            @block.sync
            def _(E):
                for f in self.prog["sp"]:
                    f(E)


def build(N, L, FF, dbg=False):
    TA = N + CTX
    NTT = TA // 128
    NLT = N // 128
    NQ = N // 4
    ROWS = N // GRID_W
    CAP = 2 * N // 16
    CCAP = 2 * CTX // 16
    NS = CAP + 128
    NST = NS // 128
    FC = FF // 128
    SEGW = N // 32
    nc = bass.Bass("TRN2", target_bir_lowering=False)

    def din(name, shape, dt=F32):
        return T(nc.dram_tensor(name, list(shape), dt, kind="ExternalInput").ap(), name, shared=True)

    def dsc(name, shape, dt=F32):
        return T(nc.dram_tensor(name, list(shape), dt).ap(), name, shared=True)

    I = dict(
        x=din("x", [TA, D]), cv=din("cv", [128, DC, 2]), wmod=din("wmod", [L, D, 1536]),
        bmod=din("bmod", [L, 6 * D]), nmix=din("nmix", [L, D]), nffn=din("nffn", [L, D]),
        fnorm=din("fnorm", [D]), w1=din("w1", [L, D, 768]), w2=din("w2", [L, D, 448]),
        cs=din("cs", [2, 64, TA]), gqk=din("gqk", [L, 128, 4]), sgn=din("sgn", [L, 64]),
        ws=din("ws", [L, 128, 128]), bs=din("bs", [L, 128]), nab=din("nab", [L, 3, 8, 128, 512]),
        wm=din("wm", [6, 128, 512]), snk=din("snk", [L, 128, 1]), onrm=din("onrm", [L, 128, 4]),
        wo=din("wo", [L, 4, 64, D]), wr=din("wr", [L, D, 16]),
        wg=din("wg", [L, 2, D * FF]), wu=din("wu", [L, 2, D * FF]), wd=din("wd", [L, 2, FF * D]),
        identf=din("identf", [128, 128]), identb=din("identb", [128, 128], BF16),
        segm=din("segm", [128, 128]), trix=din("trix", [128, 128]), e4=din("e4", [128, 4]),
        selh=din("selh", [128, 128]), oidx=din("oidx", [128, NQ // 128], U32),
    )
    out = T(nc.dram_tensor("out", [NQ, D], F32, kind="ExternalOutput").ap(), "out", shared=True)
    X = dsc("X", [TA, D])
    MODP = dsc("MODP", [2, 1536]); MODG = dsc("MODG", [L, 4, 2, 1536])
    FM = dsc("FM", [4, 128, TA], BF16); VT = dsc("VT", [TA, 192], BF16); VN = dsc("VN", [TA, 64], BF16)
    YT = dsc("YT", [4, 64, TA], BF16)
    SSQ = dsc("SSQ", [TA, 4]); SSQR = dsc("SSQR", [TA, 4])
    U = dsc("U", [TA, D]); UR = dsc("UR", [TA, D])
    H2 = dsc("H2", [TA, D], BF16); AFF = dsc("AFF", [TA, 16]); AFFT = dsc("AFFT", [16, TA])
    XE = [dsc(f"XE{e}", [NS, D], BF16) for e in range(4)]; YE = [dsc(f"YE{e}", [NS, D]) for e in range(4)]

    CH = 512 * 1024
    NCE = D * FF // CH
    BNC = dsc("BNC", [2, 128, CH // 128])
    GB = {n_: dsc("GB" + n_, [L, 2 * NCE, 2, CH]) for n_ in ("wg", "wu", "wd")}
    with ExitStack() as st:
        k = K(nc, st)
        k.ring("ld", 1); k.ring("st", 1)
        k.ring("wt", 2); k.ring("cc", 1, 1); k.ring("gs", 8)

        def ld(o, i, r, w):
            k.rdma("sp", "ld", lambda E: E.dma_start(out=o, in_=i), r, w)

        def sto(o, i, r, w):
            k.rdma("sp", "st", lambda E: E.dma_start(out=o, in_=i), r, w)

        def ldw(o, i, r, w):
            k.pdma("wt", lambda E: E.dma_start(out=o, in_=i), r, w)

        def coll(kind, op, groups, src, dst, r, w):
            k.pdma("cc", lambda E: E.collective_compute(
                kind, op, replica_groups=groups, ins=[src.opt()], outs=[dst.opt()]), r, w)

        G4 = [[0, 1, 2, 3], [4, 5, 6, 7]]

        def allreduce_rows(src, dst, rows, width):
            step = max(128, (1 << 20) // width)
            for r0 in range(0, rows, step):
                r1 = min(rows, r0 + step)
                coll("AllReduce", ALU.add, G4, src[r0:r1, :], dst[r0:r1, :], [src], [dst])

        identf = k.sb("identf", [128, 128], F32); identb = k.sb("identb", [128, 128], BF16)
        onesf = k.sb("onesf", [128, 128], F32); segm = k.sb("segm", [128, 128], F32)
        trix = k.sb("trix", [128, 128], F32); e4 = k.sb("e4", [128, 4], F32)
        selh = k.sb("selh", [128, 128], F32); epsc = k.sb("epsc", [128, 1], F32)
        for t_, n_ in ((identf, "identf"), (identb, "identb"), (segm, "segm"), (trix, "trix"),
                       (e4, "e4"), (selh, "selh")):
            ld(t_[:], I[n_][:], [I[n_]], [t_])
        k.op("dve", lambda E: E.memset(onesf[:], 1.0), w=[onesf])
        k.op("dve", lambda E: E.memset(epsc[:], EPS), w=[epsc])
        cvt = k.sb("cvt", [128, DC, 2], F32); scv = k.sb("scv", [128, DC, 2], F32)
        ld(cvt[:], I["cv"][:], [I["cv"]], [cvt])
        k.op("act", lambda E: E.activation(out=scv[:], in_=cvt[:], func=AF.Silu), r=[cvt], w=[scv])
        for t in range(NTT):
            sto(X[t * 128:(t + 1) * 128, :], I["x"][t * 128:(t + 1) * 128, :], [I["x"]], [X])

        G2 = [[0, 4], [1, 5], [2, 6], [3, 7]]
        ib = 0
        for l_ in range(L):
            for n_ in ("wg", "wu", "wd"):
                for j_ in range(2):
                    for q_ in range(NCE):
                        sto(BNC[ib % 2], I[n_][l_, j_, q_ * CH:(q_ + 1) * CH].rearrange("(p f) -> p f", p=128), [I[n_]], [BNC])
                        coll("AllGather", ALU.bypass, G2, BNC[ib % 2], GB[n_][l_, j_ * NCE + q_].rearrange("r (p f) -> (r p) f", p=128), [BNC], [GB[n_]])
                        ib += 1

        def wsrc(n_, l_, e_, q_, width):
            r_, j_ = divmod(e_, 2)
            return GB[n_][l_, j_ * NCE + q_, r_, :].rearrange("(row f) -> row f", f=width)

        def rstd_col(ssc, dst, inv_n):
            k.op("act", lambda E: E.activation(out=dst[:], in_=ssc[:], func=AF.Ln, bias=epsc[:], scale=inv_n),
                 r=[ssc, epsc], w=[dst])
            k.op("act", lambda E: E.activation(out=dst[:], in_=dst[:], func=AF.Exp, scale=-0.5), r=[dst], w=[dst])

        def mod_phase(l, ps):
            k.barrier()
            with ExitStack() as s2:
                wb = k.sb("modw", [128, DC, 512], F32, s2); row = k.sb("modrow", [2, 512], F32, s2)
                wv = I["wmod"][l].rearrange("(c p) f -> p c f", p=128)
                for blk in range(3):
                    ld(wb[:], wv[:, :, blk * 512:(blk + 1) * 512], [I["wmod"]], [wb])
                    for c in range(DC):
                        k.op("pe", lambda E, c=c: E.matmul(ps[0:2, 0:512], scv[:, c, :], wb[:, c, :],
                                                         start=(c == 0), stop=(c == DC - 1)), r=[scv, wb], w=[ps])
                    k.op("act", lambda E: E.copy(row[:], ps[0:2, 0:512]), r=[ps], w=[row])
                    sto(MODP[:, blk * 512:(blk + 1) * 512], row[:], [row], [MODP])
                coll("AllGather", ALU.bypass, G4, MODP[:, :], MODG[l].rearrange("r a f -> (r a) f"), [MODP], [MODG])

        def mod_tile(l, dst, tmp, j, which, plus_one=False, mul=None):
            for r_ in range(4):
                lo = max(j * D, r_ * 1536); hi = min((j + 1) * D, (r_ + 1) * 1536)
                if lo < hi:
                    ld(dst[:, lo - j * D:hi - j * D], MODG[l, r_, which, lo - r_ * 1536:hi - r_ * 1536].partition_broadcast(128),
                       [MODG], [dst])
            ld(tmp[:], I["bmod"][l, j * D:(j + 1) * D].partition_broadcast(128), [I["bmod"]], [tmp])
            k.op("dve", lambda E: E.tensor_tensor(out=dst[:], in0=dst[:], in1=tmp[:], op=ALU.add), r=[dst, tmp], w=[dst])
            if plus_one:
                k.op("dve", lambda E: E.tensor_scalar(out=dst[:], in0=dst[:], scalar1=1.0, scalar2=None, op0=ALU.add),
                     r=[dst], w=[dst])
            if mul is not None:
                k.op("dve", lambda E: E.tensor_tensor(out=dst[:], in0=dst[:], in1=mul[:], op=ALU.mult), r=[dst, mul], w=[dst])

        def norm_tiles(l, pend_gate_j, nrm_in, jsh, jsc, body, s2, tag):
            gn = k.sb(tag + "gn", [128, D], F32, s2); tmp = k.sb(tag + "tmp", [128, D], F32, s2)
            ld(gn[:], nrm_in[l].partition_broadcast(128), [nrm_in], [gn])
            G = [k.sb(f"{tag}G{w}", [128, D], F32, s2) for w in range(2)]
            S = [k.sb(f"{tag}S{w}", [128, D], F32, s2) for w in range(2)]
            for w in range(2):
                mod_tile(l, G[w], tmp, jsc, w, plus_one=True, mul=gn)
                mod_tile(l, S[w], tmp, jsh, w)
            PG = None
            if pend_gate_j is not None:
                PG = [k.sb(f"{tag}PG{w}", [128, D], F32, s2) for w in range(2)]
                for w in range(2):
                    mod_tile(pend_gate_j[0], PG[w], tmp, pend_gate_j[1], w)
            xt = k.sb(tag + "xt", [128, D], F32, s2); ut = k.sb(tag + "ut", [128, D], F32, s2)
            jk = k.sb(tag + "jk", [128, D], F32, s2); hf = k.sb(tag + "hf", [128, D], F32, s2)
            ss = k.sb(tag + "ss", [128, 1], F32, s2); rs = k.sb(tag + "rs", [128, 1], F32, s2)
            for t in range(NTT):
                which = 0 if t < NLT else 1
                rows = slice(t * 128, (t + 1) * 128)
                ld(xt[:], X[rows, :], [X], [xt])
                if PG is not None:
                    ld(ut[:], UR[rows, :], [UR], [ut])
                    k.op("dve", lambda E, w=which: E.tensor_tensor(out=ut[:], in0=ut[:], in1=PG[w][:], op=ALU.mult),
                         r=[ut, PG[which]], w=[ut])
                    k.op("dve", lambda E: E.tensor_tensor(out=xt[:], in0=xt[:], in1=ut[:], op=ALU.add), r=[xt, ut], w=[xt])
                    sto(X[rows, :], xt[:], [xt], [X])
                k.op("act", lambda E: E.activation(out=jk[:], in_=xt[:], func=AF.Square, accum_out=ss[:]), r=[xt], w=[jk, ss])
                rstd_col(ss, rs, 1.0 / D)
                k.op("dve", lambda E, w=which: E.scalar_tensor_tensor(out=hf[:], in0=xt[:], scalar=rs[:], in1=G[w][:],
                                                                    op0=ALU.mult, op1=ALU.mult), r=[xt, rs, G[which]], w=[hf])
                k.op("dve", lambda E, w=which: E.tensor_tensor(out=hf[:], in0=hf[:], in1=S[w][:], op=ALU.add),
                     r=[hf, S[which]], w=[hf])
                body(t, which, hf)

        def p1_phase(l, pend, P):
            k.barrier()
            with ExitStack() as s2:
                w1 = k.sb("w1", [128, DC, 768], BF16, s2); w2 = k.sb("w2", [128, DC, 448], BF16, s2)
                ldw(w1[:], I["w1"][l].rearrange("(c p) f -> p c f", p=128), [I["w1"]], [w1])
                ldw(w2[:], I["w2"][l].rearrange("(c p) f -> p c f", p=128), [I["w2"]], [w2])
                gqk = k.sb("gqk", [128, 4], F32, s2); sgn = k.sb("sgn", [128, 64], F32, s2)
                ld(gqk[:], I["gqk"][l], [I["gqk"]], [gqk])
                ld(sgn[:], I["sgn"][l].partition_broadcast(128), [I["sgn"]], [sgn])
                hb = k.sb("hb", [128, D], BF16, s2); hT = k.sb("hT", [128, DC, 512], BF16, s2)
                vt = k.sb("vtile", [128, 192], BF16, s2); gsv = k.sb("gsv", [128, 256], F32, s2)
                jk2 = k.sb("jk2", [128, 256], F32, s2); ssv = k.sb("ssv", [128, 1], F32, s2)
                rsv = k.sb("rsv", [128, 1], F32, s2); vn = k.sb("vn", [128, 64], BF16, s2)
                cos = k.sb("cos", [128, 512], F32, s2); sin = k.sb("sin", [128, 512], F32, s2)
                pp = [k.sb(f"pp{i}", [128, 512], F32, s2) for i in range(2)]
                t1 = k.sb("t1", [128, 512], F32, s2); t2 = k.sb("t2", [128, 512], F32, s2)
                sq = k.sb("sqh", [128, 512], F32, s2); rsh = k.sb("rsh", [128, 512], F32, s2)
                fmo = k.sb("fmo", [128, 512], BF16, s2)
                k.op("dve", lambda E: E.memset(sq[:], 0.0), w=[sq])
                pstr, pstok, psfm, pssq = P["a"], P["b"], P["c"], P["d"]
                pstr = P["t"]; trv = pstr.t

                def fm_tile(col0, W):
                    ld(cos[0:64, 0:W], I["cs"][0, :, col0:col0 + W], [I["cs"]], [cos])
                    ld(cos[64:128, 0:W], I["cs"][0, :, col0:col0 + W], [I["cs"]], [cos])
                    ld(sin[0:64, 0:W], I["cs"][1, :, col0:col0 + W], [I["cs"]], [sin])
                    ld(sin[64:128, 0:W], I["cs"][1, :, col0:col0 + W], [I["cs"]], [sin])

                    def proj(oc):
                        for c in range(DC):
                            k.op("pe", lambda E, c=c: E.matmul(psfm[:, 0:W], w1[:, c, oc * 128:(oc + 1) * 128], hT[:, c, 0:W],
                                                             start=(c == 0), stop=(c == DC - 1)), r=[w1, hT], w=[psfm])
                    for i, oc in enumerate((4, 5)):
                        proj(oc)
                        k.op("act", lambda E, i=i: E.copy(pp[i][:, 0:W], psfm[:, 0:W]), r=[psfm], w=[pp[i]])

                    def rope(h, part, gm, gp, norm):
                        sl = slice(h * 64, h * 64 + 64)
                        if norm:
                            k.op("act", lambda E: E.activation(out=sq[sl, 0:W], in_=psfm[sl, 0:W], func=AF.Square), r=[psfm], w=[sq])
                            k.op("pe", lambda E: E.matmul(pssq[:, 0:W], selh[:], sq[:, 0:W], start=True, stop=True), r=[selh, sq], w=[pssq])
                            k.op("act", lambda E: E.activation(out=rsh[sl, 0:W], in_=pssq[sl, 0:W], func=AF.Ln, bias=epsc[sl, :], scale=1.0 / 64),
                                 r=[pssq, epsc], w=[rsh])
                            k.op("act", lambda E: E.activation(out=rsh[sl, 0:W], in_=rsh[sl, 0:W], func=AF.Exp, scale=-0.5), r=[rsh], w=[rsh])
                            k.op("dve", lambda E: E.scalar_tensor_tensor(out=t1[sl, 0:W], in0=psfm[sl, 0:W], scalar=gqk[sl, gm:gm + 1],
                                                                         in1=cos[sl, 0:W], op0=ALU.mult, op1=ALU.mult), r=[psfm, gqk, cos], w=[t1])
                            k.op("dve", lambda E: E.scalar_tensor_tensor(out=t2[sl, 0:W], in0=part[sl, 0:W], scalar=gqk[sl, gp:gp + 1],
                                                                         in1=sin[sl, 0:W], op0=ALU.mult, op1=ALU.mult), r=[part, gqk, sin], w=[t2])
                            k.op("dve", lambda E: E.tensor_tensor(out=t1[sl, 0:W], in0=t1[sl, 0:W], in1=t2[sl, 0:W], op=ALU.add), r=[t1, t2], w=[t1])
                            k.op("dve", lambda E: E.tensor_tensor(out=fmo[sl, 0:W], in0=t1[sl, 0:W], in1=rsh[sl, 0:W], op=ALU.mult), r=[t1, rsh], w=[fmo])
                        else:
                            k.op("dve", lambda E: E.tensor_tensor(out=t1[sl, 0:W], in0=psfm[sl, 0:W], in1=cos[sl, 0:W], op=ALU.mult), r=[psfm, cos], w=[t1])
                            k.op("dve", lambda E: E.tensor_tensor(out=t2[sl, 0:W], in0=part[sl, 0:W], in1=sin[sl, 0:W], op=ALU.mult), r=[part, sin], w=[t2])
                            k.op("dve", lambda E: E.tensor_tensor(out=fmo[sl, 0:W], in0=t1[sl, 0:W], in1=t2[sl, 0:W], op=ALU.add), r=[t1, t2], w=[fmo])

                    for oc in range(4):
                        proj(oc)
                        if oc == 0:
                            k.op("act", lambda E: E.copy(fmo[0:64, 0:W], psfm[0:64, 0:W]), r=[psfm], w=[fmo])
                            rope(1, pp[0], 0, 1, True)
                        elif oc == 1:
                            k.op("act", lambda E: E.copy(fmo[0:64, 0:W], psfm[0:64, 0:W]), r=[psfm], w=[fmo])
                            rope(1, pp[1], 2, 3, True)
                        elif oc == 2:
                            rope(0, pp[0], 0, 0, False)
                            k.op("act", lambda E: E.activation(out=fmo[64:128, 0:W], in_=psfm[64:128, 0:W], func=AF.Gelu), r=[psfm], w=[fmo])
                        else:
                            rope(0, pp[1], 0, 0, False)
                            k.op("act", lambda E: E.copy(fmo[64:128, 0:W], psfm[64:128, 0:W]), r=[psfm], w=[fmo])
                        sto(FM[oc, :, col0:col0 + W], fmo[:, 0:W], [fmo], [FM])

                def body(t, which, hf):
                    sub = t % 4 if t < NLT else (t - NLT)
                    k.op("act", lambda E: E.copy(hb[:], hf[:]), r=[hf], w=[hb])
                    for c in range(DC):
                        k.op("pe", lambda E, c=c: E.transpose(trv[:, c * 128:(c + 1) * 128], hb[:, c * 128:(c + 1) * 128], identb[:]),
                             r=[hb, identb], w=[pstr])
                    k.op("dve", lambda E: E.tensor_copy(hT[:, :, sub * 128:(sub + 1) * 128],
                                                        trv[:, :].rearrange("p (c t) -> p c t", c=DC)), r=[pstr], w=[hT])
                    for c in range(DC):
                        k.op("pe", lambda E, c=c: E.matmul(pstok[:, 0:448], hT[:, c, sub * 128:(sub + 1) * 128], w2[:, c, :],
                                                         start=(c == 0), stop=(c == DC - 1)), r=[hT, w2], w=[pstok])
                    rows = slice(t * 128, (t + 1) * 128)
                    k.op("act", lambda E: E.copy(vt[:], pstok[:, 0:192]), r=[pstok], w=[vt])
                    sto(VT[rows, :], vt[:], [vt], [VT])
                    k.op("act", lambda E: E.activation(out=gsv[:], in_=pstok[:, 192:448], func=AF.Gelu), r=[pstok], w=[gsv])
                    k.op("act", lambda E: E.activation(out=jk2[:], in_=gsv[:], func=AF.Square, accum_out=ssv[:]), r=[gsv], w=[jk2, ssv])
                    rstd_col(ssv, rsv, 1.0 / 256)
                    k.op("dve", lambda E: E.scalar_tensor_tensor(out=vn[:], in0=gsv[:, 0:64], scalar=rsv[:], in1=sgn[:],
                                                                 op0=ALU.mult, op1=ALU.mult), r=[gsv, rsv, sgn], w=[vn])
                    sto(VN[rows, :], vn[:], [vn], [VN])
                    if t < NLT and sub == 3:
                        fm_tile((t - 3) * 128, 512)
                    elif t == NTT - 1:
                        fm_tile(N, CTX)

                norm_tiles(l, pend, I["nmix"], 0, 1, body, s2, "p1")

        def attn_phase(l, P):
            k.barrier()
            with ExitStack() as s2:
                qres = k.sb("qres", [128, TA], BF16, s2); kres = k.sb("kres", [128, TA], BF16, s2)
                vaug = k.sb("vaug", [128, NTT, 128], BF16, s2)
                ssqr = k.sb("ssqres", [128, NTT, 4], F32, s2)
                pt = [k.sb(f"pt{i}", [128, 512], BF16, s2) for i in range(3)]
                bt = [k.sb(f"bt{i}", [128, 512], F32, s2) for i in range(2)]
                sbt = k.sb("sbt", [128, 512], F32, s2)
                rr = k.sb("rr", [128, 512], F32, s2); rs0 = k.sb("rs0", [128, 512], F32, s2)
                y32 = k.sb("y32", [128, 512], F32, s2); ysq = k.sb("ysq", [128, 512], F32, s2)
                yb = k.sb("yb", [128, 512], BF16, s2)
                esk = k.sb("esk", [128, 1], F32, s2); skt = k.sb("skt", [128, 1], F32, s2)
                vna = k.sb("vna", [128, 4, 128], BF16, s2); wsb = k.sb("wsb", [128, 128], BF16, s2)
                bst = k.sb("bst", [128, 128], F32, s2)
                pss = [P["a"], P["b"]]; pso, psr, psq = P["c"], P["d"], P["e"]
                k.op("dve", lambda E: E.memset(vaug[:], 1.0), w=[vaug])
                k.op("dve", lambda E: E.memset(ssqr[:], 0.0), w=[ssqr])
                k.op("dve", lambda E: E.memset(vna[:], 0.0), w=[vna])
                ld(skt[:], I["snk"][l], [I["snk"]], [skt])
                k.op("act", lambda E: E.activation(out=esk[:], in_=skt[:], func=AF.Exp), r=[skt], w=[esk])
                cnt = [0, 0]

                def load_v(col):
                    for t0 in range(0, NTT, 16):
                        t1_ = min(NTT, t0 + 16)
                        ld(vaug[:, t0:t1_, 0:64], VT[t0 * 128:t1_ * 128, col * 64:(col + 1) * 64].rearrange("(t p) c -> p t c", p=128),
                           [VT], [vaug])

                def finish(p0, m, col0, W, sink):
                    if sink:
                        k.op("dve", lambda E: E.tensor_scalar(out=rr[64:128, 0:W], in0=pso[64:128, 0:W], scalar1=esk[64:128, :], scalar2=None,
                                                              op0=ALU.add), r=[pso, esk], w=[rr])
                        k.op("dve", lambda E: E.reciprocal(rr[64:128, 0:W], rr[64:128, 0:W]), r=[rr], w=[rr])
                    else:
                        k.op("dve", lambda E: E.reciprocal(rr[64:128, 0:W], pso[64:128, 0:W]), r=[pso], w=[rr])
                    k.op("pe", lambda E: E.matmul(psr[0:64, 0:W], identf[64:128, 64:128], rr[64:128, 0:W], start=True, stop=True),
                         r=[identf, rr], w=[psr])
                    k.op("act", lambda E: E.copy(rs0[0:64, 0:W], psr[0:64, 0:W]), r=[psr], w=[rs0])
                    k.op("dve", lambda E: E.tensor_tensor(out=y32[0:64, 0:W], in0=pso[0:64, 0:W], in1=rs0[0:64, 0:W], op=ALU.mult),
                         r=[pso, rs0], w=[y32])
                    ytail(slice(0, 64), m, col0, W)

                def ytail(sl, m, col0, W):
                    k.op("act", lambda E: E.copy(yb[sl, 0:W], y32[sl, 0:W]), r=[y32], w=[yb])
                    sto(YT[m, :, col0:col0 + W], yb[sl, 0:W], [yb], [YT])
                    k.op("act", lambda E: E.activation(out=ysq[sl, 0:W], in_=y32[sl, 0:W], func=AF.Square), r=[y32], w=[ysq])
                    for s_ in range(W // 128):
                        tt = col0 // 128 + s_
                        k.op("pe", lambda E, s_=s_: E.matmul(psq[:, 0:1], ysq[sl, s_ * 128:(s_ + 1) * 128], onesf[sl, 0:1], start=True, stop=True),
                             r=[ysq, onesf], w=[psq])
                        k.op("dve", lambda E, tt=tt: E.tensor_copy(ssqr[:, tt, m:m + 1], psq[:, 0:1]), r=[psq], w=[ssqr])

                def attn_tile(p0, m, col0, W, chunks, sink):
                    sl = slice(p0, p0 + 64)
                    n = len(chunks)
                    for j, (kt, bias) in enumerate(chunks):
                        ps = pss[cnt[0] % 2]; cnt[0] += 1
                        p = pt[cnt[1] % 3]; cnt[1] += 1
                        k.op("pe", lambda E, ps=ps, kt=kt: E.matmul(ps[:, 0:W], kres[sl, kt * 128:(kt + 1) * 128], qres[sl, col0:col0 + W],
                                                                   start=True, stop=True), r=[kres, qres], w=[ps])
                        if bias is not None:
                            b = bt[j % 2]
                            ld(b[:, 0:W], bias, [I["nab"], I["wm"]], [b])
                            k.op("dve", lambda E, ps=ps, b=b: E.scalar_tensor_tensor(out=sbt[:, 0:W], in0=ps[:, 0:W], scalar=0.125, in1=b[:, 0:W],
                                                                                    op0=ALU.mult, op1=ALU.add), r=[ps, b], w=[sbt])
                            k.op("act", lambda E, p=p: E.activation(out=p[:, 0:W], in_=sbt[:, 0:W], func=AF.Exp), r=[sbt], w=[p])
                        else:
                            k.op("act", lambda E, p=p, ps=ps: E.activation(out=p[:, 0:W], in_=ps[:, 0:W], func=AF.Exp, scale=0.125), r=[ps], w=[p])
                        k.op("pe", lambda E, p=p, kt=kt, j=j: E.matmul(pso[:, 0:W], vaug[:, kt, :], p[:, 0:W], start=(j == 0), stop=(j == n - 1)),
                             r=[vaug, p], w=[pso])
                    finish(p0, m, col0, W, sink)

                ctxc = [(NLT, None), (NLT + 1, None)]
                NQT = N // 512
                ld(qres[:], FM[0], [FM], [qres]); ld(kres[:], FM[1], [FM], [kres])
                load_v(0)
                for t in range(NQT):
                    v = 0 if t == 0 else (2 if t == NQT - 1 else 1)
                    ch = [(4 * t - 2 + i, I["nab"][l, v, i]) for i in range(8) if 0 <= 4 * t - 2 + i < NLT]
                    attn_tile(0, 0, t * 512, 512, ch + ctxc, False)
                attn_tile(0, 0, N, CTX, [(kt, None) for kt, _ in ctxc], False)
                load_v(1)
                allc = [(kt, None) for kt in range(NTT)]
                for t in range(NQT):
                    attn_tile(64, 1, t * 512, 512, allc, False)
                attn_tile(64, 1, N, CTX, ctxc, False)
                ld(qres[:], FM[2], [FM], [qres]); ld(kres[:], FM[3], [FM], [kres])
                load_v(2)
                for t in range(NQT):
                    ch = [(4 * t - 1 + i, I["wm"][i]) for i in range(6) if 0 <= 4 * t - 1 + i < NLT]
                    attn_tile(0, 2, t * 512, 512, ch + ctxc, True)
                attn_tile(0, 2, N, CTX, ctxc, True)
                ld(sbt[:, 0:128], I["ws"][l], [I["ws"]], [sbt])
                k.op("act", lambda E: E.copy(wsb[:], sbt[:, 0:128]), r=[sbt], w=[wsb])
                ld(bst[:], I["bs"][l].partition_broadcast(128), [I["bs"]], [bst])
                for t0 in range(0, NTT, 4):
                    nt_ = min(4, NTT - t0)
                    W = nt_ * 128
                    ld(vna[:, 0:nt_, 64:128], VN[t0 * 128:(t0 + nt_) * 128, :].rearrange("(t p) c -> p t c", p=128), [VN], [vna])
                    for s_ in range(nt_):
                        k.op("pe", lambda E, s_=s_: E.matmul(pso[:, s_ * 128:(s_ + 1) * 128], vna[:, s_, :], wsb[:], start=True, stop=True),
                             r=[vna, wsb], w=[pso])
                    for s_ in range(nt_):
                        k.op("dve", lambda E, s_=s_: E.tensor_tensor(out=y32[64:128, s_ * 128:(s_ + 1) * 128], in0=pso[64:128, s_ * 128:(s_ + 1) * 128],
                                                                    in1=bst[64:128, :], op=ALU.add), r=[pso, bst], w=[y32])
                    k.op("act", lambda E, t0=t0, W=W: E.copy(ysq[64:128, 0:W], qres[64:128, t0 * 128:t0 * 128 + W]), r=[qres], w=[ysq])
                    k.op("dve", lambda E, W=W: E.tensor_tensor(out=y32[64:128, 0:W], in0=y32[64:128, 0:W], in1=ysq[64:128, 0:W], op=ALU.mult),
                         r=[y32, ysq], w=[y32])
                    ytail(slice(64, 128), 3, t0 * 128, W)
                sto(SSQ[:, :].rearrange("(t p) m -> p t m", p=128), ssqr[:], [ssqr], [SSQ])
            coll("AllReduce", ALU.add, G4, SSQ[:, :], SSQR[:, :], [SSQ], [SSQR])

        def merge_phase(l, P):
            k.barrier()
            with ExitStack() as s2:
                wo = k.sb("wo", [128, 4, D], BF16, s2); onr = k.sb("onr", [128, 4], F32, s2)
                for m in range(4):
                    p0 = 64 if m == 3 else 0
                    ldw(wo[p0:p0 + 64, m, :], I["wo"][l, m], [I["wo"]], [wo])
                ld(onr[:], I["onrm"][l], [I["onrm"]], [onr])
                ym = k.sb("ym", [128, 4, 128], BF16, s2); yg = k.sb("yg", [128, 4, 128], BF16, s2)
                sq4 = k.sb("sq4", [128, 4], F32, s2); r4 = k.sb("r4", [128, 4], F32, s2)
                ut = k.sb("mut", [128, D], F32, s2)
                pz = [P["a"], P["b"], P["c"], P["d"]]
                for t in range(NTT):
                    cols = slice(t * 128, (t + 1) * 128)
                    for m in range(4):
                        p0 = 64 if m == 3 else 0
                        ld(ym[p0:p0 + 64, m, :], YT[m, :, cols], [YT], [ym])
                    ld(sq4[:], SSQR[cols, :], [SSQR], [sq4])
                    k.op("act", lambda E: E.activation(out=r4[:], in_=sq4[:], func=AF.Ln, bias=epsc[:], scale=1.0 / 256), r=[sq4, epsc], w=[r4])
                    k.op("act", lambda E: E.activation(out=r4[:], in_=r4[:], func=AF.Exp, scale=-0.5), r=[r4], w=[r4])
                    for m in range(4):
                        sl = slice(64, 128) if m == 3 else slice(0, 64)
                        k.op("dve", lambda E, m=m, sl=sl: E.tensor_scalar(out=yg[sl, m, :], in0=ym[sl, m, :], scalar1=onr[sl, m:m + 1], scalar2=None,
                                                                        op0=ALU.mult), r=[ym, onr], w=[yg])
                    for dh in range(2):
                        for m in range(4):
                            sl = slice(64, 128) if m == 3 else slice(0, 64)
                            k.op("pe", lambda E, m=m, sl=sl, dh=dh: E.matmul(pz[m][:, 0:512], yg[sl, m, :], wo[sl, m, dh * 512:(dh + 1) * 512],
                                                                           start=True, stop=True), r=[yg, wo], w=[pz[m]])
                        dsl = slice(dh * 512, (dh + 1) * 512)
                        k.op("dve", lambda E, dsl=dsl: E.tensor_scalar(out=ut[:, dsl], in0=pz[0][:, 0:512], scalar1=r4[:, 0:1], scalar2=None, op0=ALU.mult),
                             r=[pz[0], r4], w=[ut])
                        for m in range(1, 4):
                            k.op("dve", lambda E, m=m, dsl=dsl: E.scalar_tensor_tensor(out=ut[:, dsl], in0=pz[m][:, 0:512], scalar=r4[:, m:m + 1],
                                                                                      in1=ut[:, dsl], op0=ALU.mult, op1=ALU.add), r=[pz[m], r4, ut], w=[ut])
                    sto(U[cols, :], ut[:], [ut], [U])
            allreduce_rows(U, UR, TA, D)

        def router_phase(l, P):
            k.barrier()
            with ExitStack() as s2:
                wr = k.sb("wr", [128, DC, 16], F32, s2)
                ld(wr[:], I["wr"][l].rearrange("(c p) e -> p c e", p=128), [I["wr"]], [wr])
                hb = k.sb("h2b", [128, D], BF16, s2); hT = k.sb("h2T", [128, DC, 128], F32, s2)
                ex = k.sb("ex", [128, 16], F32, s2); sm = k.sb("sm", [128, 1], F32, s2)
                af = k.sb("af", [128, 16], F32, s2); aT = k.sb("aT", [16, 128], F32, s2)
                pta, ptb, pl, pa = P["a"], P["b"], P["c"], P["d"]

                def body(t, which, hf):
                    rows = slice(t * 128, (t + 1) * 128)
                    k.op("act", lambda E: E.copy(hb[:], hf[:]), r=[hf], w=[hb])
                    sto(H2[rows, :], hb[:], [hb], [H2])
                    for c in range(DC):
                        pt_ = pta if c < 4 else ptb
                        k.op("pe", lambda E, c=c, pt_=pt_: E.transpose(pt_[:, (c % 4) * 128:(c % 4 + 1) * 128], hf[:, c * 128:(c + 1) * 128], identf[:]),
                             r=[hf, identf], w=[pt_])
                    k.op("dve", lambda E: E.tensor_copy(hT[:, 0:4, :], pta[:, :].rearrange("p (c t) -> p c t", c=4)), r=[pta], w=[hT])
                    k.op("act", lambda E: E.copy(hT[:, 4:8, :], ptb[:, :].rearrange("p (c t) -> p c t", c=4)), r=[ptb], w=[hT])
                    for c in range(DC):
                        k.op("pe", lambda E, c=c: E.matmul(pl[:, 0:16], hT[:, c, :], wr[:, c, :], start=(c == 0), stop=(c == DC - 1)), r=[hT, wr], w=[pl])
                    k.op("act", lambda E: E.activation(out=ex[:], in_=pl[:, 0:16], func=AF.Exp, accum_out=sm[:]), r=[pl], w=[ex, sm])
                    k.op("dve", lambda E: E.reciprocal(sm[:], sm[:]), r=[sm], w=[sm])
                    k.op("dve", lambda E: E.tensor_scalar(out=af[:], in0=ex[:], scalar1=sm[:], scalar2=None, op0=ALU.mult), r=[ex, sm], w=[af])
                    sto(AFF[rows, :], af[:], [af], [AFF])
                    k.op("pe", lambda E: E.transpose(pa[0:16, 0:128], af[:], identf[:]), r=[af, identf], w=[pa])
                    k.op("act", lambda E: E.copy(aT[:], pa[0:16, 0:128]), r=[pa], w=[aT])
                    sto(AFFT[:, rows], aT[:], [aT], [AFFT])

                norm_tiles(l, (l, 2), I["nffn"], 3, 4, body, s2, "p6")

        def moe_phase(l, P):
            k.barrier()
            with ExitStack() as s2:
                slot = k.sb("slot", [128, NTT * 4], U32, s2); gmr = k.sb("gmr", [128, NTT, 4], F32, s2)
                thr = [k.sb(f"thr{w}", [128, 4], F32, s2) for w in range(2)]
                k.barrier()
                with ExitStack() as s3:
                    afl = k.sb("afl", [128, SEGW], F32, s3); afc = k.sb("afc", [128, CTX], F32, s3)
                    jb = k.sb("jb", [128, max(SEGW, CTX)], F32, s3)
                    lo = [k.sb(f"lo{w}", [128, 1], F32, s3) for w in range(2)]
                    mid = k.sb("mid", [128, 1], F32, s3); cn = k.sb("cn", [128, 1], F32, s3)
                    pr = k.sb("pr", [128, 1], F32, s3); tm = k.sb("tm", [128, 4], F32, s3)
                    pc = P["a"]
                    for e_ in range(4):
                        ld(afl[e_ * 32:(e_ + 1) * 32, :], AFFT[e_, 0:N].rearrange("(s n) -> s n", s=32), [AFFT], [afl])
                    k.op("dve", lambda E: E.memset(afc[:], 0.0), w=[afc])
                    ld(afc[0:4, :], AFFT[0:4, N:TA], [AFFT], [afc])
                    for w, (a, width, cap) in enumerate(((afl, SEGW, CAP), (afc, CTX, CCAP))):
                        k.op("dve", lambda E, w=w: E.memset(lo[w][:], 0.0), w=[lo[w]])
                        for it in range(1, 33):
                            wi = 2.0 ** (-it)
                            k.op("dve", lambda E, w=w, wi=wi: E.tensor_scalar(out=mid[:], in0=lo[w][:], scalar1=wi, scalar2=None, op0=ALU.add),
                                 r=[lo[w]], w=[mid])
                            k.op("dve", lambda E, a=a, width=width: E.tensor_scalar(out=jb[:, 0:width], in0=a[:, 0:width], scalar1=mid[:], scalar2=0.0,
                                                                                   op0=ALU.is_ge, op1=ALU.add, accum_out=cn[:]), r=[a, mid], w=[jb, cn])
                            if w == 0:
                                k.op("pe", lambda E: E.matmul(pc[:, 0:1], segm[:], cn[:], start=True, stop=True), r=[segm, cn], w=[pc])
                                src = pc[:, 0:1]; sb_ = pc
                            else:
                                src = cn[:]; sb_ = cn
                            k.op("dve", lambda E, src=src, cap=cap: E.tensor_scalar(out=pr[:], in0=src, scalar1=float(cap), scalar2=None, op0=ALU.is_ge),
                                 r=[sb_], w=[pr])
                            k.op("dve", lambda E, w=w, wi=wi: E.scalar_tensor_tensor(out=lo[w][:], in0=pr[:], scalar=wi, in1=lo[w][:],
                                                                                    op0=ALU.mult, op1=ALU.add), r=[pr, lo[w]], w=[lo[w]])
                    k.op("dve", lambda E: E.tensor_scalar(out=tm[:], in0=e4[:], scalar1=lo[0][:], scalar2=None, op0=ALU.mult), r=[e4, lo[0]], w=[tm])
                    k.op("pe", lambda E: E.matmul(pc[:, 0:4], onesf[:], tm[:], start=True, stop=True), r=[onesf, tm], w=[pc])
                    k.op("act", lambda E: E.copy(thr[0][:], pc[:, 0:4]), r=[pc], w=[thr[0]])
                    k.op("dve", lambda E: E.tensor_scalar(out=tm[0:4, :], in0=identf[0:4, 0:4], scalar1=lo[1][0:4, :], scalar2=None, op0=ALU.mult),
                         r=[identf, lo[1]], w=[tm])
                    k.op("pe", lambda E: E.matmul(pc[:, 0:4], onesf[0:4, :], tm[0:4, :], start=True, stop=True), r=[onesf, tm], w=[pc])
                    k.op("act", lambda E: E.copy(thr[1][:], pc[:, 0:4]), r=[pc], w=[thr[1]])
                k.barrier()
                with ExitStack() as s3:
                    af = k.sb("maf", [128, 16], F32, s3); mk = k.sb("mk", [128, 4], F32, s3)
                    pos = k.sb("pos", [128, 4], F32, s3); car = k.sb("car", [128, 4], F32, s3)
                    sf = k.sb("sf", [128, 4], F32, s3); m2 = k.sb("m2", [128, 4], F32, s3)
                    hr = [k.sb(f"hr{i}", [128, D], BF16, s3) for i in range(2)]
                    pp_, pcs = P["a"], P["b"]
                    k.op("dve", lambda E: E.memset(car[:], 0.0), w=[car])
                    for t in range(NTT):
                        which = 0 if t < NLT else 1
                        rows = slice(t * 128, (t + 1) * 128)
                        h = hr[t % 2]
                        if t == NLT:
                            k.op("dve", lambda E: E.memset(car[:], float(CAP)), w=[car])
                        lim = float(CAP) if which == 0 else float(CAP + CCAP)
                        ld(af[:], AFF[rows, :], [AFF], [af])
                        ld(h[:], H2[rows, :], [H2], [h])
                        k.op("dve", lambda E, w=which: E.tensor_tensor(out=mk[:], in0=af[:, 0:4], in1=thr[w][:], op=ALU.is_ge), r=[af, thr[which]], w=[mk])
                        k.op("pe", lambda E: E.matmul(pp_[:, 0:4], trix[:], mk[:], start=True, stop=True), r=[trix, mk], w=[pp_])
                        k.op("pe", lambda E: E.matmul(pcs[:, 0:4], onesf[:], mk[:], start=True, stop=True), r=[onesf, mk], w=[pcs])
                        k.op("dve", lambda E: E.tensor_tensor(out=pos[:], in0=pp_[:, 0:4], in1=car[:], op=ALU.add), r=[pp_, car], w=[pos])
                        k.op("dve", lambda E: E.tensor_tensor(out=car[:], in0=pcs[:, 0:4], in1=car[:], op=ALU.add), r=[pcs, car], w=[car])
                        k.op("dve", lambda E: E.scalar_tensor_tensor(out=sf[:], in0=pos[:], scalar=-BIG, in1=mk[:], op0=ALU.add, op1=ALU.mult), r=[pos, mk], w=[sf])
                        k.op("dve", lambda E: E.tensor_scalar(out=sf[:], in0=sf[:], scalar1=BIG, scalar2=None, op0=ALU.add), r=[sf], w=[sf])
                        k.op("dve", lambda E, lim=lim: E.tensor_scalar(out=m2[:], in0=sf[:], scalar1=lim, scalar2=None, op0=ALU.is_lt), r=[sf], w=[m2])
                        k.op("dve", lambda E: E.scalar_tensor_tensor(out=sf[:], in0=sf[:], scalar=-BIG, in1=m2[:], op0=ALU.add, op1=ALU.mult), r=[sf, m2], w=[sf])
                        k.op("dve", lambda E: E.tensor_scalar(out=sf[:], in0=sf[:], scalar1=BIG, scalar2=None, op0=ALU.add), r=[sf], w=[sf])
                        k.op("dve", lambda E, t=t: E.tensor_copy(slot[:, t * 4:t * 4 + 4], sf[:]), r=[sf], w=[slot])
                        k.op("dve", lambda E, t=t: E.tensor_tensor(out=gmr[:, t, :], in0=af[:, 0:4], in1=m2[:], op=ALU.mult), r=[af, m2], w=[gmr])
                        for e in range(4):
                            k.pdma("gs", lambda E, e=e, t=t, h=h: E.indirect_dma_start(
                                out=XE[e][:, :], out_offset=bass.IndirectOffsetOnAxis(ap=slot[:, t * 4 + e:t * 4 + e + 1], axis=0), in_=h[:], in_offset=None,
                                bounds_check=k.preg(E, NS - 1), oob_is_err=False), r=[slot, h], w=[XE[e]], slot=(t % 2) * 4 + e, selfwait=False)
                k.barrier()
                with ExitStack() as s3:
                    HT = (NST + 1) // 2
                    xr = k.sb("xr", [128, D], BF16, s3); xeT = k.sb("xeT", [128, DC, HT * 128], BF16, s3)
                    hid = k.sb("hid", [128, FC, HT * 128], BF16, s3)
                    wgs = k.sb("wgs", [128, DC, 512], BF16, s3); wus = k.sb("wus", [128, DC, 512], BF16, s3)
                    wds = k.sb("wds", [128, FC, D], BF16, s3)
                    sg = k.sb("sg", [128, 512], F32, s3); ye = k.sb("ye", [128, D], F32, s3)
                    ptr, pg, pu, py0, py1 = P["a"], P["b"], P["c"], P["d"], P["e"]
                    ptr = P["t"]; trv = ptr.t
                    for e in range(4):
                        for hs in range(2):
                            tiles = list(range(hs * HT, min(NST, (hs + 1) * HT)))
                            if not tiles:
                                continue
                            for i, stl in enumerate(tiles):
                                ld(xr[:], XE[e][stl * 128:(stl + 1) * 128, :], [XE[e]], [xr])
                                for c in range(DC):
                                    k.op("pe", lambda E, c=c: E.transpose(trv[:, c * 128:(c + 1) * 128], xr[:, c * 128:(c + 1) * 128], identb[:]),
                                         r=[xr, identb], w=[ptr])
                                k.op("dve", lambda E, i=i: E.tensor_copy(xeT[:, :, i * 128:(i + 1) * 128], trv[:, :].rearrange("p (c t) -> p c t", c=DC)),
                                     r=[ptr], w=[xeT])
                            SW = len(tiles) * 128
                            groups = [(g0, min(512, SW - g0)) for g0 in range(0, SW, 512)]
                            for fb in range(FF // 512):
                                cq = DC // NCE
                                for q_ in range(NCE):
                                    ldw(wgs[:, q_ * cq:(q_ + 1) * cq, :], wsrc("wg", l, e, q_, FF)[:, fb * 512:(fb + 1) * 512].rearrange("(c p) f -> p c f", p=128), [GB["wg"]], [wgs])
                                    ldw(wus[:, q_ * cq:(q_ + 1) * cq, :], wsrc("wu", l, e, q_, FF)[:, fb * 512:(fb + 1) * 512].rearrange("(c p) f -> p c f", p=128), [GB["wu"]], [wus])
                                for fc in range(4):
                                    for (g0, gw) in groups:
                                        for c in range(DC):
                                            k.op("pe", lambda E, c=c, fc=fc, g0=g0, gw=gw: E.matmul(pg[:, 0:gw], wgs[:, c, fc * 128:(fc + 1) * 128], xeT[:, c, g0:g0 + gw],
                                                                                                  start=(c == 0), stop=(c == DC - 1)), r=[wgs, xeT], w=[pg])
                                        for c in range(DC):
                                            k.op("pe", lambda E, c=c, fc=fc, g0=g0, gw=gw: E.matmul(pu[:, 0:gw], wus[:, c, fc * 128:(fc + 1) * 128], xeT[:, c, g0:g0 + gw],
                                                                                                  start=(c == 0), stop=(c == DC - 1)), r=[wus, xeT], w=[pu])
                                        k.op("act", lambda E, gw=gw: E.activation(out=sg[:, 0:gw], in_=pg[:, 0:gw], func=AF.Silu), r=[pg], w=[sg])
                                        k.op("dve", lambda E, fb=fb, fc=fc, g0=g0, gw=gw: E.tensor_tensor(out=hid[:, fb * 4 + fc, g0:g0 + gw], in0=sg[:, 0:gw], in1=pu[:, 0:gw], op=ALU.mult),
                                             r=[sg, pu], w=[hid])
                            cqd = FC // NCE
                            for q_ in range(NCE):
                                ldw(wds[:, q_ * cqd:(q_ + 1) * cqd, :], wsrc("wd", l, e, q_, D).rearrange("(c p) d -> p c d", p=128), [GB["wd"]], [wds])
                            for i, stl in enumerate(tiles):
                                for dh, py in enumerate((py0, py1)):
                                    for fc in range(FC):
                                        k.op("pe", lambda E, fc=fc, i=i, dh=dh, py=py: E.matmul(py[:, 0:512], hid[:, fc, i * 128:(i + 1) * 128], wds[:, fc, dh * 512:(dh + 1) * 512],
                                                                                              start=(fc == 0), stop=(fc == FC - 1)), r=[hid, wds], w=[py])
                                k.op("act", lambda E: E.copy(ye[:, 0:512], py0[:, 0:512]), r=[py0], w=[ye])
                                k.op("dve", lambda E: E.tensor_copy(ye[:, 512:1024], py1[:, 0:512]), r=[py1], w=[ye])
                                sto(YE[e][stl * 128:(stl + 1) * 128, :], ye[:], [ye], [YE[e]])
                k.barrier()
                with ExitStack() as s3:
                    gt = [k.sb(f"gt{i}", [128, D], F32, s3) for i in range(2)]
                    acc = k.sb("acc", [128, D], F32, s3)
                    for g_ in gt:
                        k.op("dve", lambda E, g_=g_: E.memset(g_[:], 0.0), w=[g_])
                    for t in range(NTT):
                        rows = slice(t * 128, (t + 1) * 128)
                        for e in range(4):
                            g_ = gt[e % 2]
                            k.pdma("gs", lambda E, e=e, t=t, g_=g_: E.indirect_dma_start(
                                out=g_[:], out_offset=None, in_=YE[e][:, :], in_offset=bass.IndirectOffsetOnAxis(ap=slot[:, t * 4 + e:t * 4 + e + 1], axis=0),
                                bounds_check=k.preg(E, NS - 1), oob_is_err=False), r=[slot, YE[e]], w=[g_], slot=e % 2, selfwait=False)
                            if e == 0:
                                k.op("dve", lambda E, t=t, g_=g_: E.tensor_scalar(out=acc[:], in0=g_[:], scalar1=gmr[:, t, 0:1], scalar2=None, op0=ALU.mult),
                                     r=[g_, gmr], w=[acc])
                            else:
                                k.op("dve", lambda E, t=t, e=e, g_=g_: E.scalar_tensor_tensor(out=acc[:], in0=g_[:], scalar=gmr[:, t, e:e + 1], in1=acc[:],
                                                                                             op0=ALU.mult, op1=ALU.add), r=[g_, gmr, acc], w=[acc])
                        sto(U[rows, :], acc[:], [acc], [U])
            allreduce_rows(U, UR, TA, D)

        def final_phase(P):
            k.barrier()
            with ExitStack() as s2:
                tmp = k.sb("ftmp", [128, D], F32, s2); g2t = k.sb("fg2", [128, D], F32, s2)
                mod_tile(L - 1, g2t, tmp, 5, 0)
                gf = k.sb("fgf", [128, D], F32, s2)
                ld(gf[:], I["fnorm"][:].partition_broadcast(128), [I["fnorm"]], [gf])
                oi = k.sb("oi", [128, NQ // 128], U32, s2)
                ld(oi[:], I["oidx"][:], [I["oidx"]], [oi])
                xt = k.sb("fxt", [128, D], F32, s2); ut = k.sb("fut", [128, D], F32, s2)
                jk = k.sb("fjk", [128, D], F32, s2); ss = k.sb("fss", [128, 1], F32, s2); rs = k.sb("frs", [128, 1], F32, s2)
                yo = k.sb("fyo", [128, D], F32, s2)
                for t in range(NQ // 128):
                    for dst, src in ((xt, X), (ut, UR)):
                        k.pdma("gs", lambda E, dst=dst, src=src, t=t: E.indirect_dma_start(
                            out=dst[:], out_offset=None, in_=src[:, :], in_offset=bass.IndirectOffsetOnAxis(ap=oi[:, t:t + 1], axis=0),
                            bounds_check=k.preg(E, TA - 1), oob_is_err=False), r=[oi, src], w=[dst], slot=(0 if src is X else 1), selfwait=False)
                    k.op("dve", lambda E: E.tensor_tensor(out=ut[:], in0=ut[:], in1=g2t[:], op=ALU.mult), r=[ut, g2t], w=[ut])
                    k.op("dve", lambda E: E.tensor_tensor(out=xt[:], in0=xt[:], in1=ut[:], op=ALU.add), r=[xt, ut], w=[xt])
                    k.op("act", lambda E: E.activation(out=jk[:], in_=xt[:], func=AF.Square, accum_out=ss[:]), r=[xt], w=[jk, ss])
                    rstd_col(ss, rs, 1.0 / D)
                    k.op("dve", lambda E: E.scalar_tensor_tensor(out=yo[:], in0=xt[:], scalar=rs[:], in1=gf[:], op0=ALU.mult, op1=ALU.mult),
                         r=[xt, rs, gf], w=[yo])
                    sto(out[t * 128:(t + 1) * 128, :], yo[:], [yo], [out])

        P = {n_: k.ps("ps_" + n_, [128, 512], F32) for n_ in "abcde"}
        P["t"] = k.ps("ps_t", [128, 1024], BF16)
        for l in range(L):
            mod_phase(l, P["a"])
            p1_phase(l, None if l == 0 else (l - 1, 5), P)
            attn_phase(l, P)
            merge_phase(l, P)
            router_phase(l, P)
            moe_phase(l, P)
        final_phase(P)
        if dbg:
            dl = dict(X=X, MODG=MODG, FM=FM, VT=VT, VN=VN, YT=YT, SSQR=SSQR, UR=UR, U=U, H2=H2, AFF=AFF, AFFT=AFFT,
                      XE0=XE[0], YE0=YE[0])
            for n_, t_ in dl.items():
                ap = t_.t
                shp = list(ap.shape)
                o_ = nc.dram_tensor("dbg_" + n_, shp, ap.dtype, kind="ExternalOutput").ap()
                fl = "a b c d -> (a b c) d" if len(shp) == 4 else ("a b c -> (a b) c" if len(shp) == 3 else None)
                a2 = ap.rearrange(fl) if fl else ap
                o2 = o_.rearrange(fl) if fl else o_
                R_ = a2.shape[0]
                for r0 in range(0, R_, 1024):
                    r1 = min(R_, r0 + 1024)
                    sto(o2[r0:r1, :], a2[r0:r1, :], [t_], [])
        k.drain("sp", k.rings["st"] + k.rings["ld"] + k.rings["gs"] + k.rings["wt"] + k.rings["cc"])
        k.emit()
        build.ninst = k.ninst
    return nc


def _rope_tables(N):
    t = np.arange(N)
    row = (t // GRID_W).astype(np.float32)
    col = (t % GRID_W).astype(np.float32)
    freqs = (10000.0 ** (-np.arange(16, dtype=np.float32) / 16)).astype(np.float32)
    cos = np.ones((64, N + CTX), np.float32)
    sins = np.zeros((64, N + CTX), np.float32)
    for ax, p in enumerate((row, col)):
        ang = p[None, :] * freqs[:, None]
        c, s = np.cos(ang), np.sin(ang)
        cos[ax * 32:ax * 32 + 16, :N] = c
        cos[ax * 32 + 16:ax * 32 + 32, :N] = c
        sins[ax * 32:ax * 32 + 16, :N] = -s
        sins[ax * 32 + 16:ax * 32 + 32, :N] = s
    return np.stack([cos, sins]).astype(np.float32)


_PARTNER = np.concatenate([np.arange(16, 32), np.arange(0, 16), np.arange(48, 64), np.arange(32, 48)])


def _na_bias(rpb_h, N):
    ROWS = N // GRID_W
    out = np.full((3, 8, 2, 64, 8, 64), NEG, np.float32)
    cols = np.arange(GRID_W)
    c_start = np.clip(cols - 8, 0, GRID_W - 16)
    kc = np.arange(GRID_W)[:, None]
    qc = np.arange(GRID_W)[None, :]
    cmask = (kc >= c_start[None, :]) & (kc < c_start[None, :] + 16)
    coff = np.clip(kc - qc + 15, 0, 30)
    win_r = min(8, ROWS)
    for v, R0 in enumerate((0, 8, ROWS - 8)):
        for i in range(8):
            for krl in range(2):
                KR = R0 - 4 + 2 * i + krl
                if KR < 0 or KR >= ROWS:
                    continue
                for qr in range(8):
                    R = R0 + qr
                    rs = int(np.clip(R - win_r // 2, 0, ROWS - win_r))
                    if not (rs <= KR < rs + win_r):
                        continue
                    ro = KR - R + 7
                    blk = rpb_h[ro][coff]
                    out[v, i, krl, :, qr, :] = np.where(cmask, blk, np.float32(NEG))
    return out.reshape(3, 8, 128, 512)


def _win_masks():
    out = np.full((6, 128, 512), NEG, np.float32)
    kk = np.arange(128)[:, None]
    qq = np.arange(512)[None, :]
    for i in range(6):
        kpos = (i - 1) * 128 + kk
        out[i] = np.where(np.abs(kpos - qq) <= 128, np.float32(0.0), np.float32(NEG))
    return out


def prep_inputs(N, L, FF, x, c, ctx, c_ctx, w_mod, b_mod, norm_mix, norm_ffn, w_in, rpb, q_norm, k_norm, sink,
                sgu_norm, w_sgu, b_sgu, out_norm, w_out, w_router, w_gate, w_up, w_down, final_norm):
    f = lambda a: np.ascontiguousarray(np.asarray(a, dtype=np.float32))
    x, c, ctx, c_ctx, w_mod, b_mod, norm_mix, norm_ffn, w_in, rpb, q_norm, k_norm, sink, sgu_norm, w_sgu, b_sgu, out_norm, \
        w_out, w_router, w_gate, w_up, w_down, final_norm = map(f, (
            x, c, ctx, c_ctx, w_mod, b_mod, norm_mix, norm_ffn, w_in, rpb, q_norm, k_norm, sink, sgu_norm, w_sgu, b_sgu,
            out_norm, w_out, w_router, w_gate, w_up, w_down, final_norm))
    TA = N + CTX
    NQ = N // 4
    cs = _rope_tables(N)
    wm = _win_masks()
    identf = np.eye(128, dtype=np.float32)
    identb = np.eye(128, dtype=np.float32).astype(ml_dtypes.bfloat16)
    pi = np.arange(128)
    segm = (pi[:, None] // 32 == pi[None, :] // 32).astype(np.float32)
    trix = (pi[:, None] < pi[None, :]).astype(np.float32)
    e4 = np.zeros((128, 4), np.float32)
    e4[[0, 32, 64, 96], [0, 1, 2, 3]] = 1.0
    selh = ((pi[:, None] >= 64) & (pi[None, :] >= 64)).astype(np.float32)
    o = dict(qa=0, ka=256, va=512, qb=768, kb=1024, vb=1152, qs=1280, ks=1536, vs=1664, su=1792, sv=2048)
    maps = []
    for core in range(N_CORES):
        b, kk = divmod(core, 4)
        kv = kk // 2
        r64 = np.arange(64)
        qa = o["qa"] + kk * 64 + r64; ka = o["ka"] + kk * 64 + r64; va = o["va"] + kk * 64 + r64
        qb = o["qb"] + kk * 64 + r64; kb = o["kb"] + kv * 64 + r64; vb = o["vb"] + kv * 64 + r64
        qs = o["qs"] + kk * 64 + r64; ks = o["ks"] + kv * 64 + r64; vs = o["vs"] + kv * 64 + r64
        su = o["su"] + kk * 64 + r64
        pad = None
        w1 = np.zeros((L, D, 768), np.float32)
        for j, cols in enumerate((qa, qb, ka, kb, qs, su, ks, pad, qs[_PARTNER], qb[_PARTNER], ks[_PARTNER], kb[_PARTNER])):
            if cols is not None:
                w1[:, :, j * 64:(j + 1) * 64] = w_in[:, :, cols]
        grp = np.concatenate([np.arange(kk * 64, kk * 64 + 64), np.delete(np.arange(256), np.arange(kk * 64, kk * 64 + 64))])
        w2 = np.concatenate([w_in[:, :, va], w_in[:, :, vb], w_in[:, :, vs], w_in[:, :, o["sv"] + grp]], axis=2)
        gqk = np.stack([np.tile(q_norm, (1, 2)), np.tile(q_norm[:, _PARTNER], (1, 2)),
                        np.tile(k_norm, (1, 2)), np.tile(k_norm[:, _PARTNER], (1, 2))], axis=2)
        eperm = np.concatenate([np.arange(4 * kk, 4 * kk + 4), np.delete(np.arange(16), np.arange(4 * kk, 4 * kk + 4))])
        onrm = np.stack([np.tile(out_norm[:, m * 256 + kk * 64:m * 256 + kk * 64 + 64], (1, 2)) for m in range(4)], axis=2)
        m = dict(
            x=np.concatenate([x[b], ctx[b]], axis=0),
            cv=np.ascontiguousarray(np.stack([c[b].reshape(DC, 128).T, c_ctx.reshape(DC, 128).T], axis=2)),
            wmod=np.ascontiguousarray(w_mod[:, :, kk * 1536:(kk + 1) * 1536]), bmod=b_mod, nmix=norm_mix, nffn=norm_ffn,
            fnorm=final_norm, w1=w1, w2=np.ascontiguousarray(w2), cs=cs, gqk=np.ascontiguousarray(gqk),
            sgn=np.ascontiguousarray(sgu_norm[:, kk * 64:(kk + 1) * 64]),
            ws=np.ascontiguousarray(np.transpose(w_sgu[:, kk], (0, 2, 1))), bs=np.ascontiguousarray(b_sgu[:, kk]),
            nab=np.stack([_na_bias(rpb[l, kk], N) for l in range(L)]), wm=wm,
            snk=np.ascontiguousarray(np.broadcast_to(sink[:, kk][:, None, None], (L, 128, 1))), onrm=np.ascontiguousarray(onrm),
            wo=np.ascontiguousarray(np.stack([w_out[:, mm * 256 + kk * 64:mm * 256 + kk * 64 + 64, :] for mm in range(4)], axis=1)),
            wr=np.ascontiguousarray(w_router[:, :, eperm]),
            wg=np.ascontiguousarray(w_gate[:, 4 * kk + 2 * b:4 * kk + 2 * b + 2]).reshape(L, 2, -1),
            wu=np.ascontiguousarray(w_up[:, 4 * kk + 2 * b:4 * kk + 2 * b + 2]).reshape(L, 2, -1),
            wd=np.ascontiguousarray(w_down[:, 4 * kk + 2 * b:4 * kk + 2 * b + 2]).reshape(L, 2, -1),
            identf=identf, identb=identb, segm=segm, trix=trix, e4=e4, selh=selh,
            oidx=np.ascontiguousarray((kk * NQ + np.arange(NQ)).reshape(NQ // 128, 128).T.astype(np.uint32)),
        )
        maps.append(m)
    return maps


_NC_CACHE = {}


def run(N, L, FF, inputs, dbg=False):
    key = (N, L, FF, dbg)
    if key not in _NC_CACHE:
        _NC_CACHE[key] = build(N, L, FF, dbg)
    nc = _NC_CACHE[key]
    maps = prep_inputs(N, L, FF, **inputs)
    res = run_bass_kernel_spmd(nc, maps, core_ids=list(range(N_CORES)))
    run.last = res
    NQ = N // 4
    outp = np.zeros((2, N, D), np.float32)
    for core in range(N_CORES):
        b, kk = divmod(core, 4)
        outp[b, kk * NQ:(kk + 1) * NQ] = res.results[core]["out"]
    return outp


def kernel(x, c, ctx, c_ctx, w_mod, b_mod, norm_mix, norm_ffn, w_in, rpb, q_norm, k_norm, sink,
           sgu_norm, w_sgu, b_sgu, out_norm, w_out, w_router, w_gate, w_up, w_down, final_norm):
    inputs = dict(x=x, c=c, ctx=ctx, c_ctx=c_ctx, w_mod=w_mod, b_mod=b_mod, norm_mix=norm_mix, norm_ffn=norm_ffn,
                  w_in=w_in, rpb=rpb, q_norm=q_norm, k_norm=k_norm, sink=sink, sgu_norm=sgu_norm, w_sgu=w_sgu,
                  b_sgu=b_sgu, out_norm=out_norm, w_out=w_out, w_router=w_router, w_gate=w_gate, w_up=w_up,
                  w_down=w_down, final_norm=final_norm)
    N = np.asarray(x).shape[1]
    L = np.asarray(w_mod).shape[0]
    FF = np.asarray(w_gate).shape[3]
    return run(N, L, FF, inputs)
```

```python
from contextlib import ExitStack

import numpy as np
import ml_dtypes
import concourse.bass as bass
import concourse.mybir as mybir
from concourse.bass_utils import run_bass_kernel_spmd

F32 = mybir.dt.float32
BF16 = mybir.dt.bfloat16
U32 = mybir.dt.uint32
AF = mybir.ActivationFunctionType
ALU = mybir.AluOpType
AX = mybir.AxisListType

D = 1024
DC = 8
CTX = 256
GRID_W = 64
EPS = 1e-6
NEG = -30000.0
BIG = 1.0e6
N_CORES = 8


class Buf:
    def __init__(self, name, shared=False):
        self.name = name
        self.shared = shared
        self.w = {}
        self.r = {}


class T:
    def __init__(self, t, name, shared=False):
        self.t = t
        self.b = Buf(name, shared)

    def __getitem__(self, idx):
        return self.t[idx]


class Cnt:
    def __init__(self, sem, step):
        self.sem = sem
        self.step = step
        self.n = 0


def _bufs(xs):
    return [x.b if isinstance(x, T) else x for x in xs]


class K:
    ENG = ("pe", "act", "dve", "pool", "sp")

    def __init__(self, nc, st):
        self.nc = nc
        self.st = st
        self.prog = {e: [] for e in self.ENG}
        self.q = {}
        for e in ("pe", "act", "dve", "pool"):
            self.q[e] = Cnt(st.enter_context(nc.semaphore("c_" + e)), 1)
        self.seen = {}
        self.ninst = 0

    def dmaq(self, name, step=16):
        self.q[name] = Cnt(self.st.enter_context(self.nc.semaphore("d_" + name)), step)
        return name

    def sb(self, name, shape, dt, st=None):
        self.uid = getattr(self, "uid", 0) + 1
        name = f"s{self.uid}_{name}"
        return T((st or self.st).enter_context(self.nc.sbuf_tensor(name, shape, dt)), name)

    def ps(self, name, shape, dt=F32, st=None):
        self.uid = getattr(self, "uid", 0) + 1
        name = f"p{self.uid}_{name}"
        return T((st or self.st).enter_context(self.nc.psum_tensor(name, shape, dt)), name)

    def _deps(self, r, w):
        deps = {}

        def add(m):
            for qn, s in m.items():
                if s > deps.get(qn, 0):
                    deps[qn] = s
        for b in r:
            add(b.w)
        for b in w:
            add(b.r)
            if not b.shared:
                add(b.w)
        return deps

    def _wait(self, eng, deps):
        for qn, v in deps.items():
            if qn == "pe" and eng == "pe":
                continue
            q = self.q[qn]
            if q.step == 16:
                v = q.n
            if self.seen.get((eng, qn), 0) >= v:
                continue
            self.seen[(eng, qn)] = v
            self.prog[eng].append(lambda E, sem=q.sem, val=v * q.step: E.wait_ge(sem, val))

    def _mark(self, me, r, w):
        for b in r:
            if me[1] > b.r.get(me[0], 0):
                b.r[me[0]] = me[1]
        for b in w:
            if b.shared:
                if me[1] > b.w.get(me[0], 0):
                    b.w[me[0]] = me[1]
            else:
                b.w = {me[0]: me[1]}
                b.r = {}

    def op(self, eng, fn, r=(), w=()):
        r, w = _bufs(r), _bufs(w)
        self._wait(eng, self._deps(r, w))
        q = self.q[eng]
        q.n += 1
        self.ninst += 1
        self.prog[eng].append(lambda E, sem=q.sem: fn(E).then_inc(sem, 1))
        self._mark((eng, q.n), r, w)

    def dma(self, eng, qn, out, in_, r=(), w=()):
        self.dmaop(eng, qn, lambda E: E.dma_start(out=out, in_=in_), r, w)

    def dmaop(self, eng, qn, fn, r=(), w=()):
        r, w = _bufs(r), _bufs(w)
        self._wait(eng, self._deps(r, w))
        q = self.q[qn]
        q.n += 1
        self.ninst += 1
        self.prog[eng].append(lambda E, sem=q.sem, step=q.step: fn(E).then_inc(sem, step))
        self._mark((qn, q.n), r, w)

    def ring(self, name, n, step=16):
        self.rings = getattr(self, "rings", {})
        self.rings[name] = [self.dmaq(f"{name}{i}", step) for i in range(n)]
        self.ringpos = getattr(self, "ringpos", {})
        self.ringpos[name] = 0

    def rdma(self, eng, ring, fn, r=(), w=(), slot=None, selfwait=True):
        names = self.rings[ring]
        if slot is None:
            slot = self.ringpos[ring]
            self.ringpos[ring] += 1
        qn = names[slot % len(names)]
        q = self.q[qn]
        r, w = _bufs(r), _bufs(w)
        deps = self._deps(r, w)
        if q.n and selfwait:
            deps[qn] = q.n
        self._wait(eng, deps)
        q.n += 1
        self.ninst += 1
        self.prog[eng].append(lambda E, sem=q.sem, step=q.step: fn(E).then_inc(sem, step))
        self._mark((qn, q.n), r, w)

    def pdma(self, ring, fn, r=(), w=(), slot=None, selfwait=True):
        self.rdma("pool", ring, fn, r, w, slot, selfwait)

    def barrier(self):
        allq = {qn: q.n for qn, q in self.q.items() if q.n}
        for eng in self.ENG:
            self._wait(eng, dict(allq))

    def preg(self, E, val):
        self._regs = getattr(self, "_regs", {})
        if val not in self._regs:
            self._regs[val] = E.to_reg(val)
        return self._regs[val]

    def drain(self, eng, qns):
        for qn in qns:
            q = self.q[qn]
            if q.n:
                self.prog[eng].append(lambda E, sem=q.sem, val=q.n * q.step: E.wait_ge(sem, val))

    def emit(self):
        with self.nc.Block() as block:
            @block.tensor
            def _(E):
                for f in self.prog["pe"]:
                    f(E)

            @block.scalar
            def _(E):
                for f in self.prog["act"]:
                    f(E)

            @block.vector
            def _(E):
                for f in self.prog["dve"]:
                    f(E)

            @block.gpsimd
            def _(E):
                for f in self.prog["pool"]:
                    f(E)

            @block.sync
            def _(E):
                for f in self.prog["sp"]:
                    f(E)


def build(N, L, FF, dbg=False):
    TA = N + CTX
    NTT = TA // 128
    NLT = N // 128
    NQ = N // 4
    ROWS = N // GRID_W
    CAP = 2 * N // 16
    CCAP = 2 * CTX // 16
    NS = CAP + 128
    NST = NS // 128
    FC = FF // 128
    SEGW = N // 32
    nc = bass.Bass("TRN2", target_bir_lowering=False)

    def din(name, shape, dt=F32):
        return T(nc.dram_tensor(name, list(shape), dt, kind="ExternalInput").ap(), name, shared=True)

    def dsc(name, shape, dt=F32):
        return T(nc.dram_tensor(name, list(shape), dt).ap(), name, shared=True)

    I = dict(
        x=din("x", [TA, D]), cv=din("cv", [128, DC, 2]), wmod=din("wmod", [L, D, 1536]),
        bmod=din("bmod", [L, 6 * D]), nmix=din("nmix", [L, D]), nffn=din("nffn", [L, D]),
        fnorm=din("fnorm", [D]), w1=din("w1", [L, D, 768]), w2=din("w2", [L, D, 448]),
        cs=din("cs", [2, 64, TA]), gqk=din("gqk", [L, 128, 4]), sgn=din("sgn", [L, 64]),
        ws=din("ws", [L, 128, 128]), bs=din("bs", [L, 128]), nab=din("nab", [L, 3, 8, 128, 512]),
        wm=din("wm", [6, 128, 512]), snk=din("snk", [L, 128, 1]), onrm=din("onrm", [L, 128, 4]),
        wo=din("wo", [L, 4, 64, D]), wr=din("wr", [L, D, 16]),
        wg=din("wg", [L, 2, D * FF]), wu=din("wu", [L, 2, D * FF]), wd=din("wd", [L, 2, FF * D]),
        identf=din("identf", [128, 128]), identb=din("identb", [128, 128], BF16),
        segm=din("segm", [128, 128]), trix=din("trix", [128, 128]), e4=din("e4", [128, 4]),
        selh=din("selh", [128, 128]), oidx=din("oidx", [128, NQ // 128], U32),
    )
    out = T(nc.dram_tensor("out", [NQ, D], F32, kind="ExternalOutput").ap(), "out", shared=True)
    X = dsc("X", [TA, D])
    MODP = dsc("MODP", [2, 1536]); MODG = dsc("MODG", [L, 4, 2, 1536])
    FM = dsc("FM", [4, 128, TA], BF16); VT = dsc("VT", [TA, 192], BF16); VN = dsc("VN", [TA, 64], BF16)
    YT = dsc("YT", [4, 64, TA], BF16)
    SSQ = dsc("SSQ", [TA, 4]); SSQR = dsc("SSQR", [TA, 4])
    U = dsc("U", [TA, D]); UR = dsc("UR", [TA, D])
    H2 = dsc("H2", [TA, D], BF16); AFF = dsc("AFF", [TA, 16]); AFFT = dsc("AFFT", [16, TA])
    XE = [dsc(f"XE{e}", [NS, D], BF16) for e in range(4)]; YE = [dsc(f"YE{e}", [NS, D]) for e in range(4)]

    CH = 512 * 1024
    NCE = D * FF // CH
    BNC = dsc("BNC", [2, 128, CH // 128])
    GB = {n_: dsc("GB" + n_, [L, 2 * NCE, 2, CH]) for n_ in ("wg", "wu", "wd")}
    with ExitStack() as st:
        k = K(nc, st)
        k.ring("ld", 1); k.ring("st", 1)
        k.ring("wt", 4); k.ring("cc", 2, 1); k.ring("gs", 8)

        def ld(o, i, r, w):
            k.rdma("sp", "ld", lambda E: E.dma_start(out=o, in_=i), r, w)

        def sto(o, i, r, w):
            k.rdma("sp", "st", lambda E: E.dma_start(out=o, in_=i), r, w)

        def ldw(o, i, r, w):
            k.pdma("wt", lambda E: E.dma_start(out=o, in_=i), r, w)

        def coll(kind, op, groups, src, dst, r, w):
            k.pdma("cc", lambda E: E.collective_compute(
                kind, op, replica_groups=groups, ins=[src.opt()], outs=[dst.opt()]), r, w)

        G4 = [[0, 1, 2, 3], [4, 5, 6, 7]]

        def allreduce_rows(src, dst, rows, width):
            step = max(128, (1 << 20) // width)
            for r0 in range(0, rows, step):
                r1 = min(rows, r0 + step)
                coll("AllReduce", ALU.add, G4, src[r0:r1, :], dst[r0:r1, :], [src], [dst])

        identf = k.sb("identf", [128, 128], F32); identb = k.sb("identb", [128, 128], BF16)
        onesf = k.sb("onesf", [128, 128], F32); segm = k.sb("segm", [128, 128], F32)
        trix = k.sb("trix", [128, 128], F32); e4 = k.sb("e4", [128, 4], F32)
        selh = k.sb("selh", [128, 128], F32); epsc = k.sb("epsc", [128, 1], F32)
        for t_, n_ in ((identf, "identf"), (identb, "identb"), (segm, "segm"), (trix, "trix"),
                       (e4, "e4"), (selh, "selh")):
            ld(t_[:], I[n_][:], [I[n_]], [t_])
        k.op("dve", lambda E: E.memset(onesf[:], 1.0), w=[onesf])
        k.op("dve", lambda E: E.memset(epsc[:], EPS), w=[epsc])
        cvt = k.sb("cvt", [128, DC, 2], F32); scv = k.sb("scv", [128, DC, 2], F32)
        ld(cvt[:], I["cv"][:], [I["cv"]], [cvt])
        k.op("act", lambda E: E.activation(out=scv[:], in_=cvt[:], func=AF.Silu), r=[cvt], w=[scv])
        for t in range(NTT):
            sto(X[t * 128:(t + 1) * 128, :], I["x"][t * 128:(t + 1) * 128, :], [I["x"]], [X])

        G2 = [[0, 4], [1, 5], [2, 6], [3, 7]]
        ib = 0
        for l_ in range(L):
            for n_ in ("wg", "wu", "wd"):
                for j_ in range(2):
                    for q_ in range(NCE):
                        sto(BNC[ib % 2], I[n_][l_, j_, q_ * CH:(q_ + 1) * CH].rearrange("(p f) -> p f", p=128), [I[n_]], [BNC])
                        coll("AllGather", ALU.bypass, G2, BNC[ib % 2], GB[n_][l_, j_ * NCE + q_].rearrange("r (p f) -> (r p) f", p=128), [BNC], [GB[n_]])
                        ib += 1

        def wsrc(n_, l_, e_, q_, width):
            r_, j_ = divmod(e_, 2)
            return GB[n_][l_, j_ * NCE + q_, r_, :].rearrange("(row f) -> row f", f=width)

        def rstd_col(ssc, dst, inv_n):
            k.op("act", lambda E: E.activation(out=dst[:], in_=ssc[:], func=AF.Ln, bias=epsc[:], scale=inv_n),
                 r=[ssc, epsc], w=[dst])
            k.op("act", lambda E: E.activation(out=dst[:], in_=dst[:], func=AF.Exp, scale=-0.5), r=[dst], w=[dst])

        def mod_phase(l, ps):
            k.barrier()
            with ExitStack() as s2:
                wb = k.sb("modw", [128, DC, 512], F32, s2); row = k.sb("modrow", [2, 512], F32, s2)
                wv = I["wmod"][l].rearrange("(c p) f -> p c f", p=128)
                for blk in range(3):
                    ld(wb[:], wv[:, :, blk * 512:(blk + 1) * 512], [I["wmod"]], [wb])
                    for c in range(DC):
                        k.op("pe", lambda E, c=c: E.matmul(ps[0:2, 0:512], scv[:, c, :], wb[:, c, :],
                                                         start=(c == 0), stop=(c == DC - 1)), r=[scv, wb], w=[ps])
                    k.op("act", lambda E: E.copy(row[:], ps[0:2, 0:512]), r=[ps], w=[row])
                    sto(MODP[:, blk * 512:(blk + 1) * 512], row[:], [row], [MODP])
                coll("AllGather", ALU.bypass, G4, MODP[:, :], MODG[l].rearrange("r a f -> (r a) f"), [MODP], [MODG])

        def mod_tile(l, dst, tmp, j, which, plus_one=False, mul=None):
            for r_ in range(4):
                lo = max(j * D, r_ * 1536); hi = min((j + 1) * D, (r_ + 1) * 1536)
                if lo < hi:
                    ld(dst[:, lo - j * D:hi - j * D], MODG[l, r_, which, lo - r_ * 1536:hi - r_ * 1536].partition_broadcast(128),
                       [MODG], [dst])
            ld(tmp[:], I["bmod"][l, j * D:(j + 1) * D].partition_broadcast(128), [I["bmod"]], [tmp])
            k.op("dve", lambda E: E.tensor_tensor(out=dst[:], in0=dst[:], in1=tmp[:], op=ALU.add), r=[dst, tmp], w=[dst])
            if plus_one:
                k.op("dve", lambda E: E.tensor_scalar(out=dst[:], in0=dst[:], scalar1=1.0, scalar2=None, op0=ALU.add),
                     r=[dst], w=[dst])
            if mul is not None:
                k.op("dve", lambda E: E.tensor_tensor(out=dst[:], in0=dst[:], in1=mul[:], op=ALU.mult), r=[dst, mul], w=[dst])

        def norm_tiles(l, pend_gate_j, nrm_in, jsh, jsc, body, s2, tag):
            gn = k.sb(tag + "gn", [128, D], F32, s2); tmp = k.sb(tag + "tmp", [128, D], F32, s2)
            ld(gn[:], nrm_in[l].partition_broadcast(128), [nrm_in], [gn])
            G = [k.sb(f"{tag}G{w}", [128, D], F32, s2) for w in range(2)]
            S = [k.sb(f"{tag}S{w}", [128, D], F32, s2) for w in range(2)]
            for w in range(2):
                mod_tile(l, G[w], tmp, jsc, w, plus_one=True, mul=gn)
                mod_tile(l, S[w], tmp, jsh, w)
            PG = None
            if pend_gate_j is not None:
                PG = [k.sb(f"{tag}PG{w}", [128, D], F32, s2) for w in range(2)]
                for w in range(2):
                    mod_tile(pend_gate_j[0], PG[w], tmp, pend_gate_j[1], w)
            bufs = [tuple(k.sb(f"{tag}{n_}{i_}", [128, D if n_ in ("xt", "ut", "jk", "hf") else 1], F32, s2)
                          for n_ in ("xt", "ut", "jk", "hf", "ss", "rs")) for i_ in range(2)]

            def one_tile(t, xt, ut, jk, hf, ss, rs):
                which = 0 if t < NLT else 1
                rows = slice(t * 128, (t + 1) * 128)
                ld(xt[:], X[rows, :], [X], [xt])
                if PG is not None:
                    ld(ut[:], UR[rows, :], [UR], [ut])
                    k.op("dve", lambda E: E.tensor_tensor(out=ut[:], in0=ut[:], in1=PG[which][:], op=ALU.mult),
                         r=[ut, PG[which]], w=[ut])
                    k.op("dve", lambda E: E.tensor_tensor(out=xt[:], in0=xt[:], in1=ut[:], op=ALU.add), r=[xt, ut], w=[xt])
                    sto(X[rows, :], xt[:], [xt], [X])
                k.op("act", lambda E: E.activation(out=jk[:], in_=xt[:], func=AF.Square, accum_out=ss[:]), r=[xt], w=[jk, ss])
                rstd_col(ss, rs, 1.0 / D)
                k.op("dve", lambda E: E.scalar_tensor_tensor(out=hf[:], in0=xt[:], scalar=rs[:], in1=G[which][:],
                                                             op0=ALU.mult, op1=ALU.mult), r=[xt, rs, G[which]], w=[hf])
                k.op("dve", lambda E: E.tensor_tensor(out=hf[:], in0=hf[:], in1=S[which][:], op=ALU.add),
                     r=[hf, S[which]], w=[hf])
                body(t, which, hf)

            for t in range(NTT):
                one_tile(t, *bufs[t % 2])

        def p1_phase(l, pend, P):
            k.barrier()
            with ExitStack() as s2:
                w1 = k.sb("w1", [128, DC, 768], BF16, s2); w2 = k.sb("w2", [128, DC, 448], BF16, s2)
                ldw(w1[:], I["w1"][l].rearrange("(c p) f -> p c f", p=128), [I["w1"]], [w1])
                ldw(w2[:], I["w2"][l].rearrange("(c p) f -> p c f", p=128), [I["w2"]], [w2])
                gqk = k.sb("gqk", [128, 4], F32, s2); sgn = k.sb("sgn", [128, 64], F32, s2)
                ld(gqk[:], I["gqk"][l], [I["gqk"]], [gqk])
                ld(sgn[:], I["sgn"][l].partition_broadcast(128), [I["sgn"]], [sgn])
                hb = k.sb("hb", [128, D], BF16, s2); hT = k.sb("hT", [128, DC, 512], BF16, s2)
                vt = k.sb("vtile", [128, 192], BF16, s2); gsv = k.sb("gsv", [128, 256], F32, s2)
                jk2 = k.sb("jk2", [128, 256], F32, s2); ssv = k.sb("ssv", [128, 1], F32, s2)
                rsv = k.sb("rsv", [128, 1], F32, s2); vn = k.sb("vn", [128, 64], BF16, s2)
                cos = k.sb("cos", [128, 512], F32, s2); sin = k.sb("sin", [128, 512], F32, s2)
                pp = [k.sb(f"pp{i}", [128, 512], F32, s2) for i in range(2)]
                t1 = k.sb("t1", [128, 512], F32, s2); t2 = k.sb("t2", [128, 512], F32, s2)
                sq = k.sb("sqh", [128, 512], F32, s2); rsh = k.sb("rsh", [128, 512], F32, s2)
                fmo = k.sb("fmo", [128, 512], BF16, s2)
                k.op("dve", lambda E: E.memset(sq[:], 0.0), w=[sq])
                pstr, pstok, psfm, pssq = P["a"], P["b"], P["c"], P["d"]
                pstr = P["t"]; trv = pstr.t

                def fm_tile(col0, W):
                    ld(cos[0:64, 0:W], I["cs"][0, :, col0:col0 + W], [I["cs"]], [cos])
                    ld(cos[64:128, 0:W], I["cs"][0, :, col0:col0 + W], [I["cs"]], [cos])
                    ld(sin[0:64, 0:W], I["cs"][1, :, col0:col0 + W], [I["cs"]], [sin])
                    ld(sin[64:128, 0:W], I["cs"][1, :, col0:col0 + W], [I["cs"]], [sin])

                    def proj(oc):
                        for c in range(DC):
                            k.op("pe", lambda E, c=c: E.matmul(psfm[:, 0:W], w1[:, c, oc * 128:(oc + 1) * 128], hT[:, c, 0:W],
                                                             start=(c == 0), stop=(c == DC - 1)), r=[w1, hT], w=[psfm])
                    for i, oc in enumerate((4, 5)):
                        proj(oc)
                        k.op("act", lambda E, i=i: E.copy(pp[i][:, 0:W], psfm[:, 0:W]), r=[psfm], w=[pp[i]])

                    def rope(h, part, gm, gp, norm):
                        sl = slice(h * 64, h * 64 + 64)
                        if norm:
                            k.op("act", lambda E: E.activation(out=sq[sl, 0:W], in_=psfm[sl, 0:W], func=AF.Square), r=[psfm], w=[sq])
                            k.op("pe", lambda E: E.matmul(pssq[:, 0:W], selh[:], sq[:, 0:W], start=True, stop=True), r=[selh, sq], w=[pssq])
                            k.op("act", lambda E: E.activation(out=rsh[sl, 0:W], in_=pssq[sl, 0:W], func=AF.Ln, bias=epsc[sl, :], scale=1.0 / 64),
                                 r=[pssq, epsc], w=[rsh])
                            k.op("act", lambda E: E.activation(out=rsh[sl, 0:W], in_=rsh[sl, 0:W], func=AF.Exp, scale=-0.5), r=[rsh], w=[rsh])
                            k.op("dve", lambda E: E.scalar_tensor_tensor(out=t1[sl, 0:W], in0=psfm[sl, 0:W], scalar=gqk[sl, gm:gm + 1],
                                                                         in1=cos[sl, 0:W], op0=ALU.mult, op1=ALU.mult), r=[psfm, gqk, cos], w=[t1])
                            k.op("dve", lambda E: E.scalar_tensor_tensor(out=t2[sl, 0:W], in0=part[sl, 0:W], scalar=gqk[sl, gp:gp + 1],
                                                                         in1=sin[sl, 0:W], op0=ALU.mult, op1=ALU.mult), r=[part, gqk, sin], w=[t2])
                            k.op("dve", lambda E: E.tensor_tensor(out=t1[sl, 0:W], in0=t1[sl, 0:W], in1=t2[sl, 0:W], op=ALU.add), r=[t1, t2], w=[t1])
                            k.op("dve", lambda E: E.tensor_tensor(out=fmo[sl, 0:W], in0=t1[sl, 0:W], in1=rsh[sl, 0:W], op=ALU.mult), r=[t1, rsh], w=[fmo])
                        else:
                            k.op("dve", lambda E: E.tensor_tensor(out=t1[sl, 0:W], in0=psfm[sl, 0:W], in1=cos[sl, 0:W], op=ALU.mult), r=[psfm, cos], w=[t1])
                            k.op("dve", lambda E: E.tensor_tensor(out=t2[sl, 0:W], in0=part[sl, 0:W], in1=sin[sl, 0:W], op=ALU.mult), r=[part, sin], w=[t2])
                            k.op("dve", lambda E: E.tensor_tensor(out=fmo[sl, 0:W], in0=t1[sl, 0:W], in1=t2[sl, 0:W], op=ALU.add), r=[t1, t2], w=[fmo])

                    for oc in range(4):
                        proj(oc)
                        if oc == 0:
                            k.op("act", lambda E: E.copy(fmo[0:64, 0:W], psfm[0:64, 0:W]), r=[psfm], w=[fmo])
                            rope(1, pp[0], 0, 1, True)
                        elif oc == 1:
                            k.op("act", lambda E: E.copy(fmo[0:64, 0:W], psfm[0:64, 0:W]), r=[psfm], w=[fmo])
                            rope(1, pp[1], 2, 3, True)
                        elif oc == 2:
                            rope(0, pp[0], 0, 0, False)
                            k.op("act", lambda E: E.activation(out=fmo[64:128, 0:W], in_=psfm[64:128, 0:W], func=AF.Gelu), r=[psfm], w=[fmo])
                        else:
                            rope(0, pp[1], 0, 0, False)
                            k.op("act", lambda E: E.copy(fmo[64:128, 0:W], psfm[64:128, 0:W]), r=[psfm], w=[fmo])
                        sto(FM[oc, :, col0:col0 + W], fmo[:, 0:W], [fmo], [FM])

                def body(t, which, hf):
                    sub = t % 4 if t < NLT else (t - NLT)
                    k.op("act", lambda E: E.copy(hb[:], hf[:]), r=[hf], w=[hb])
                    for c in range(DC):
                        k.op("pe", lambda E, c=c: E.transpose(trv[:, c * 128:(c + 1) * 128], hb[:, c * 128:(c + 1) * 128], identb[:]),
                             r=[hb, identb], w=[pstr])
                    k.op("dve", lambda E: E.tensor_copy(hT[:, :, sub * 128:(sub + 1) * 128],
                                                        trv[:, :].rearrange("p (c t) -> p c t", c=DC)), r=[pstr], w=[hT])
                    for c in range(DC):
                        k.op("pe", lambda E, c=c: E.matmul(pstok[:, 0:448], hT[:, c, sub * 128:(sub + 1) * 128], w2[:, c, :],
                                                         start=(c == 0), stop=(c == DC - 1)), r=[hT, w2], w=[pstok])
                    rows = slice(t * 128, (t + 1) * 128)
                    k.op("act", lambda E: E.copy(vt[:], pstok[:, 0:192]), r=[pstok], w=[vt])
                    sto(VT[rows, :], vt[:], [vt], [VT])
                    k.op("act", lambda E: E.activation(out=gsv[:], in_=pstok[:, 192:448], func=AF.Gelu), r=[pstok], w=[gsv])
                    k.op("act", lambda E: E.activation(out=jk2[:], in_=gsv[:], func=AF.Square, accum_out=ssv[:]), r=[gsv], w=[jk2, ssv])
                    rstd_col(ssv, rsv, 1.0 / 256)
                    k.op("dve", lambda E: E.scalar_tensor_tensor(out=vn[:], in0=gsv[:, 0:64], scalar=rsv[:], in1=sgn[:],
                                                                 op0=ALU.mult, op1=ALU.mult), r=[gsv, rsv, sgn], w=[vn])
                    sto(VN[rows, :], vn[:], [vn], [VN])
                    if t < NLT and sub == 3:
                        fm_tile((t - 3) * 128, 512)
                    elif t == NTT - 1:
                        fm_tile(N, CTX)

                norm_tiles(l, pend, I["nmix"], 0, 1, body, s2, "p1")

        def attn_phase(l, P):
            k.barrier()
            with ExitStack() as s2:
                qres = k.sb("qres", [128, TA], BF16, s2); kres = k.sb("kres", [128, TA], BF16, s2)
                vaug = k.sb("vaug", [128, NTT, 128], BF16, s2)
                ssqr = k.sb("ssqres", [128, NTT, 4], F32, s2)
                pt = [k.sb(f"pt{i}", [128, 512], BF16, s2) for i in range(3)]
                bt = [k.sb(f"bt{i}", [128, 512], F32, s2) for i in range(2)]
                sbt = k.sb("sbt", [128, 512], F32, s2)
                rr = k.sb("rr", [128, 512], F32, s2); rs0 = k.sb("rs0", [128, 512], F32, s2)
                y32 = k.sb("y32", [128, 512], F32, s2); ysq = k.sb("ysq", [128, 512], F32, s2)
                yb = k.sb("yb", [128, 512], BF16, s2)
                esk = k.sb("esk", [128, 1], F32, s2); skt = k.sb("skt", [128, 1], F32, s2)
                vna = k.sb("vna", [128, 4, 128], BF16, s2); wsb = k.sb("wsb", [128, 128], BF16, s2)
                bst = k.sb("bst", [128, 128], F32, s2)
                pss = [P["a"], P["b"]]; pso, psr, psq = P["c"], P["d"], P["e"]
                k.op("dve", lambda E: E.memset(vaug[:], 1.0), w=[vaug])
                k.op("dve", lambda E: E.memset(ssqr[:], 0.0), w=[ssqr])
                k.op("dve", lambda E: E.memset(vna[:], 0.0), w=[vna])
                ld(skt[:], I["snk"][l], [I["snk"]], [skt])
                k.op("act", lambda E: E.activation(out=esk[:], in_=skt[:], func=AF.Exp), r=[skt], w=[esk])
                cnt = [0, 0]

                def load_v(col):
                    for t0 in range(0, NTT, 16):
                        t1_ = min(NTT, t0 + 16)
                        ld(vaug[:, t0:t1_, 0:64], VT[t0 * 128:t1_ * 128, col * 64:(col + 1) * 64].rearrange("(t p) c -> p t c", p=128),
                           [VT], [vaug])

                def finish(p0, m, col0, W, sink):
                    if sink:
                        k.op("dve", lambda E: E.tensor_scalar(out=rr[64:128, 0:W], in0=pso[64:128, 0:W], scalar1=esk[64:128, :], scalar2=None,
                                                              op0=ALU.add), r=[pso, esk], w=[rr])
                        k.op("dve", lambda E: E.reciprocal(rr[64:128, 0:W], rr[64:128, 0:W]), r=[rr], w=[rr])
                    else:
                        k.op("dve", lambda E: E.reciprocal(rr[64:128, 0:W], pso[64:128, 0:W]), r=[pso], w=[rr])
                    k.op("pe", lambda E: E.matmul(psr[0:64, 0:W], identf[64:128, 64:128], rr[64:128, 0:W], start=True, stop=True),
                         r=[identf, rr], w=[psr])
                    k.op("act", lambda E: E.copy(rs0[0:64, 0:W], psr[0:64, 0:W]), r=[psr], w=[rs0])
                    k.op("dve", lambda E: E.tensor_tensor(out=y32[0:64, 0:W], in0=pso[0:64, 0:W], in1=rs0[0:64, 0:W], op=ALU.mult),
                         r=[pso, rs0], w=[y32])
                    ytail(slice(0, 64), m, col0, W)

                def ytail(sl, m, col0, W):
                    k.op("act", lambda E: E.copy(yb[sl, 0:W], y32[sl, 0:W]), r=[y32], w=[yb])
                    sto(YT[m, :, col0:col0 + W], yb[sl, 0:W], [yb], [YT])
                    k.op("act", lambda E: E.activation(out=ysq[sl, 0:W], in_=y32[sl, 0:W], func=AF.Square), r=[y32], w=[ysq])
                    for s_ in range(W // 128):
                        tt = col0 // 128 + s_
                        k.op("pe", lambda E, s_=s_: E.matmul(psq[:, 0:1], ysq[sl, s_ * 128:(s_ + 1) * 128], onesf[sl, 0:1], start=True, stop=True),
                             r=[ysq, onesf], w=[psq])
                        k.op("dve", lambda E, tt=tt: E.tensor_copy(ssqr[:, tt, m:m + 1], psq[:, 0:1]), r=[psq], w=[ssqr])

                def attn_tile(p0, m, col0, W, chunks, sink):
                    sl = slice(p0, p0 + 64)
                    n = len(chunks)
                    for j, (kt, bias) in enumerate(chunks):
                        ps = pss[cnt[0] % 2]; cnt[0] += 1
                        p = pt[cnt[1] % 3]; cnt[1] += 1
                        k.op("pe", lambda E, ps=ps, kt=kt: E.matmul(ps[:, 0:W], kres[sl, kt * 128:(kt + 1) * 128], qres[sl, col0:col0 + W],
                                                                   start=True, stop=True), r=[kres, qres], w=[ps])
                        if bias is not None:
                            b = bt[j % 2]
                            ld(b[:, 0:W], bias, [I["nab"], I["wm"]], [b])
                            k.op("dve", lambda E, ps=ps, b=b: E.scalar_tensor_tensor(out=sbt[:, 0:W], in0=ps[:, 0:W], scalar=0.125, in1=b[:, 0:W],
                                                                                    op0=ALU.mult, op1=ALU.add), r=[ps, b], w=[sbt])
                            k.op("act", lambda E, p=p: E.activation(out=p[:, 0:W], in_=sbt[:, 0:W], func=AF.Exp), r=[sbt], w=[p])
                        else:
                            k.op("act", lambda E, p=p, ps=ps: E.activation(out=p[:, 0:W], in_=ps[:, 0:W], func=AF.Exp, scale=0.125), r=[ps], w=[p])
                        k.op("pe", lambda E, p=p, kt=kt, j=j: E.matmul(pso[:, 0:W], vaug[:, kt, :], p[:, 0:W], start=(j == 0), stop=(j == n - 1)),
                             r=[vaug, p], w=[pso])
                    finish(p0, m, col0, W, sink)

                ctxc = [(NLT, None), (NLT + 1, None)]
                NQT = N // 512
                ld(qres[:], FM[0], [FM], [qres]); ld(kres[:], FM[1], [FM], [kres])
                load_v(0)
                for t in range(NQT):
                    v = 0 if t == 0 else (2 if t == NQT - 1 else 1)
                    ch = [(4 * t - 2 + i, I["nab"][l, v, i]) for i in range(8) if 0 <= 4 * t - 2 + i < NLT]
                    attn_tile(0, 0, t * 512, 512, ch + ctxc, False)
                attn_tile(0, 0, N, CTX, [(kt, None) for kt, _ in ctxc], False)
                load_v(1)
                allc = [(kt, None) for kt in range(NTT)]
                for t in range(NQT):
                    attn_tile(64, 1, t * 512, 512, allc, False)
                attn_tile(64, 1, N, CTX, ctxc, False)
                ld(qres[:], FM[2], [FM], [qres]); ld(kres[:], FM[3], [FM], [kres])
                load_v(2)
                for t in range(NQT):
                    ch = [(4 * t - 1 + i, I["wm"][i]) for i in range(6) if 0 <= 4 * t - 1 + i < NLT]
                    attn_tile(0, 2, t * 512, 512, ch + ctxc, True)
                attn_tile(0, 2, N, CTX, ctxc, True)
                ld(sbt[:, 0:128], I["ws"][l], [I["ws"]], [sbt])
                k.op("act", lambda E: E.copy(wsb[:], sbt[:, 0:128]), r=[sbt], w=[wsb])
                ld(bst[:], I["bs"][l].partition_broadcast(128), [I["bs"]], [bst])
                for t0 in range(0, NTT, 4):
                    nt_ = min(4, NTT - t0)
                    W = nt_ * 128
                    ld(vna[:, 0:nt_, 64:128], VN[t0 * 128:(t0 + nt_) * 128, :].rearrange("(t p) c -> p t c", p=128), [VN], [vna])
                    for s_ in range(nt_):
                        k.op("pe", lambda E, s_=s_: E.matmul(pso[:, s_ * 128:(s_ + 1) * 128], vna[:, s_, :], wsb[:], start=True, stop=True),
                             r=[vna, wsb], w=[pso])
                    for s_ in range(nt_):
                        k.op("dve", lambda E, s_=s_: E.tensor_tensor(out=y32[64:128, s_ * 128:(s_ + 1) * 128], in0=pso[64:128, s_ * 128:(s_ + 1) * 128],
                                                                    in1=bst[64:128, :], op=ALU.add), r=[pso, bst], w=[y32])
                    k.op("act", lambda E, t0=t0, W=W: E.copy(ysq[64:128, 0:W], qres[64:128, t0 * 128:t0 * 128 + W]), r=[qres], w=[ysq])
                    k.op("dve", lambda E, W=W: E.tensor_tensor(out=y32[64:128, 0:W], in0=y32[64:128, 0:W], in1=ysq[64:128, 0:W], op=ALU.mult),
                         r=[y32, ysq], w=[y32])
                    ytail(slice(64, 128), 3, t0 * 128, W)
                sto(SSQ[:, :].rearrange("(t p) m -> p t m", p=128), ssqr[:], [ssqr], [SSQ])
            coll("AllReduce", ALU.add, G4, SSQ[:, :], SSQR[:, :], [SSQ], [SSQR])

        def merge_phase(l, P):
            k.barrier()
            with ExitStack() as s2:
                wo = k.sb("wo", [128, 4, D], BF16, s2); onr = k.sb("onr", [128, 4], F32, s2)
                for m in range(4):
                    p0 = 64 if m == 3 else 0
                    ldw(wo[p0:p0 + 64, m, :], I["wo"][l, m], [I["wo"]], [wo])
                ld(onr[:], I["onrm"][l], [I["onrm"]], [onr])
                mb = [(k.sb(f"ym{i_}", [128, 4, 128], BF16, s2), k.sb(f"yg{i_}", [128, 4, 128], BF16, s2),
                       k.sb(f"sq4{i_}", [128, 4], F32, s2), k.sb(f"r4{i_}", [128, 4], F32, s2),
                       k.sb(f"mut{i_}", [128, D], F32, s2)) for i_ in range(2)]
                pz = [P["a"], P["b"], P["c"], P["d"]]

                def mtile(t, ym, yg, sq4, r4, ut):
                    cols = slice(t * 128, (t + 1) * 128)
                    for m in range(4):
                        p0 = 64 if m == 3 else 0
                        ld(ym[p0:p0 + 64, m, :], YT[m, :, cols], [YT], [ym])
                    ld(sq4[:], SSQR[cols, :], [SSQR], [sq4])
                    k.op("act", lambda E: E.activation(out=r4[:], in_=sq4[:], func=AF.Ln, bias=epsc[:], scale=1.0 / 256), r=[sq4, epsc], w=[r4])
                    k.op("act", lambda E: E.activation(out=r4[:], in_=r4[:], func=AF.Exp, scale=-0.5), r=[r4], w=[r4])
                    for m in range(4):
                        sl = slice(64, 128) if m == 3 else slice(0, 64)
                        k.op("dve", lambda E, m=m, sl=sl: E.tensor_scalar(out=yg[sl, m, :], in0=ym[sl, m, :], scalar1=onr[sl, m:m + 1], scalar2=None,
                                                                        op0=ALU.mult), r=[ym, onr], w=[yg])
                    for dh in range(2):
                        for m in range(4):
                            sl = slice(64, 128) if m == 3 else slice(0, 64)
                            k.op("pe", lambda E, m=m, sl=sl, dh=dh: E.matmul(pz[m][:, 0:512], yg[sl, m, :], wo[sl, m, dh * 512:(dh + 1) * 512],
                                                                           start=True, stop=True), r=[yg, wo], w=[pz[m]])
                        dsl = slice(dh * 512, (dh + 1) * 512)
                        k.op("dve", lambda E, dsl=dsl: E.tensor_scalar(out=ut[:, dsl], in0=pz[0][:, 0:512], scalar1=r4[:, 0:1], scalar2=None, op0=ALU.mult),
                             r=[pz[0], r4], w=[ut])
                        for m in range(1, 4):
                            k.op("dve", lambda E, m=m, dsl=dsl: E.scalar_tensor_tensor(out=ut[:, dsl], in0=pz[m][:, 0:512], scalar=r4[:, m:m + 1],
                                                                                      in1=ut[:, dsl], op0=ALU.mult, op1=ALU.add), r=[pz[m], r4, ut], w=[ut])
                    sto(U[cols, :], ut[:], [ut], [U])

                for t in range(NTT):
                    mtile(t, *mb[t % 2])
            allreduce_rows(U, UR, TA, D)

        def router_phase(l, P):
            k.barrier()
            with ExitStack() as s2:
                wr = k.sb("wr", [128, DC, 16], F32, s2)
                ld(wr[:], I["wr"][l].rearrange("(c p) e -> p c e", p=128), [I["wr"]], [wr])
                hb = k.sb("h2b", [128, D], BF16, s2); hT = k.sb("h2T", [128, DC, 128], F32, s2)
                ex = k.sb("ex", [128, 16], F32, s2); sm = k.sb("sm", [128, 1], F32, s2)
                af = k.sb("af", [128, 16], F32, s2); aT = k.sb("aT", [16, 128], F32, s2)
                pta, ptb, pl, pa = P["a"], P["b"], P["c"], P["d"]

                def body(t, which, hf):
                    rows = slice(t * 128, (t + 1) * 128)
                    k.op("act", lambda E: E.copy(hb[:], hf[:]), r=[hf], w=[hb])
                    sto(H2[rows, :], hb[:], [hb], [H2])
                    for c in range(DC):
                        pt_ = pta if c < 4 else ptb
                        k.op("pe", lambda E, c=c, pt_=pt_: E.transpose(pt_[:, (c % 4) * 128:(c % 4 + 1) * 128], hf[:, c * 128:(c + 1) * 128], identf[:]),
                             r=[hf, identf], w=[pt_])
                    k.op("dve", lambda E: E.tensor_copy(hT[:, 0:4, :], pta[:, :].rearrange("p (c t) -> p c t", c=4)), r=[pta], w=[hT])
                    k.op("act", lambda E: E.copy(hT[:, 4:8, :], ptb[:, :].rearrange("p (c t) -> p c t", c=4)), r=[ptb], w=[hT])
                    for c in range(DC):
                        k.op("pe", lambda E, c=c: E.matmul(pl[:, 0:16], hT[:, c, :], wr[:, c, :], start=(c == 0), stop=(c == DC - 1)), r=[hT, wr], w=[pl])
                    k.op("act", lambda E: E.activation(out=ex[:], in_=pl[:, 0:16], func=AF.Exp, accum_out=sm[:]), r=[pl], w=[ex, sm])
                    k.op("dve", lambda E: E.reciprocal(sm[:], sm[:]), r=[sm], w=[sm])
                    k.op("dve", lambda E: E.tensor_scalar(out=af[:], in0=ex[:], scalar1=sm[:], scalar2=None, op0=ALU.mult), r=[ex, sm], w=[af])
                    sto(AFF[rows, :], af[:], [af], [AFF])
                    k.op("pe", lambda E: E.transpose(pa[0:16, 0:128], af[:], identf[:]), r=[af, identf], w=[pa])
                    k.op("act", lambda E: E.copy(aT[:], pa[0:16, 0:128]), r=[pa], w=[aT])
                    sto(AFFT[:, rows], aT[:], [aT], [AFFT])

                norm_tiles(l, (l, 2), I["nffn"], 3, 4, body, s2, "p6")

        def moe_phase(l, P):
            k.barrier()
            with ExitStack() as s2:
                slot = k.sb("slot", [128, NTT * 4], U32, s2); gmr = k.sb("gmr", [128, NTT, 4], F32, s2)
                thr = [k.sb(f"thr{w}", [128, 4], F32, s2) for w in range(2)]
                k.barrier()
                with ExitStack() as s3:
                    afl = k.sb("afl", [128, SEGW], F32, s3); afc = k.sb("afc", [128, CTX], F32, s3)
                    jb = k.sb("jb", [128, max(SEGW, CTX)], F32, s3)
                    lo = [k.sb(f"lo{w}", [128, 1], F32, s3) for w in range(2)]
                    mid = k.sb("mid", [128, 1], F32, s3); cn = k.sb("cn", [128, 1], F32, s3)
                    pr = k.sb("pr", [128, 1], F32, s3); tm = k.sb("tm", [128, 4], F32, s3)
                    pc = P["a"]
                    for e_ in range(4):
                        ld(afl[e_ * 32:(e_ + 1) * 32, :], AFFT[e_, 0:N].rearrange("(s n) -> s n", s=32), [AFFT], [afl])
                    k.op("dve", lambda E: E.memset(afc[:], 0.0), w=[afc])
                    ld(afc[0:4, :], AFFT[0:4, N:TA], [AFFT], [afc])
                    for w, (a, width, cap) in enumerate(((afl, SEGW, CAP), (afc, CTX, CCAP))):
                        k.op("dve", lambda E, w=w: E.memset(lo[w][:], 0.0), w=[lo[w]])
                        for it in range(1, 33):
                            wi = 2.0 ** (-it)
                            k.op("dve", lambda E, w=w, wi=wi: E.tensor_scalar(out=mid[:], in0=lo[w][:], scalar1=wi, scalar2=None, op0=ALU.add),
                                 r=[lo[w]], w=[mid])
                            k.op("dve", lambda E, a=a, width=width: E.tensor_scalar(out=jb[:, 0:width], in0=a[:, 0:width], scalar1=mid[:], scalar2=0.0,
                                                                                   op0=ALU.is_ge, op1=ALU.add, accum_out=cn[:]), r=[a, mid], w=[jb, cn])
                            if w == 0:
                                k.op("pe", lambda E: E.matmul(pc[:, 0:1], segm[:], cn[:], start=True, stop=True), r=[segm, cn], w=[pc])
                                src = pc[:, 0:1]; sb_ = pc
                            else:
                                src = cn[:]; sb_ = cn
                            k.op("dve", lambda E, src=src, cap=cap: E.tensor_scalar(out=pr[:], in0=src, scalar1=float(cap), scalar2=None, op0=ALU.is_ge),
                                 r=[sb_], w=[pr])
                            k.op("dve", lambda E, w=w, wi=wi: E.scalar_tensor_tensor(out=lo[w][:], in0=pr[:], scalar=wi, in1=lo[w][:],
                                                                                    op0=ALU.mult, op1=ALU.add), r=[pr, lo[w]], w=[lo[w]])
                    k.op("dve", lambda E: E.tensor_scalar(out=tm[:], in0=e4[:], scalar1=lo[0][:], scalar2=None, op0=ALU.mult), r=[e4, lo[0]], w=[tm])
                    k.op("pe", lambda E: E.matmul(pc[:, 0:4], onesf[:], tm[:], start=True, stop=True), r=[onesf, tm], w=[pc])
                    k.op("act", lambda E: E.copy(thr[0][:], pc[:, 0:4]), r=[pc], w=[thr[0]])
                    k.op("dve", lambda E: E.tensor_scalar(out=tm[0:4, :], in0=identf[0:4, 0:4], scalar1=lo[1][0:4, :], scalar2=None, op0=ALU.mult),
                         r=[identf, lo[1]], w=[tm])
                    k.op("pe", lambda E: E.matmul(pc[:, 0:4], onesf[0:4, :], tm[0:4, :], start=True, stop=True), r=[onesf, tm], w=[pc])
                    k.op("act", lambda E: E.copy(thr[1][:], pc[:, 0:4]), r=[pc], w=[thr[1]])
                k.barrier()
                with ExitStack() as s3:
                    af = k.sb("maf", [128, 16], F32, s3); mk = k.sb("mk", [128, 4], F32, s3)
                    pos = k.sb("pos", [128, 4], F32, s3); car = k.sb("car", [128, 4], F32, s3)
                    sf = k.sb("sf", [128, 4], F32, s3); m2 = k.sb("m2", [128, 4], F32, s3)
                    hr = [k.sb(f"hr{i}", [128, D], BF16, s3) for i in range(2)]
                    pp_, pcs = P["a"], P["b"]
                    k.op("dve", lambda E: E.memset(car[:], 0.0), w=[car])
                    for t in range(NTT):
                        which = 0 if t < NLT else 1
                        rows = slice(t * 128, (t + 1) * 128)
                        h = hr[t % 2]
                        if t == NLT:
                            k.op("dve", lambda E: E.memset(car[:], float(CAP)), w=[car])
                        lim = float(CAP) if which == 0 else float(CAP + CCAP)
                        ld(af[:], AFF[rows, :], [AFF], [af])
                        ld(h[:], H2[rows, :], [H2], [h])
                        k.op("dve", lambda E, w=which: E.tensor_tensor(out=mk[:], in0=af[:, 0:4], in1=thr[w][:], op=ALU.is_ge), r=[af, thr[which]], w=[mk])
                        k.op("pe", lambda E: E.matmul(pp_[:, 0:4], trix[:], mk[:], start=True, stop=True), r=[trix, mk], w=[pp_])
                        k.op("pe", lambda E: E.matmul(pcs[:, 0:4], onesf[:], mk[:], start=True, stop=True), r=[onesf, mk], w=[pcs])
                        k.op("dve", lambda E: E.tensor_tensor(out=pos[:], in0=pp_[:, 0:4], in1=car[:], op=ALU.add), r=[pp_, car], w=[pos])
                        k.op("dve", lambda E: E.tensor_tensor(out=car[:], in0=pcs[:, 0:4], in1=car[:], op=ALU.add), r=[pcs, car], w=[car])
                        k.op("dve", lambda E: E.scalar_tensor_tensor(out=sf[:], in0=pos[:], scalar=-BIG, in1=mk[:], op0=ALU.add, op1=ALU.mult), r=[pos, mk], w=[sf])
                        k.op("dve", lambda E: E.tensor_scalar(out=sf[:], in0=sf[:], scalar1=BIG, scalar2=None, op0=ALU.add), r=[sf], w=[sf])
                        k.op("dve", lambda E, lim=lim: E.tensor_scalar(out=m2[:], in0=sf[:], scalar1=lim, scalar2=None, op0=ALU.is_lt), r=[sf], w=[m2])
                        k.op("dve", lambda E: E.scalar_tensor_tensor(out=sf[:], in0=sf[:], scalar=-BIG, in1=m2[:], op0=ALU.add, op1=ALU.mult), r=[sf, m2], w=[sf])
                        k.op("dve", lambda E: E.tensor_scalar(out=sf[:], in0=sf[:], scalar1=BIG, scalar2=None, op0=ALU.add), r=[sf], w=[sf])
                        k.op("dve", lambda E, t=t: E.tensor_copy(slot[:, t * 4:t * 4 + 4], sf[:]), r=[sf], w=[slot])
                        k.op("dve", lambda E, t=t: E.tensor_tensor(out=gmr[:, t, :], in0=af[:, 0:4], in1=m2[:], op=ALU.mult), r=[af, m2], w=[gmr])
                        for e in range(4):
                            k.pdma("gs", lambda E, e=e, t=t, h=h: E.indirect_dma_start(
                                out=XE[e][:, :], out_offset=bass.IndirectOffsetOnAxis(ap=slot[:, t * 4 + e:t * 4 + e + 1], axis=0), in_=h[:], in_offset=None,
                                bounds_check=k.preg(E, NS - 1), oob_is_err=False), r=[slot, h], w=[XE[e]], slot=(t % 2) * 4 + e, selfwait=False)
                k.barrier()
                with ExitStack() as s3:
                    HT = (NST + 1) // 2
                    xr = k.sb("xr", [128, D], BF16, s3); xeT = k.sb("xeT", [128, DC, HT * 128], BF16, s3)
                    hid = k.sb("hid", [128, FC, HT * 128], BF16, s3)
                    wgs = k.sb("wgs", [128, DC, 512], BF16, s3); wus = k.sb("wus", [128, DC, 512], BF16, s3)
                    wds = k.sb("wds", [128, FC, D], BF16, s3)
                    sg = k.sb("sg", [128, 512], F32, s3); ye = k.sb("ye", [128, D], F32, s3)
                    ptr, pg, pu, py0, py1 = P["a"], P["b"], P["c"], P["d"], P["e"]
                    ptr = P["t"]; trv = ptr.t
                    for e in range(4):
                        for hs in range(2):
                            tiles = list(range(hs * HT, min(NST, (hs + 1) * HT)))
                            if not tiles:
                                continue
                            for i, stl in enumerate(tiles):
                                ld(xr[:], XE[e][stl * 128:(stl + 1) * 128, :], [XE[e]], [xr])
                                for c in range(DC):
                                    k.op("pe", lambda E, c=c: E.transpose(trv[:, c * 128:(c + 1) * 128], xr[:, c * 128:(c + 1) * 128], identb[:]),
                                         r=[xr, identb], w=[ptr])
                                k.op("dve", lambda E, i=i: E.tensor_copy(xeT[:, :, i * 128:(i + 1) * 128], trv[:, :].rearrange("p (c t) -> p c t", c=DC)),
                                     r=[ptr], w=[xeT])
                            SW = len(tiles) * 128
                            groups = [(g0, min(512, SW - g0)) for g0 in range(0, SW, 512)]
                            for fb in range(FF // 512):
                                cq = DC // NCE
                                for q_ in range(NCE):
                                    ldw(wgs[:, q_ * cq:(q_ + 1) * cq, :], wsrc("wg", l, e, q_, FF)[:, fb * 512:(fb + 1) * 512].rearrange("(c p) f -> p c f", p=128), [GB["wg"]], [wgs])
                                    ldw(wus[:, q_ * cq:(q_ + 1) * cq, :], wsrc("wu", l, e, q_, FF)[:, fb * 512:(fb + 1) * 512].rearrange("(c p) f -> p c f", p=128), [GB["wu"]], [wus])
                                for fc in range(4):
                                    for (g0, gw) in groups:
                                        for c in range(DC):
                                            k.op("pe", lambda E, c=c, fc=fc, g0=g0, gw=gw: E.matmul(pg[:, 0:gw], wgs[:, c, fc * 128:(fc + 1) * 128], xeT[:, c, g0:g0 + gw],
                                                                                                  start=(c == 0), stop=(c == DC - 1)), r=[wgs, xeT], w=[pg])
                                        for c in range(DC):
                                            k.op("pe", lambda E, c=c, fc=fc, g0=g0, gw=gw: E.matmul(pu[:, 0:gw], wus[:, c, fc * 128:(fc + 1) * 128], xeT[:, c, g0:g0 + gw],
                                                                                                  start=(c == 0), stop=(c == DC - 1)), r=[wus, xeT], w=[pu])
                                        k.op("act", lambda E, gw=gw: E.activation(out=sg[:, 0:gw], in_=pg[:, 0:gw], func=AF.Silu), r=[pg], w=[sg])
                                        k.op("dve", lambda E, fb=fb, fc=fc, g0=g0, gw=gw: E.tensor_tensor(out=hid[:, fb * 4 + fc, g0:g0 + gw], in0=sg[:, 0:gw], in1=pu[:, 0:gw], op=ALU.mult),
                                             r=[sg, pu], w=[hid])
                            cqd = FC // NCE
                            for q_ in range(NCE):
                                ldw(wds[:, q_ * cqd:(q_ + 1) * cqd, :], wsrc("wd", l, e, q_, D).rearrange("(c p) d -> p c d", p=128), [GB["wd"]], [wds])
                            for i, stl in enumerate(tiles):
                                for dh, py in enumerate((py0, py1)):
                                    for fc in range(FC):
                                        k.op("pe", lambda E, fc=fc, i=i, dh=dh, py=py: E.matmul(py[:, 0:512], hid[:, fc, i * 128:(i + 1) * 128], wds[:, fc, dh * 512:(dh + 1) * 512],
                                                                                              start=(fc == 0), stop=(fc == FC - 1)), r=[hid, wds], w=[py])
                                k.op("act", lambda E: E.copy(ye[:, 0:512], py0[:, 0:512]), r=[py0], w=[ye])
                                k.op("dve", lambda E: E.tensor_copy(ye[:, 512:1024], py1[:, 0:512]), r=[py1], w=[ye])
                                sto(YE[e][stl * 128:(stl + 1) * 128, :], ye[:], [ye], [YE[e]])
                k.barrier()
                with ExitStack() as s3:
                    gt = [k.sb(f"gt{i}", [128, D], F32, s3) for i in range(2)]
                    acc = k.sb("acc", [128, D], F32, s3)
                    for g_ in gt:
                        k.op("dve", lambda E, g_=g_: E.memset(g_[:], 0.0), w=[g_])
                    for t in range(NTT):
                        rows = slice(t * 128, (t + 1) * 128)
                        for e in range(4):
                            g_ = gt[e % 2]
                            k.pdma("gs", lambda E, e=e, t=t, g_=g_: E.indirect_dma_start(
                                out=g_[:], out_offset=None, in_=YE[e][:, :], in_offset=bass.IndirectOffsetOnAxis(ap=slot[:, t * 4 + e:t * 4 + e + 1], axis=0),
                                bounds_check=k.preg(E, NS - 1), oob_is_err=False), r=[slot, YE[e]], w=[g_], slot=e % 2, selfwait=False)
                            if e == 0:
                                k.op("dve", lambda E, t=t, g_=g_: E.tensor_scalar(out=acc[:], in0=g_[:], scalar1=gmr[:, t, 0:1], scalar2=None, op0=ALU.mult),
                                     r=[g_, gmr], w=[acc])
                            else:
                                k.op("dve", lambda E, t=t, e=e, g_=g_: E.scalar_tensor_tensor(out=acc[:], in0=g_[:], scalar=gmr[:, t, e:e + 1], in1=acc[:],
                                                                                             op0=ALU.mult, op1=ALU.add), r=[g_, gmr, acc], w=[acc])
                        sto(U[rows, :], acc[:], [acc], [U])
            allreduce_rows(U, UR, TA, D)

        def final_phase(P):
            k.barrier()
            with ExitStack() as s2:
                tmp = k.sb("ftmp", [128, D], F32, s2); g2t = k.sb("fg2", [128, D], F32, s2)
                mod_tile(L - 1, g2t, tmp, 5, 0)
                gf = k.sb("fgf", [128, D], F32, s2)
                ld(gf[:], I["fnorm"][:].partition_broadcast(128), [I["fnorm"]], [gf])
                oi = k.sb("oi", [128, NQ // 128], U32, s2)
                ld(oi[:], I["oidx"][:], [I["oidx"]], [oi])
                xt = k.sb("fxt", [128, D], F32, s2); ut = k.sb("fut", [128, D], F32, s2)
                jk = k.sb("fjk", [128, D], F32, s2); ss = k.sb("fss", [128, 1], F32, s2); rs = k.sb("frs", [128, 1], F32, s2)
                yo = k.sb("fyo", [128, D], F32, s2)
                for t in range(NQ // 128):
                    for dst, src in ((xt, X), (ut, UR)):
                        k.pdma("gs", lambda E, dst=dst, src=src, t=t: E.indirect_dma_start(
                            out=dst[:], out_offset=None, in_=src[:, :], in_offset=bass.IndirectOffsetOnAxis(ap=oi[:, t:t + 1], axis=0),
                            bounds_check=k.preg(E, TA - 1), oob_is_err=False), r=[oi, src], w=[dst], slot=(0 if src is X else 1), selfwait=False)
                    k.op("dve", lambda E: E.tensor_tensor(out=ut[:], in0=ut[:], in1=g2t[:], op=ALU.mult), r=[ut, g2t], w=[ut])
                    k.op("dve", lambda E: E.tensor_tensor(out=xt[:], in0=xt[:], in1=ut[:], op=ALU.add), r=[xt, ut], w=[xt])
                    k.op("act", lambda E: E.activation(out=jk[:], in_=xt[:], func=AF.Square, accum_out=ss[:]), r=[xt], w=[jk, ss])
                    rstd_col(ss, rs, 1.0 / D)
                    k.op("dve", lambda E: E.scalar_tensor_tensor(out=yo[:], in0=xt[:], scalar=rs[:], in1=gf[:], op0=ALU.mult, op1=ALU.mult),
                         r=[xt, rs, gf], w=[yo])
                    sto(out[t * 128:(t + 1) * 128, :], yo[:], [yo], [out])

        P = {n_: k.ps("ps_" + n_, [128, 512], F32) for n_ in "abcde"}
        P["t"] = k.ps("ps_t", [128, 1024], BF16)
        for l in range(L):
            mod_phase(l, P["a"])
            p1_phase(l, None if l == 0 else (l - 1, 5), P)
            attn_phase(l, P)
            merge_phase(l, P)
            router_phase(l, P)
            moe_phase(l, P)
        final_phase(P)
        if dbg:
            dl = dict(X=X, MODG=MODG, FM=FM, VT=VT, VN=VN, YT=YT, SSQR=SSQR, UR=UR, U=U, H2=H2, AFF=AFF, AFFT=AFFT,
                      XE0=XE[0], YE0=YE[0])
            for n_, t_ in dl.items():
                ap = t_.t
                shp = list(ap.shape)
                o_ = nc.dram_tensor("dbg_" + n_, shp, ap.dtype, kind="ExternalOutput").ap()
                fl = "a b c d -> (a b c) d" if len(shp) == 4 else ("a b c -> (a b) c" if len(shp) == 3 else None)
                a2 = ap.rearrange(fl) if fl else ap
                o2 = o_.rearrange(fl) if fl else o_
                R_ = a2.shape[0]
                for r0 in range(0, R_, 1024):
                    r1 = min(R_, r0 + 1024)
                    sto(o2[r0:r1, :], a2[r0:r1, :], [t_], [])
        k.drain("sp", k.rings["st"] + k.rings["ld"] + k.rings["gs"] + k.rings["wt"] + k.rings["cc"])
        k.emit()
        build.ninst = k.ninst
    return nc


def _rope_tables(N):
    t = np.arange(N)
    row = (t // GRID_W).astype(np.float32)
    col = (t % GRID_W).astype(np.float32)
    freqs = (10000.0 ** (-np.arange(16, dtype=np.float32) / 16)).astype(np.float32)
    cos = np.ones((64, N + CTX), np.float32)
    sins = np.zeros((64, N + CTX), np.float32)
    for ax, p in enumerate((row, col)):
        ang = p[None, :] * freqs[:, None]
        c, s = np.cos(ang), np.sin(ang)
        cos[ax * 32:ax * 32 + 16, :N] = c
        cos[ax * 32 + 16:ax * 32 + 32, :N] = c
        sins[ax * 32:ax * 32 + 16, :N] = -s
        sins[ax * 32 + 16:ax * 32 + 32, :N] = s
    return np.stack([cos, sins]).astype(np.float32)


_PARTNER = np.concatenate([np.arange(16, 32), np.arange(0, 16), np.arange(48, 64), np.arange(32, 48)])


def _na_bias(rpb_h, N):
    ROWS = N // GRID_W
    out = np.full((3, 8, 2, 64, 8, 64), NEG, np.float32)
    cols = np.arange(GRID_W)
    c_start = np.clip(cols - 8, 0, GRID_W - 16)
    kc = np.arange(GRID_W)[:, None]
    qc = np.arange(GRID_W)[None, :]
    cmask = (kc >= c_start[None, :]) & (kc < c_start[None, :] + 16)
    coff = np.clip(kc - qc + 15, 0, 30)
    win_r = min(8, ROWS)
    for v, R0 in enumerate((0, 8, ROWS - 8)):
        for i in range(8):
            for krl in range(2):
                KR = R0 - 4 + 2 * i + krl
                if KR < 0 or KR >= ROWS:
                    continue
                for qr in range(8):
                    R = R0 + qr
                    rs = int(np.clip(R - win_r // 2, 0, ROWS - win_r))
                    if not (rs <= KR < rs + win_r):
                        continue
                    ro = KR - R + 7
                    blk = rpb_h[ro][coff]
                    out[v, i, krl, :, qr, :] = np.where(cmask, blk, np.float32(NEG))
    return out.reshape(3, 8, 128, 512)


def _win_masks():
    out = np.full((6, 128, 512), NEG, np.float32)
    kk = np.arange(128)[:, None]
    qq = np.arange(512)[None, :]
    for i in range(6):
        kpos = (i - 1) * 128 + kk
        out[i] = np.where(np.abs(kpos - qq) <= 128, np.float32(0.0), np.float32(NEG))
    return out


def prep_inputs(N, L, FF, x, c, ctx, c_ctx, w_mod, b_mod, norm_mix, norm_ffn, w_in, rpb, q_norm, k_norm, sink,
                sgu_norm, w_sgu, b_sgu, out_norm, w_out, w_router, w_gate, w_up, w_down, final_norm):
    f = lambda a: np.ascontiguousarray(np.asarray(a, dtype=np.float32))
    x, c, ctx, c_ctx, w_mod, b_mod, norm_mix, norm_ffn, w_in, rpb, q_norm, k_norm, sink, sgu_norm, w_sgu, b_sgu, out_norm, \
        w_out, w_router, w_gate, w_up, w_down, final_norm = map(f, (
            x, c, ctx, c_ctx, w_mod, b_mod, norm_mix, norm_ffn, w_in, rpb, q_norm, k_norm, sink, sgu_norm, w_sgu, b_sgu,
            out_norm, w_out, w_router, w_gate, w_up, w_down, final_norm))
    TA = N + CTX
    NQ = N // 4
    cs = _rope_tables(N)
    wm = _win_masks()
    identf = np.eye(128, dtype=np.float32)
    identb = np.eye(128, dtype=np.float32).astype(ml_dtypes.bfloat16)
    pi = np.arange(128)
    segm = (pi[:, None] // 32 == pi[None, :] // 32).astype(np.float32)
    trix = (pi[:, None] < pi[None, :]).astype(np.float32)
    e4 = np.zeros((128, 4), np.float32)
    e4[[0, 32, 64, 96], [0, 1, 2, 3]] = 1.0
    selh = ((pi[:, None] >= 64) & (pi[None, :] >= 64)).astype(np.float32)
    o = dict(qa=0, ka=256, va=512, qb=768, kb=1024, vb=1152, qs=1280, ks=1536, vs=1664, su=1792, sv=2048)
    maps = []
    for core in range(N_CORES):
        b, kk = divmod(core, 4)
        kv = kk // 2
        r64 = np.arange(64)
        qa = o["qa"] + kk * 64 + r64; ka = o["ka"] + kk * 64 + r64; va = o["va"] + kk * 64 + r64
        qb = o["qb"] + kk * 64 + r64; kb = o["kb"] + kv * 64 + r64; vb = o["vb"] + kv * 64 + r64
        qs = o["qs"] + kk * 64 + r64; ks = o["ks"] + kv * 64 + r64; vs = o["vs"] + kv * 64 + r64
        su = o["su"] + kk * 64 + r64
        pad = None
        w1 = np.zeros((L, D, 768), np.float32)
        for j, cols in enumerate((qa, qb, ka, kb, qs, su, ks, pad, qs[_PARTNER], qb[_PARTNER], ks[_PARTNER], kb[_PARTNER])):
            if cols is not None:
                w1[:, :, j * 64:(j + 1) * 64] = w_in[:, :, cols]
        grp = np.concatenate([np.arange(kk * 64, kk * 64 + 64), np.delete(np.arange(256), np.arange(kk * 64, kk * 64 + 64))])
        w2 = np.concatenate([w_in[:, :, va], w_in[:, :, vb], w_in[:, :, vs], w_in[:, :, o["sv"] + grp]], axis=2)
        gqk = np.stack([np.tile(q_norm, (1, 2)), np.tile(q_norm[:, _PARTNER], (1, 2)),
                        np.tile(k_norm, (1, 2)), np.tile(k_norm[:, _PARTNER], (1, 2))], axis=2)
        eperm = np.concatenate([np.arange(4 * kk, 4 * kk + 4), np.delete(np.arange(16), np.arange(4 * kk, 4 * kk + 4))])
        onrm = np.stack([np.tile(out_norm[:, m * 256 + kk * 64:m * 256 + kk * 64 + 64], (1, 2)) for m in range(4)], axis=2)
        m = dict(
            x=np.concatenate([x[b], ctx[b]], axis=0),
            cv=np.ascontiguousarray(np.stack([c[b].reshape(DC, 128).T, c_ctx.reshape(DC, 128).T], axis=2)),
            wmod=np.ascontiguousarray(w_mod[:, :, kk * 1536:(kk + 1) * 1536]), bmod=b_mod, nmix=norm_mix, nffn=norm_ffn,
            fnorm=final_norm, w1=w1, w2=np.ascontiguousarray(w2), cs=cs, gqk=np.ascontiguousarray(gqk),
            sgn=np.ascontiguousarray(sgu_norm[:, kk * 64:(kk + 1) * 64]),
            ws=np.ascontiguousarray(np.transpose(w_sgu[:, kk], (0, 2, 1))), bs=np.ascontiguousarray(b_sgu[:, kk]),
            nab=np.stack([_na_bias(rpb[l, kk], N) for l in range(L)]), wm=wm,
            snk=np.ascontiguousarray(np.broadcast_to(sink[:, kk][:, None, None], (L, 128, 1))), onrm=np.ascontiguousarray(onrm),
            wo=np.ascontiguousarray(np.stack([w_out[:, mm * 256 + kk * 64:mm * 256 + kk * 64 + 64, :] for mm in range(4)], axis=1)),
            wr=np.ascontiguousarray(w_router[:, :, eperm]),
            wg=np.ascontiguousarray(w_gate[:, 4 * kk + 2 * b:4 * kk + 2 * b + 2]).reshape(L, 2, -1),
            wu=np.ascontiguousarray(w_up[:, 4 * kk + 2 * b:4 * kk + 2 * b + 2]).reshape(L, 2, -1),
            wd=np.ascontiguousarray(w_down[:, 4 * kk + 2 * b:4 * kk + 2 * b + 2]).reshape(L, 2, -1),
            identf=identf, identb=identb, segm=segm, trix=trix, e4=e4, selh=selh,
            oidx=np.ascontiguousarray((kk * NQ + np.arange(NQ)).reshape(NQ // 128, 128).T.astype(np.uint32)),
        )
        maps.append(m)
    return maps


_NC_CACHE = {}


def run(N, L, FF, inputs, dbg=False):
    key = (N, L, FF, dbg)
    if key not in _NC_CACHE:
        _NC_CACHE[key] = build(N, L, FF, dbg)
    nc = _NC_CACHE[key]
    maps = prep_inputs(N, L, FF, **inputs)
    res = run_bass_kernel_spmd(nc, maps, core_ids=list(range(N_CORES)))
    run.last = res
    NQ = N // 4
    outp = np.zeros((2, N, D), np.float32)
    for core in range(N_CORES):
        b, kk = divmod(core, 4)
        outp[b, kk * NQ:(kk + 1) * NQ] = res.results[core]["out"]
    return outp


def kernel(x, c, ctx, c_ctx, w_mod, b_mod, norm_mix, norm_ffn, w_in, rpb, q_norm, k_norm, sink,
           sgu_norm, w_sgu, b_sgu, out_norm, w_out, w_router, w_gate, w_up, w_down, final_norm):
    inputs = dict(x=x, c=c, ctx=ctx, c_ctx=c_ctx, w_mod=w_mod, b_mod=b_mod, norm_mix=norm_mix, norm_ffn=norm_ffn,
                  w_in=w_in, rpb=rpb, q_norm=q_norm, k_norm=k_norm, sink=sink, sgu_norm=sgu_norm, w_sgu=w_sgu,
                  b_sgu=b_sgu, out_norm=out_norm, w_out=w_out, w_router=w_router, w_gate=w_gate, w_up=w_up,
                  w_down=w_down, final_norm=final_norm)
    N = np.asarray(x).shape[1]
    L = np.asarray(w_mod).shape[0]
    FF = np.asarray(w_gate).shape[3]
    return run(N, L, FF, inputs)
```

```python
from contextlib import ExitStack

import numpy as np
import ml_dtypes
import concourse.bass as bass
import concourse.mybir as mybir
from concourse.bass_utils import run_bass_kernel_spmd

F32 = mybir.dt.float32
BF16 = mybir.dt.bfloat16
U32 = mybir.dt.uint32
AF = mybir.ActivationFunctionType
ALU = mybir.AluOpType
AX = mybir.AxisListType

D = 1024
DC = 8
CTX = 256
GRID_W = 64
EPS = 1e-6
NEG = -30000.0
BIG = 1.0e6
N_CORES = 8


class Buf:
    def __init__(self, name, shared=False):
        self.name = name
        self.shared = shared
        self.w = {}
        self.r = {}


class T:
    def __init__(self, t, name, shared=False):
        self.t = t
        self.b = Buf(name, shared)

    def __getitem__(self, idx):
        return self.t[idx]


class Cnt:
    def __init__(self, sem, step):
        self.sem = sem
        self.step = step
        self.n = 0


def _bufs(xs):
    return [x.b if isinstance(x, T) else x for x in xs]


class K:
    ENG = ("pe", "act", "dve", "pool", "sp")

    def __init__(self, nc, st):
        self.nc = nc
        self.st = st
        self.prog = {e: [] for e in self.ENG}
        self.q = {}
        for e in ("pe", "act", "dve", "pool"):
            self.q[e] = Cnt(st.enter_context(nc.semaphore("c_" + e)), 1)
        self.seen = {}
        self.ninst = 0

    def dmaq(self, name, step=16):
        self.q[name] = Cnt(self.st.enter_context(self.nc.semaphore("d_" + name)), step)
        return name

    def sb(self, name, shape, dt, st=None):
        self.uid = getattr(self, "uid", 0) + 1
        name = f"s{self.uid}_{name}"
        return T((st or self.st).enter_context(self.nc.sbuf_tensor(name, shape, dt)), name)

    def ps(self, name, shape, dt=F32, st=None):
        self.uid = getattr(self, "uid", 0) + 1
        name = f"p{self.uid}_{name}"
        return T((st or self.st).enter_context(self.nc.psum_tensor(name, shape, dt)), name)

    def _deps(self, r, w):
        deps = {}

        def add(m):
            for qn, s in m.items():
                if s > deps.get(qn, 0):
                    deps[qn] = s
        for b in r:
            add(b.w)
        for b in w:
            add(b.r)
            if not b.shared:
                add(b.w)
        return deps

    def _wait(self, eng, deps):
        for qn, v in deps.items():
            if qn == "pe" and eng == "pe":
                continue
            q = self.q[qn]
            if q.step == 16:
                v = q.n
            if self.seen.get((eng, qn), 0) >= v:
                continue
            self.seen[(eng, qn)] = v
            self.prog[eng].append(lambda E, sem=q.sem, val=v * q.step: E.wait_ge(sem, val))

    def _mark(self, me, r, w):
        for b in r:
            if me[1] > b.r.get(me[0], 0):
                b.r[me[0]] = me[1]
        for b in w:
            if b.shared:
                if me[1] > b.w.get(me[0], 0):
                    b.w[me[0]] = me[1]
            else:
                b.w = {me[0]: me[1]}
                b.r = {}

    def op(self, eng, fn, r=(), w=()):
        r, w = _bufs(r), _bufs(w)
        self._wait(eng, self._deps(r, w))
        q = self.q[eng]
        q.n += 1
        self.ninst += 1
        self.prog[eng].append(lambda E, sem=q.sem: fn(E).then_inc(sem, 1))
        self._mark((eng, q.n), r, w)

    def dma(self, eng, qn, out, in_, r=(), w=()):
        self.dmaop(eng, qn, lambda E: E.dma_start(out=out, in_=in_), r, w)

    def dmaop(self, eng, qn, fn, r=(), w=()):
        r, w = _bufs(r), _bufs(w)
        self._wait(eng, self._deps(r, w))
        q = self.q[qn]
        q.n += 1
        self.ninst += 1
        self.prog[eng].append(lambda E, sem=q.sem, step=q.step: fn(E).then_inc(sem, step))
        self._mark((qn, q.n), r, w)

    def ring(self, name, n, step=16):
        self.rings = getattr(self, "rings", {})
        self.rings[name] = [self.dmaq(f"{name}{i}", step) for i in range(n)]
        self.ringpos = getattr(self, "ringpos", {})
        self.ringpos[name] = 0

    def rdma(self, eng, ring, fn, r=(), w=(), slot=None, selfwait=True):
        names = self.rings[ring]
        if slot is None:
            slot = self.ringpos[ring]
            self.ringpos[ring] += 1
        qn = names[slot % len(names)]
        q = self.q[qn]
        r, w = _bufs(r), _bufs(w)
        deps = self._deps(r, w)
        if q.n and selfwait:
            deps[qn] = q.n
        self._wait(eng, deps)
        q.n += 1
        self.ninst += 1
        self.prog[eng].append(lambda E, sem=q.sem, step=q.step: fn(E).then_inc(sem, step))
        self._mark((qn, q.n), r, w)

    def pdma(self, ring, fn, r=(), w=(), slot=None, selfwait=True):
        self.rdma("pool", ring, fn, r, w, slot, selfwait)

    def barrier(self):
        allq = {qn: q.n for qn, q in self.q.items() if q.n}
        for eng in self.ENG:
            self._wait(eng, dict(allq))

    def preg(self, E, val):
        self._regs = getattr(self, "_regs", {})
        if val not in self._regs:
            self._regs[val] = E.to_reg(val)
        return self._regs[val]

    def drain(self, eng, qns):
        for qn in qns:
            q = self.q[qn]
            if q.n:
                self.prog[eng].append(lambda E, sem=q.sem, val=q.n * q.step: E.wait_ge(sem, val))

    def emit(self):
        with self.nc.Block() as block:
            @block.tensor
            def _(E):
                for f in self.prog["pe"]:
                    f(E)

            @block.scalar
            def _(E):
                for f in self.prog["act"]:
                    f(E)

            @block.vector
            def _(E):
                for f in self.prog["dve"]:
                    f(E)

            @block.gpsimd
            def _(E):
                for f in self.prog["pool"]:
                    f(E)

            @block.sync
            def _(E):
                for f in self.prog["sp"]:
                    f(E)


def build(N, L, FF, dbg=False):
    TA = N + CTX
    NTT = TA // 128
    NLT = N // 128
    NQ = N // 4
    ROWS = N // GRID_W
    CAP = 2 * N // 16
    CCAP = 2 * CTX // 16
    NS = CAP + 128
    NST = NS // 128
    FC = FF // 128
    SEGW = N // 32
    nc = bass.Bass("TRN2", target_bir_lowering=False)

    def din(name, shape, dt=F32):
        return T(nc.dram_tensor(name, list(shape), dt, kind="ExternalInput").ap(), name, shared=True)

    def dsc(name, shape, dt=F32):
        return T(nc.dram_tensor(name, list(shape), dt).ap(), name, shared=True)

    I = dict(
        x=din("x", [TA, D]), cv=din("cv", [128, DC, 2]), wmod=din("wmod", [L, D, 1536]),
        bmod=din("bmod", [L, 6 * D]), nmix=din("nmix", [L, D]), nffn=din("nffn", [L, D]),
        fnorm=din("fnorm", [D]), w1=din("w1", [L, D, 768]), w2=din("w2", [L, D, 448]),
        cs=din("cs", [2, 64, TA]), gqk=din("gqk", [L, 128, 4]), sgn=din("sgn", [L, 64]),
        ws=din("ws", [L, 128, 128]), bs=din("bs", [L, 128]), nab=din("nab", [L, 3, 8, 128, 512]),
        wm=din("wm", [6, 128, 512]), snk=din("snk", [L, 128, 1]), onrm=din("onrm", [L, 128, 4]),
        wo=din("wo", [L, 4, 64, D]), wr=din("wr", [L, D, 16]),
        wg=din("wg", [L, 2, D * FF]), wu=din("wu", [L, 2, D * FF]), wd=din("wd", [L, 2, FF * D]),
        identf=din("identf", [128, 128]), identb=din("identb", [128, 128], BF16),
        segm=din("segm", [128, 128]), trix=din("trix", [128, 128]), e4=din("e4", [128, 4]),
        selh=din("selh", [128, 128]), oidx=din("oidx", [128, NQ // 128], U32),
    )
    out = T(nc.dram_tensor("out", [NQ, D], F32, kind="ExternalOutput").ap(), "out", shared=True)
    X = dsc("X", [TA, D])
    MODP = dsc("MODP", [2, 1536]); MODG = dsc("MODG", [L, 4, 2, 1536])
    FM = dsc("FM", [4, 128, TA], BF16); VT = dsc("VT", [TA, 192], BF16); VN = dsc("VN", [TA, 64], BF16)
    YT = dsc("YT", [4, 64, TA], BF16)
    SSQ = dsc("SSQ", [TA, 4]); SSQR = dsc("SSQR", [TA, 4])
    U = dsc("U", [TA, D]); UR = dsc("UR", [TA, D])
    H2 = dsc("H2", [TA, D], BF16); AFF = dsc("AFF", [TA, 16]); AFFT = dsc("AFFT", [16, TA])
    XE = [dsc(f"XE{e}", [NS, D], BF16) for e in range(4)]; YE = [dsc(f"YE{e}", [NS, D]) for e in range(4)]

    CH = 512 * 1024
    NCE = D * FF // CH
    BNC = dsc("BNC", [2, 128, CH // 128])
    GB = {n_: dsc("GB" + n_, [L, 2 * NCE, 2, CH]) for n_ in ("wg", "wu", "wd")}
    with ExitStack() as st:
        k = K(nc, st)
        k.ring("ld", 1); k.ring("st", 1)
        k.ring("wt", 4); k.ring("cc", 2, 1); k.ring("gs", 8)

        def ld(o, i, r, w):
            k.rdma("sp", "ld", lambda E: E.dma_start(out=o, in_=i), r, w)

        def sto(o, i, r, w):
            k.rdma("sp", "st", lambda E: E.dma_start(out=o, in_=i), r, w)

        def ldw(o, i, r, w):
            k.pdma("wt", lambda E: E.dma_start(out=o, in_=i), r, w)

        def coll(kind, op, groups, src, dst, r, w):
            k.pdma("cc", lambda E: E.collective_compute(
                kind, op, replica_groups=groups, ins=[src.opt()], outs=[dst.opt()]), r, w)

        G4 = [[0, 1, 2, 3], [4, 5, 6, 7]]

        def allreduce_rows(src, dst, rows, width):
            step = max(128, (1 << 20) // width)
            for r0 in range(0, rows, step):
                r1 = min(rows, r0 + step)
                coll("AllReduce", ALU.add, G4, src[r0:r1, :], dst[r0:r1, :], [src], [dst])

        identf = k.sb("identf", [128, 128], F32); identb = k.sb("identb", [128, 128], BF16)
        onesf = k.sb("onesf", [128, 128], F32); segm = k.sb("segm", [128, 128], F32)
        trix = k.sb("trix", [128, 128], F32); e4 = k.sb("e4", [128, 4], F32)
        selh = k.sb("selh", [128, 128], F32); epsc = k.sb("epsc", [128, 1], F32)
        for t_, n_ in ((identf, "identf"), (identb, "identb"), (segm, "segm"), (trix, "trix"),
                       (e4, "e4"), (selh, "selh")):
            ld(t_[:], I[n_][:], [I[n_]], [t_])
        k.op("dve", lambda E: E.memset(onesf[:], 1.0), w=[onesf])
        k.op("dve", lambda E: E.memset(epsc[:], EPS), w=[epsc])
        cvt = k.sb("cvt", [128, DC, 2], F32); scv = k.sb("scv", [128, DC, 2], F32)
        ld(cvt[:], I["cv"][:], [I["cv"]], [cvt])
        k.op("act", lambda E: E.activation(out=scv[:], in_=cvt[:], func=AF.Silu), r=[cvt], w=[scv])
        for t in range(NTT):
            sto(X[t * 128:(t + 1) * 128, :], I["x"][t * 128:(t + 1) * 128, :], [I["x"]], [X])

        G2 = [[0, 4], [1, 5], [2, 6], [3, 7]]
        ib = 0
        for l_ in range(L):
            for n_ in ("wg", "wu", "wd"):
                for j_ in range(2):
                    for q_ in range(NCE):
                        sto(BNC[ib % 2], I[n_][l_, j_, q_ * CH:(q_ + 1) * CH].rearrange("(p f) -> p f", p=128), [I[n_]], [BNC])
                        coll("AllGather", ALU.bypass, G2, BNC[ib % 2], GB[n_][l_, j_ * NCE + q_].rearrange("r (p f) -> (r p) f", p=128), [BNC], [GB[n_]])
                        ib += 1

        def wsrc(n_, l_, e_, q_, width):
            r_, j_ = divmod(e_, 2)
            return GB[n_][l_, j_ * NCE + q_, r_, :].rearrange("(row f) -> row f", f=width)

        def rstd_col(ssc, dst, inv_n):
            k.op("act", lambda E: E.activation(out=dst[:], in_=ssc[:], func=AF.Ln, bias=epsc[:], scale=inv_n),
                 r=[ssc, epsc], w=[dst])
            k.op("act", lambda E: E.activation(out=dst[:], in_=dst[:], func=AF.Exp, scale=-0.5), r=[dst], w=[dst])

        def mod_phase(l, ps):
            k.barrier()
            with ExitStack() as s2:
                wb = k.sb("modw", [128, DC, 512], F32, s2); row = k.sb("modrow", [2, 512], F32, s2)
                wv = I["wmod"][l].rearrange("(c p) f -> p c f", p=128)
                for blk in range(3):
                    ld(wb[:], wv[:, :, blk * 512:(blk + 1) * 512], [I["wmod"]], [wb])
                    for c in range(DC):
                        k.op("pe", lambda E, c=c: E.matmul(ps[0:2, 0:512], scv[:, c, :], wb[:, c, :],
                                                         start=(c == 0), stop=(c == DC - 1)), r=[scv, wb], w=[ps])
                    k.op("act", lambda E: E.copy(row[:], ps[0:2, 0:512]), r=[ps], w=[row])
                    sto(MODP[:, blk * 512:(blk + 1) * 512], row[:], [row], [MODP])
                coll("AllGather", ALU.bypass, G4, MODP[:, :], MODG[l].rearrange("r a f -> (r a) f"), [MODP], [MODG])

        def mod_tile(l, dst, tmp, j, which, plus_one=False, mul=None):
            for r_ in range(4):
                lo = max(j * D, r_ * 1536); hi = min((j + 1) * D, (r_ + 1) * 1536)
                if lo < hi:
                    ld(dst[:, lo - j * D:hi - j * D], MODG[l, r_, which, lo - r_ * 1536:hi - r_ * 1536].partition_broadcast(128),
                       [MODG], [dst])
            ld(tmp[:], I["bmod"][l, j * D:(j + 1) * D].partition_broadcast(128), [I["bmod"]], [tmp])
            k.op("dve", lambda E: E.tensor_tensor(out=dst[:], in0=dst[:], in1=tmp[:], op=ALU.add), r=[dst, tmp], w=[dst])
            if plus_one:
                k.op("dve", lambda E: E.tensor_scalar(out=dst[:], in0=dst[:], scalar1=1.0, scalar2=None, op0=ALU.add),
                     r=[dst], w=[dst])
            if mul is not None:
                k.op("dve", lambda E: E.tensor_tensor(out=dst[:], in0=dst[:], in1=mul[:], op=ALU.mult), r=[dst, mul], w=[dst])

        def norm_tiles(l, pend_gate_j, nrm_in, jsh, jsc, body, s2, tag):
            gn = k.sb(tag + "gn", [128, D], F32, s2); tmp = k.sb(tag + "tmp", [128, D], F32, s2)
            ld(gn[:], nrm_in[l].partition_broadcast(128), [nrm_in], [gn])
            G = [k.sb(f"{tag}G{w}", [128, D], F32, s2) for w in range(2)]
            S = [k.sb(f"{tag}S{w}", [128, D], F32, s2) for w in range(2)]
            for w in range(2):
                mod_tile(l, G[w], tmp, jsc, w, plus_one=True, mul=gn)
                mod_tile(l, S[w], tmp, jsh, w)
            PG = None
            if pend_gate_j is not None:
                PG = [k.sb(f"{tag}PG{w}", [128, D], F32, s2) for w in range(2)]
                for w in range(2):
                    mod_tile(pend_gate_j[0], PG[w], tmp, pend_gate_j[1], w)
            bufs = [tuple(k.sb(f"{tag}{n_}{i_}", [128, D if n_ in ("xt", "ut", "jk", "hf") else 1], F32, s2)
                          for n_ in ("xt", "ut", "jk", "hf", "ss", "rs")) for i_ in range(2)]

            def one_tile(t, xt, ut, jk, hf, ss, rs):
                which = 0 if t < NLT else 1
                rows = slice(t * 128, (t + 1) * 128)
                ld(xt[:], X[rows, :], [X], [xt])
                if PG is not None:
                    ld(ut[:], UR[rows, :], [UR], [ut])
                    k.op("dve", lambda E: E.tensor_tensor(out=ut[:], in0=ut[:], in1=PG[which][:], op=ALU.mult),
                         r=[ut, PG[which]], w=[ut])
                    k.op("dve", lambda E: E.tensor_tensor(out=xt[:], in0=xt[:], in1=ut[:], op=ALU.add), r=[xt, ut], w=[xt])
                    sto(X[rows, :], xt[:], [xt], [X])
                k.op("act", lambda E: E.activation(out=jk[:], in_=xt[:], func=AF.Square, accum_out=ss[:]), r=[xt], w=[jk, ss])
                rstd_col(ss, rs, 1.0 / D)
                k.op("dve", lambda E: E.scalar_tensor_tensor(out=hf[:], in0=xt[:], scalar=rs[:], in1=G[which][:],
                                                             op0=ALU.mult, op1=ALU.mult), r=[xt, rs, G[which]], w=[hf])
                k.op("dve", lambda E: E.tensor_tensor(out=hf[:], in0=hf[:], in1=S[which][:], op=ALU.add),
                     r=[hf, S[which]], w=[hf])
                body(t, which, hf)

            for t in range(NTT):
                one_tile(t, *bufs[t % 2])

        def p1_phase(l, pend, P):
            k.barrier()
            with ExitStack() as s2:
                w1 = k.sb("w1", [128, DC, 768], BF16, s2); w2 = k.sb("w2", [128, DC, 448], BF16, s2)
                ldw(w1[:], I["w1"][l].rearrange("(c p) f -> p c f", p=128), [I["w1"]], [w1])
                ldw(w2[:], I["w2"][l].rearrange("(c p) f -> p c f", p=128), [I["w2"]], [w2])
                gqk = k.sb("gqk", [128, 4], F32, s2); sgn = k.sb("sgn", [128, 64], F32, s2)
                ld(gqk[:], I["gqk"][l], [I["gqk"]], [gqk])
                ld(sgn[:], I["sgn"][l].partition_broadcast(128), [I["sgn"]], [sgn])
                hb = k.sb("hb", [128, D], BF16, s2); hT = k.sb("hT", [128, DC, 512], BF16, s2)
                vt = k.sb("vtile", [128, 192], BF16, s2); gsv = k.sb("gsv", [128, 256], F32, s2)
                jk2 = k.sb("jk2", [128, 256], F32, s2); ssv = k.sb("ssv", [128, 1], F32, s2)
                rsv = k.sb("rsv", [128, 1], F32, s2); vn = k.sb("vn", [128, 64], BF16, s2)
                cos = k.sb("cos", [128, 512], F32, s2); sin = k.sb("sin", [128, 512], F32, s2)
                pp = [k.sb(f"pp{i}", [128, 512], F32, s2) for i in range(2)]
                t1 = k.sb("t1", [128, 512], F32, s2); t2 = k.sb("t2", [128, 512], F32, s2)
                sq = k.sb("sqh", [128, 512], F32, s2); rsh = k.sb("rsh", [128, 512], F32, s2)
                fmo = k.sb("fmo", [128, 512], BF16, s2)
                k.op("dve", lambda E: E.memset(sq[:], 0.0), w=[sq])
                pstr, pstok, psfm, pssq = P["a"], P["b"], P["c"], P["d"]
                pstr = P["t"]; trv = pstr.t

                def fm_tile(col0, W):
                    ld(cos[0:64, 0:W], I["cs"][0, :, col0:col0 + W], [I["cs"]], [cos])
                    ld(cos[64:128, 0:W], I["cs"][0, :, col0:col0 + W], [I["cs"]], [cos])
                    ld(sin[0:64, 0:W], I["cs"][1, :, col0:col0 + W], [I["cs"]], [sin])
                    ld(sin[64:128, 0:W], I["cs"][1, :, col0:col0 + W], [I["cs"]], [sin])

                    def proj(oc):
                        for c in range(DC):
                            k.op("pe", lambda E, c=c: E.matmul(psfm[:, 0:W], w1[:, c, oc * 128:(oc + 1) * 128], hT[:, c, 0:W],
                                                             start=(c == 0), stop=(c == DC - 1)), r=[w1, hT], w=[psfm])
                    for i, oc in enumerate((4, 5)):
                        proj(oc)
                        k.op("act", lambda E, i=i: E.copy(pp[i][:, 0:W], psfm[:, 0:W]), r=[psfm], w=[pp[i]])

                    def rope(h, part, gm, gp, norm):
                        sl = slice(h * 64, h * 64 + 64)
                        if norm:
                            k.op("act", lambda E: E.activation(out=sq[sl, 0:W], in_=psfm[sl, 0:W], func=AF.Square), r=[psfm], w=[sq])
                            k.op("pe", lambda E: E.matmul(pssq[:, 0:W], selh[:], sq[:, 0:W], start=True, stop=True), r=[selh, sq], w=[pssq])
                            k.op("act", lambda E: E.activation(out=rsh[sl, 0:W], in_=pssq[sl, 0:W], func=AF.Ln, bias=epsc[sl, :], scale=1.0 / 64),
                                 r=[pssq, epsc], w=[rsh])
                            k.op("act", lambda E: E.activation(out=rsh[sl, 0:W], in_=rsh[sl, 0:W], func=AF.Exp, scale=-0.5), r=[rsh], w=[rsh])
                            k.op("dve", lambda E: E.scalar_tensor_tensor(out=t1[sl, 0:W], in0=psfm[sl, 0:W], scalar=gqk[sl, gm:gm + 1],
                                                                         in1=cos[sl, 0:W], op0=ALU.mult, op1=ALU.mult), r=[psfm, gqk, cos], w=[t1])
                            k.op("dve", lambda E: E.scalar_tensor_tensor(out=t2[sl, 0:W], in0=part[sl, 0:W], scalar=gqk[sl, gp:gp + 1],
                                                                         in1=sin[sl, 0:W], op0=ALU.mult, op1=ALU.mult), r=[part, gqk, sin], w=[t2])
                            k.op("dve", lambda E: E.tensor_tensor(out=t1[sl, 0:W], in0=t1[sl, 0:W], in1=t2[sl, 0:W], op=ALU.add), r=[t1, t2], w=[t1])
                            k.op("dve", lambda E: E.tensor_tensor(out=fmo[sl, 0:W], in0=t1[sl, 0:W], in1=rsh[sl, 0:W], op=ALU.mult), r=[t1, rsh], w=[fmo])
                        else:
                            k.op("dve", lambda E: E.tensor_tensor(out=t1[sl, 0:W], in0=psfm[sl, 0:W], in1=cos[sl, 0:W], op=ALU.mult), r=[psfm, cos], w=[t1])
                            k.op("dve", lambda E: E.tensor_tensor(out=t2[sl, 0:W], in0=part[sl, 0:W], in1=sin[sl, 0:W], op=ALU.mult), r=[part, sin], w=[t2])
                            k.op("dve", lambda E: E.tensor_tensor(out=fmo[sl, 0:W], in0=t1[sl, 0:W], in1=t2[sl, 0:W], op=ALU.add), r=[t1, t2], w=[fmo])

                    for oc in range(4):
                        proj(oc)
                        if oc == 0:
                            k.op("act", lambda E: E.copy(fmo[0:64, 0:W], psfm[0:64, 0:W]), r=[psfm], w=[fmo])
                            rope(1, pp[0], 0, 1, True)
                        elif oc == 1:
                            k.op("act", lambda E: E.copy(fmo[0:64, 0:W], psfm[0:64, 0:W]), r=[psfm], w=[fmo])
                            rope(1, pp[1], 2, 3, True)
                        elif oc == 2:
                            rope(0, pp[0], 0, 0, False)
                            k.op("act", lambda E: E.activation(out=fmo[64:128, 0:W], in_=psfm[64:128, 0:W], func=AF.Gelu), r=[psfm], w=[fmo])
                        else:
                            rope(0, pp[1], 0, 0, False)
                            k.op("act", lambda E: E.copy(fmo[64:128, 0:W], psfm[64:128, 0:W]), r=[psfm], w=[fmo])
                        sto(FM[oc, :, col0:col0 + W], fmo[:, 0:W], [fmo], [FM])

                def body(t, which, hf):
                    sub = t % 4 if t < NLT else (t - NLT)
                    k.op("act", lambda E: E.copy(hb[:], hf[:]), r=[hf], w=[hb])
                    for c in range(DC):
                        k.op("pe", lambda E, c=c: E.transpose(trv[:, c * 128:(c + 1) * 128], hb[:, c * 128:(c + 1) * 128], identb[:]),
                             r=[hb, identb], w=[pstr])
                    k.op("dve", lambda E: E.tensor_copy(hT[:, :, sub * 128:(sub + 1) * 128],
                                                        trv[:, :].rearrange("p (c t) -> p c t", c=DC)), r=[pstr], w=[hT])
                    for c in range(DC):
                        k.op("pe", lambda E, c=c: E.matmul(pstok[:, 0:448], hT[:, c, sub * 128:(sub + 1) * 128], w2[:, c, :],
                                                         start=(c == 0), stop=(c == DC - 1)), r=[hT, w2], w=[pstok])
                    rows = slice(t * 128, (t + 1) * 128)
                    k.op("act", lambda E: E.copy(vt[:], pstok[:, 0:192]), r=[pstok], w=[vt])
                    sto(VT[rows, :], vt[:], [vt], [VT])
                    k.op("act", lambda E: E.activation(out=gsv[:], in_=pstok[:, 192:448], func=AF.Gelu), r=[pstok], w=[gsv])
                    k.op("act", lambda E: E.activation(out=jk2[:], in_=gsv[:], func=AF.Square, accum_out=ssv[:]), r=[gsv], w=[jk2, ssv])
                    rstd_col(ssv, rsv, 1.0 / 256)
                    k.op("dve", lambda E: E.scalar_tensor_tensor(out=vn[:], in0=gsv[:, 0:64], scalar=rsv[:], in1=sgn[:],
                                                                 op0=ALU.mult, op1=ALU.mult), r=[gsv, rsv, sgn], w=[vn])
                    sto(VN[rows, :], vn[:], [vn], [VN])
                    if t < NLT and sub == 3:
                        fm_tile((t - 3) * 128, 512)
                    elif t == NTT - 1:
                        fm_tile(N, CTX)

                norm_tiles(l, pend, I["nmix"], 0, 1, body, s2, "p1")

        def attn_phase(l, P):
            k.barrier()
            with ExitStack() as s2:
                qres = k.sb("qres", [128, TA], BF16, s2); kres = k.sb("kres", [128, TA], BF16, s2)
                vaug = k.sb("vaug", [128, NTT, 128], BF16, s2)
                ssqr = k.sb("ssqres", [128, NTT, 4], F32, s2)
                pt = [k.sb(f"pt{i}", [128, 512], BF16, s2) for i in range(3)]
                bt = [k.sb(f"bt{i}", [128, 512], F32, s2) for i in range(2)]
                sbt = k.sb("sbt", [128, 512], F32, s2)
                rr = k.sb("rr", [128, 512], F32, s2); rs0 = k.sb("rs0", [128, 512], F32, s2)
                y32 = k.sb("y32", [128, 512], F32, s2); ysq = k.sb("ysq", [128, 512], F32, s2)
                yb = k.sb("yb", [128, 512], BF16, s2)
                esk = k.sb("esk", [128, 1], F32, s2); skt = k.sb("skt", [128, 1], F32, s2)
                vna = k.sb("vna", [128, 4, 128], BF16, s2); wsb = k.sb("wsb", [128, 128], BF16, s2)
                bst = k.sb("bst", [128, 128], F32, s2)
                pss = [P["a"], P["b"]]; pso, psr, psq = P["c"], P["d"], P["e"]
                k.op("dve", lambda E: E.memset(vaug[:], 1.0), w=[vaug])
                k.op("dve", lambda E: E.memset(ssqr[:], 0.0), w=[ssqr])
                k.op("dve", lambda E: E.memset(vna[:], 0.0), w=[vna])
                ld(skt[:], I["snk"][l], [I["snk"]], [skt])
                k.op("act", lambda E: E.activation(out=esk[:], in_=skt[:], func=AF.Exp), r=[skt], w=[esk])
                cnt = [0, 0]

                def load_v(col):
                    for t0 in range(0, NTT, 16):
                        t1_ = min(NTT, t0 + 16)
                        ld(vaug[:, t0:t1_, 0:64], VT[t0 * 128:t1_ * 128, col * 64:(col + 1) * 64].rearrange("(t p) c -> p t c", p=128),
                           [VT], [vaug])

                def finish(p0, m, col0, W, sink):
                    if sink:
                        k.op("dve", lambda E: E.tensor_scalar(out=rr[64:128, 0:W], in0=pso[64:128, 0:W], scalar1=esk[64:128, :], scalar2=None,
                                                              op0=ALU.add), r=[pso, esk], w=[rr])
                        k.op("dve", lambda E: E.reciprocal(rr[64:128, 0:W], rr[64:128, 0:W]), r=[rr], w=[rr])
                    else:
                        k.op("dve", lambda E: E.reciprocal(rr[64:128, 0:W], pso[64:128, 0:W]), r=[pso], w=[rr])
                    k.op("pe", lambda E: E.matmul(psr[0:64, 0:W], identf[64:128, 64:128], rr[64:128, 0:W], start=True, stop=True),
                         r=[identf, rr], w=[psr])
                    k.op("act", lambda E: E.copy(rs0[0:64, 0:W], psr[0:64, 0:W]), r=[psr], w=[rs0])
                    k.op("dve", lambda E: E.tensor_tensor(out=y32[0:64, 0:W], in0=pso[0:64, 0:W], in1=rs0[0:64, 0:W], op=ALU.mult),
                         r=[pso, rs0], w=[y32])
                    ytail(slice(0, 64), m, col0, W)

                def ytail(sl, m, col0, W):
                    k.op("act", lambda E: E.copy(yb[sl, 0:W], y32[sl, 0:W]), r=[y32], w=[yb])
                    sto(YT[m, :, col0:col0 + W], yb[sl, 0:W], [yb], [YT])
                    k.op("act", lambda E: E.activation(out=ysq[sl, 0:W], in_=y32[sl, 0:W], func=AF.Square), r=[y32], w=[ysq])
                    for s_ in range(W // 128):
                        tt = col0 // 128 + s_
                        k.op("pe", lambda E, s_=s_: E.matmul(psq[:, 0:1], ysq[sl, s_ * 128:(s_ + 1) * 128], onesf[sl, 0:1], start=True, stop=True),
                             r=[ysq, onesf], w=[psq])
                        k.op("dve", lambda E, tt=tt: E.tensor_copy(ssqr[:, tt, m:m + 1], psq[:, 0:1]), r=[psq], w=[ssqr])

                def attn_tile(p0, m, col0, W, chunks, sink):
                    sl = slice(p0, p0 + 64)
                    n = len(chunks)
                    def qk(j):
                        kt = chunks[j][0]
                        ps = pss[cnt[0] % 2]; cnt[0] += 1
                        k.op("pe", lambda E, ps=ps, kt=kt: E.matmul(ps[:, 0:W], kres[sl, kt * 128:(kt + 1) * 128], qres[sl, col0:col0 + W],
                                                                   start=True, stop=True), r=[kres, qres], w=[ps])
                        return ps

                    ps_next = qk(0)
                    for j, (kt, bias) in enumerate(chunks):
                        ps = ps_next
                        if j + 1 < n:
                            ps_next = qk(j + 1)
                        p = pt[cnt[1] % 3]; cnt[1] += 1
                        if bias is not None:
                            b = bt[j % 2]
                            ld(b[:, 0:W], bias, [I["nab"], I["wm"]], [b])
                            k.op("dve", lambda E, ps=ps, b=b: E.scalar_tensor_tensor(out=sbt[:, 0:W], in0=ps[:, 0:W], scalar=0.125, in1=b[:, 0:W],
                                                                                    op0=ALU.mult, op1=ALU.add), r=[ps, b], w=[sbt])
                            k.op("act", lambda E, p=p: E.activation(out=p[:, 0:W], in_=sbt[:, 0:W], func=AF.Exp), r=[sbt], w=[p])
                        else:
                            k.op("act", lambda E, p=p, ps=ps: E.activation(out=p[:, 0:W], in_=ps[:, 0:W], func=AF.Exp, scale=0.125), r=[ps], w=[p])
                        k.op("pe", lambda E, p=p, kt=kt, j=j: E.matmul(pso[:, 0:W], vaug[:, kt, :], p[:, 0:W], start=(j == 0), stop=(j == n - 1)),
                             r=[vaug, p], w=[pso])
                    finish(p0, m, col0, W, sink)

                ctxc = [(NLT, None), (NLT + 1, None)]
                NQT = N // 512
                ld(qres[:], FM[0], [FM], [qres]); ld(kres[:], FM[1], [FM], [kres])
                load_v(0)
                for t in range(NQT):
                    v = 0 if t == 0 else (2 if t == NQT - 1 else 1)
                    ch = [(4 * t - 2 + i, I["nab"][l, v, i]) for i in range(8) if 0 <= 4 * t - 2 + i < NLT]
                    attn_tile(0, 0, t * 512, 512, ch + ctxc, False)
                attn_tile(0, 0, N, CTX, [(kt, None) for kt, _ in ctxc], False)
                load_v(1)
                allc = [(kt, None) for kt in range(NTT)]
                for t in range(NQT):
                    attn_tile(64, 1, t * 512, 512, allc, False)
                attn_tile(64, 1, N, CTX, ctxc, False)
                ld(qres[:], FM[2], [FM], [qres]); ld(kres[:], FM[3], [FM], [kres])
                load_v(2)
                for t in range(NQT):
                    ch = [(4 * t - 1 + i, I["wm"][i]) for i in range(6) if 0 <= 4 * t - 1 + i < NLT]
                    attn_tile(0, 2, t * 512, 512, ch + ctxc, True)
                attn_tile(0, 2, N, CTX, ctxc, True)
                ld(sbt[:, 0:128], I["ws"][l], [I["ws"]], [sbt])
                k.op("act", lambda E: E.copy(wsb[:], sbt[:, 0:128]), r=[sbt], w=[wsb])
                ld(bst[:], I["bs"][l].partition_broadcast(128), [I["bs"]], [bst])
                for t0 in range(0, NTT, 4):
                    nt_ = min(4, NTT - t0)
                    W = nt_ * 128
                    ld(vna[:, 0:nt_, 64:128], VN[t0 * 128:(t0 + nt_) * 128, :].rearrange("(t p) c -> p t c", p=128), [VN], [vna])
                    for s_ in range(nt_):
                        k.op("pe", lambda E, s_=s_: E.matmul(pso[:, s_ * 128:(s_ + 1) * 128], vna[:, s_, :], wsb[:], start=True, stop=True),
                             r=[vna, wsb], w=[pso])
                    for s_ in range(nt_):
                        k.op("dve", lambda E, s_=s_: E.tensor_tensor(out=y32[64:128, s_ * 128:(s_ + 1) * 128], in0=pso[64:128, s_ * 128:(s_ + 1) * 128],
                                                                    in1=bst[64:128, :], op=ALU.add), r=[pso, bst], w=[y32])
                    k.op("act", lambda E, t0=t0, W=W: E.copy(ysq[64:128, 0:W], qres[64:128, t0 * 128:t0 * 128 + W]), r=[qres], w=[ysq])
                    k.op("dve", lambda E, W=W: E.tensor_tensor(out=y32[64:128, 0:W], in0=y32[64:128, 0:W], in1=ysq[64:128, 0:W], op=ALU.mult),
                         r=[y32, ysq], w=[y32])
                    ytail(slice(64, 128), 3, t0 * 128, W)
                sto(SSQ[:, :].rearrange("(t p) m -> p t m", p=128), ssqr[:], [ssqr], [SSQ])
            coll("AllReduce", ALU.add, G4, SSQ[:, :], SSQR[:, :], [SSQ], [SSQR])

        def merge_phase(l, P):
            k.barrier()
            with ExitStack() as s2:
                wo = k.sb("wo", [128, 4, D], BF16, s2); onr = k.sb("onr", [128, 4], F32, s2)
                for m in range(4):
                    p0 = 64 if m == 3 else 0
                    ldw(wo[p0:p0 + 64, m, :], I["wo"][l, m], [I["wo"]], [wo])
                ld(onr[:], I["onrm"][l], [I["onrm"]], [onr])
                mb = [(k.sb(f"ym{i_}", [128, 4, 128], BF16, s2), k.sb(f"yg{i_}", [128, 4, 128], BF16, s2),
                       k.sb(f"sq4{i_}", [128, 4], F32, s2), k.sb(f"r4{i_}", [128, 4], F32, s2),
                       k.sb(f"mut{i_}", [128, D], F32, s2)) for i_ in range(2)]
                pz = [P["a"], P["b"], P["c"], P["d"]]

                def mtile(t, ym, yg, sq4, r4, ut):
                    cols = slice(t * 128, (t + 1) * 128)
                    for m in range(4):
                        p0 = 64 if m == 3 else 0
                        ld(ym[p0:p0 + 64, m, :], YT[m, :, cols], [YT], [ym])
                    ld(sq4[:], SSQR[cols, :], [SSQR], [sq4])
                    k.op("act", lambda E: E.activation(out=r4[:], in_=sq4[:], func=AF.Ln, bias=epsc[:], scale=1.0 / 256), r=[sq4, epsc], w=[r4])
                    k.op("act", lambda E: E.activation(out=r4[:], in_=r4[:], func=AF.Exp, scale=-0.5), r=[r4], w=[r4])
                    for m in range(4):
                        sl = slice(64, 128) if m == 3 else slice(0, 64)
                        k.op("dve", lambda E, m=m, sl=sl: E.tensor_scalar(out=yg[sl, m, :], in0=ym[sl, m, :], scalar1=onr[sl, m:m + 1], scalar2=None,
                                                                        op0=ALU.mult), r=[ym, onr], w=[yg])
                    for dh in range(2):
                        for m in range(4):
                            sl = slice(64, 128) if m == 3 else slice(0, 64)
                            k.op("pe", lambda E, m=m, sl=sl, dh=dh: E.matmul(pz[m][:, 0:512], yg[sl, m, :], wo[sl, m, dh * 512:(dh + 1) * 512],
                                                                           start=True, stop=True), r=[yg, wo], w=[pz[m]])
                        dsl = slice(dh * 512, (dh + 1) * 512)
                        k.op("dve", lambda E, dsl=dsl: E.tensor_scalar(out=ut[:, dsl], in0=pz[0][:, 0:512], scalar1=r4[:, 0:1], scalar2=None, op0=ALU.mult),
                             r=[pz[0], r4], w=[ut])
                        for m in range(1, 4):
                            k.op("dve", lambda E, m=m, dsl=dsl: E.scalar_tensor_tensor(out=ut[:, dsl], in0=pz[m][:, 0:512], scalar=r4[:, m:m + 1],
                                                                                      in1=ut[:, dsl], op0=ALU.mult, op1=ALU.add), r=[pz[m], r4, ut], w=[ut])
                    sto(U[cols, :], ut[:], [ut], [U])

                for t in range(NTT):
                    mtile(t, *mb[t % 2])
            allreduce_rows(U, UR, TA, D)

        def router_phase(l, P):
            k.barrier()
            with ExitStack() as s2:
                wr = k.sb("wr", [128, DC, 16], F32, s2)
                ld(wr[:], I["wr"][l].rearrange("(c p) e -> p c e", p=128), [I["wr"]], [wr])
                hb = k.sb("h2b", [128, D], BF16, s2); hT = k.sb("h2T", [128, DC, 128], F32, s2)
                ex = k.sb("ex", [128, 16], F32, s2); sm = k.sb("sm", [128, 1], F32, s2)
                af = k.sb("af", [128, 16], F32, s2); aT = k.sb("aT", [16, 128], F32, s2)
                pta, ptb, pl, pa = P["a"], P["b"], P["c"], P["d"]

                def body(t, which, hf):
                    rows = slice(t * 128, (t + 1) * 128)
                    k.op("act", lambda E: E.copy(hb[:], hf[:]), r=[hf], w=[hb])
                    sto(H2[rows, :], hb[:], [hb], [H2])
                    for c in range(DC):
                        pt_ = pta if c < 4 else ptb
                        k.op("pe", lambda E, c=c, pt_=pt_: E.transpose(pt_[:, (c % 4) * 128:(c % 4 + 1) * 128], hf[:, c * 128:(c + 1) * 128], identf[:]),
                             r=[hf, identf], w=[pt_])
                    k.op("dve", lambda E: E.tensor_copy(hT[:, 0:4, :], pta[:, :].rearrange("p (c t) -> p c t", c=4)), r=[pta], w=[hT])
                    k.op("act", lambda E: E.copy(hT[:, 4:8, :], ptb[:, :].rearrange("p (c t) -> p c t", c=4)), r=[ptb], w=[hT])
                    for c in range(DC):
                        k.op("pe", lambda E, c=c: E.matmul(pl[:, 0:16], hT[:, c, :], wr[:, c, :], start=(c == 0), stop=(c == DC - 1)), r=[hT, wr], w=[pl])
                    k.op("act", lambda E: E.activation(out=ex[:], in_=pl[:, 0:16], func=AF.Exp, accum_out=sm[:]), r=[pl], w=[ex, sm])
                    k.op("dve", lambda E: E.reciprocal(sm[:], sm[:]), r=[sm], w=[sm])
                    k.op("dve", lambda E: E.tensor_scalar(out=af[:], in0=ex[:], scalar1=sm[:], scalar2=None, op0=ALU.mult), r=[ex, sm], w=[af])
                    sto(AFF[rows, :], af[:], [af], [AFF])
                    k.op("pe", lambda E: E.transpose(pa[0:16, 0:128], af[:], identf[:]), r=[af, identf], w=[pa])
                    k.op("act", lambda E: E.copy(aT[:], pa[0:16, 0:128]), r=[pa], w=[aT])
                    sto(AFFT[:, rows], aT[:], [aT], [AFFT])

                norm_tiles(l, (l, 2), I["nffn"], 3, 4, body, s2, "p6")

        def moe_phase(l, P):
            k.barrier()
            with ExitStack() as s2:
                slot = k.sb("slot", [128, NTT * 4], U32, s2); gmr = k.sb("gmr", [128, NTT, 4], F32, s2)
                thr = [k.sb(f"thr{w}", [128, 4], F32, s2) for w in range(2)]
                k.barrier()
                with ExitStack() as s3:
                    afl = k.sb("afl", [128, SEGW], F32, s3); afc = k.sb("afc", [128, CTX], F32, s3)
                    jb = k.sb("jb", [128, max(SEGW, CTX)], F32, s3)
                    lo = [k.sb(f"lo{w}", [128, 1], F32, s3) for w in range(2)]
                    mid = k.sb("mid", [128, 1], F32, s3); cn = k.sb("cn", [128, 1], F32, s3)
                    pr = k.sb("pr", [128, 1], F32, s3); tm = k.sb("tm", [128, 4], F32, s3)
                    pc = P["a"]
                    for e_ in range(4):
                        ld(afl[e_ * 32:(e_ + 1) * 32, :], AFFT[e_, 0:N].rearrange("(s n) -> s n", s=32), [AFFT], [afl])
                    k.op("dve", lambda E: E.memset(afc[:], 0.0), w=[afc])
                    ld(afc[0:4, :], AFFT[0:4, N:TA], [AFFT], [afc])
                    for w, (a, width, cap) in enumerate(((afl, SEGW, CAP), (afc, CTX, CCAP))):
                        k.op("dve", lambda E, w=w: E.memset(lo[w][:], 0.0), w=[lo[w]])
                        for it in range(1, 33):
                            wi = 2.0 ** (-it)
                            k.op("dve", lambda E, w=w, wi=wi: E.tensor_scalar(out=mid[:], in0=lo[w][:], scalar1=wi, scalar2=None, op0=ALU.add),
                                 r=[lo[w]], w=[mid])
                            k.op("dve", lambda E, a=a, width=width: E.tensor_scalar(out=jb[:, 0:width], in0=a[:, 0:width], scalar1=mid[:], scalar2=0.0,
                                                                                   op0=ALU.is_ge, op1=ALU.add, accum_out=cn[:]), r=[a, mid], w=[jb, cn])
                            if w == 0:
                                k.op("pe", lambda E: E.matmul(pc[:, 0:1], segm[:], cn[:], start=True, stop=True), r=[segm, cn], w=[pc])
                                src = pc[:, 0:1]; sb_ = pc
                            else:
                                src = cn[:]; sb_ = cn
                            k.op("dve", lambda E, src=src, cap=cap: E.tensor_scalar(out=pr[:], in0=src, scalar1=float(cap), scalar2=None, op0=ALU.is_ge),
                                 r=[sb_], w=[pr])
                            k.op("dve", lambda E, w=w, wi=wi: E.scalar_tensor_tensor(out=lo[w][:], in0=pr[:], scalar=wi, in1=lo[w][:],
                                                                                    op0=ALU.mult, op1=ALU.add), r=[pr, lo[w]], w=[lo[w]])
                    k.op("dve", lambda E: E.tensor_scalar(out=tm[:], in0=e4[:], scalar1=lo[0][:], scalar2=None, op0=ALU.mult), r=[e4, lo[0]], w=[tm])
                    k.op("pe", lambda E: E.matmul(pc[:, 0:4], onesf[:], tm[:], start=True, stop=True), r=[onesf, tm], w=[pc])
                    k.op("act", lambda E: E.copy(thr[0][:], pc[:, 0:4]), r=[pc], w=[thr[0]])
                    k.op("dve", lambda E: E.tensor_scalar(out=tm[0:4, :], in0=identf[0:4, 0:4], scalar1=lo[1][0:4, :], scalar2=None, op0=ALU.mult),
                         r=[identf, lo[1]], w=[tm])
                    k.op("pe", lambda E: E.matmul(pc[:, 0:4], onesf[0:4, :], tm[0:4, :], start=True, stop=True), r=[onesf, tm], w=[pc])
                    k.op("act", lambda E: E.copy(thr[1][:], pc[:, 0:4]), r=[pc], w=[thr[1]])
                k.barrier()
                with ExitStack() as s3:
                    af = k.sb("maf", [128, 16], F32, s3); mk = k.sb("mk", [128, 4], F32, s3)
                    pos = k.sb("pos", [128, 4], F32, s3); car = k.sb("car", [128, 4], F32, s3)
                    sf = k.sb("sf", [128, 4], F32, s3); m2 = k.sb("m2", [128, 4], F32, s3)
                    hr = [k.sb(f"hr{i}", [128, D], BF16, s3) for i in range(2)]
                    pp_, pcs = P["a"], P["b"]
                    k.op("dve", lambda E: E.memset(car[:], 0.0), w=[car])
                    for t in range(NTT):
                        which = 0 if t < NLT else 1
                        rows = slice(t * 128, (t + 1) * 128)
                        h = hr[t % 2]
                        if t == NLT:
                            k.op("dve", lambda E: E.memset(car[:], float(CAP)), w=[car])
                        lim = float(CAP) if which == 0 else float(CAP + CCAP)
                        ld(af[:], AFF[rows, :], [AFF], [af])
                        ld(h[:], H2[rows, :], [H2], [h])
                        k.op("dve", lambda E, w=which: E.tensor_tensor(out=mk[:], in0=af[:, 0:4], in1=thr[w][:], op=ALU.is_ge), r=[af, thr[which]], w=[mk])
                        k.op("pe", lambda E: E.matmul(pp_[:, 0:4], trix[:], mk[:], start=True, stop=True), r=[trix, mk], w=[pp_])
                        k.op("pe", lambda E: E.matmul(pcs[:, 0:4], onesf[:], mk[:], start=True, stop=True), r=[onesf, mk], w=[pcs])
                        k.op("dve", lambda E: E.tensor_tensor(out=pos[:], in0=pp_[:, 0:4], in1=car[:], op=ALU.add), r=[pp_, car], w=[pos])
                        k.op("dve", lambda E: E.tensor_tensor(out=car[:], in0=pcs[:, 0:4], in1=car[:], op=ALU.add), r=[pcs, car], w=[car])
                        k.op("dve", lambda E: E.scalar_tensor_tensor(out=sf[:], in0=pos[:], scalar=-BIG, in1=mk[:], op0=ALU.add, op1=ALU.mult), r=[pos, mk], w=[sf])
                        k.op("dve", lambda E: E.tensor_scalar(out=sf[:], in0=sf[:], scalar1=BIG, scalar2=None, op0=ALU.add), r=[sf], w=[sf])
                        k.op("dve", lambda E, lim=lim: E.tensor_scalar(out=m2[:], in0=sf[:], scalar1=lim, scalar2=None, op0=ALU.is_lt), r=[sf], w=[m2])
                        k.op("dve", lambda E: E.scalar_tensor_tensor(out=sf[:], in0=sf[:], scalar=-BIG, in1=m2[:], op0=ALU.add, op1=ALU.mult), r=[sf, m2], w=[sf])
                        k.op("dve", lambda E: E.tensor_scalar(out=sf[:], in0=sf[:], scalar1=BIG, scalar2=None, op0=ALU.add), r=[sf], w=[sf])
                        k.op("dve", lambda E, t=t: E.tensor_copy(slot[:, t * 4:t * 4 + 4], sf[:]), r=[sf], w=[slot])
                        k.op("dve", lambda E, t=t: E.tensor_tensor(out=gmr[:, t, :], in0=af[:, 0:4], in1=m2[:], op=ALU.mult), r=[af, m2], w=[gmr])
                        for e in range(4):
                            k.pdma("gs", lambda E, e=e, t=t, h=h: E.indirect_dma_start(
                                out=XE[e][:, :], out_offset=bass.IndirectOffsetOnAxis(ap=slot[:, t * 4 + e:t * 4 + e + 1], axis=0), in_=h[:], in_offset=None,
                                bounds_check=k.preg(E, NS - 1), oob_is_err=False), r=[slot, h], w=[XE[e]], slot=(t % 2) * 4 + e, selfwait=False)
                k.barrier()
                with ExitStack() as s3:
                    HT = (NST + 1) // 2
                    xr = k.sb("xr", [128, D], BF16, s3); xeT = k.sb("xeT", [128, DC, HT * 128], BF16, s3)
                    hid = k.sb("hid", [128, FC, HT * 128], BF16, s3)
                    wgs = k.sb("wgs", [128, DC, 512], BF16, s3); wus = k.sb("wus", [128, DC, 512], BF16, s3)
                    wds = k.sb("wds", [128, FC, D], BF16, s3)
                    sg = k.sb("sg", [128, 512], F32, s3); ye = k.sb("ye", [128, D], F32, s3)
                    ptr, pg, pu, py0, py1 = P["a"], P["b"], P["c"], P["d"], P["e"]
                    ptr = P["t"]; trv = ptr.t
                    for e in range(4):
                        for hs in range(2):
                            tiles = list(range(hs * HT, min(NST, (hs + 1) * HT)))
                            if not tiles:
                                continue
                            for i, stl in enumerate(tiles):
                                ld(xr[:], XE[e][stl * 128:(stl + 1) * 128, :], [XE[e]], [xr])
                                for c in range(DC):
                                    k.op("pe", lambda E, c=c: E.transpose(trv[:, c * 128:(c + 1) * 128], xr[:, c * 128:(c + 1) * 128], identb[:]),
                                         r=[xr, identb], w=[ptr])
                                k.op("dve", lambda E, i=i: E.tensor_copy(xeT[:, :, i * 128:(i + 1) * 128], trv[:, :].rearrange("p (c t) -> p c t", c=DC)),
                                     r=[ptr], w=[xeT])
                            SW = len(tiles) * 128
                            groups = [(g0, min(512, SW - g0)) for g0 in range(0, SW, 512)]
                            for fb in range(FF // 512):
                                cq = DC // NCE
                                for q_ in range(NCE):
                                    ldw(wgs[:, q_ * cq:(q_ + 1) * cq, :], wsrc("wg", l, e, q_, FF)[:, fb * 512:(fb + 1) * 512].rearrange("(c p) f -> p c f", p=128), [GB["wg"]], [wgs])
                                    ldw(wus[:, q_ * cq:(q_ + 1) * cq, :], wsrc("wu", l, e, q_, FF)[:, fb * 512:(fb + 1) * 512].rearrange("(c p) f -> p c f", p=128), [GB["wu"]], [wus])
                                for fc in range(4):
                                    for (g0, gw) in groups:
                                        for c in range(DC):
                                            k.op("pe", lambda E, c=c, fc=fc, g0=g0, gw=gw: E.matmul(pg[:, 0:gw], wgs[:, c, fc * 128:(fc + 1) * 128], xeT[:, c, g0:g0 + gw],
                                                                                                  start=(c == 0), stop=(c == DC - 1)), r=[wgs, xeT], w=[pg])
                                        for c in range(DC):
                                            k.op("pe", lambda E, c=c, fc=fc, g0=g0, gw=gw: E.matmul(pu[:, 0:gw], wus[:, c, fc * 128:(fc + 1) * 128], xeT[:, c, g0:g0 + gw],
                                                                                                  start=(c == 0), stop=(c == DC - 1)), r=[wus, xeT], w=[pu])
                                        k.op("act", lambda E, gw=gw: E.activation(out=sg[:, 0:gw], in_=pg[:, 0:gw], func=AF.Silu), r=[pg], w=[sg])
                                        k.op("dve", lambda E, fb=fb, fc=fc, g0=g0, gw=gw: E.tensor_tensor(out=hid[:, fb * 4 + fc, g0:g0 + gw], in0=sg[:, 0:gw], in1=pu[:, 0:gw], op=ALU.mult),
                                             r=[sg, pu], w=[hid])
                            cqd = FC // NCE
                            for q_ in range(NCE):
                                ldw(wds[:, q_ * cqd:(q_ + 1) * cqd, :], wsrc("wd", l, e, q_, D).rearrange("(c p) d -> p c d", p=128), [GB["wd"]], [wds])
                            for i, stl in enumerate(tiles):
                                for dh, py in enumerate((py0, py1)):
                                    for fc in range(FC):
                                        k.op("pe", lambda E, fc=fc, i=i, dh=dh, py=py: E.matmul(py[:, 0:512], hid[:, fc, i * 128:(i + 1) * 128], wds[:, fc, dh * 512:(dh + 1) * 512],
                                                                                              start=(fc == 0), stop=(fc == FC - 1)), r=[hid, wds], w=[py])
                                k.op("act", lambda E: E.copy(ye[:, 0:512], py0[:, 0:512]), r=[py0], w=[ye])
                                k.op("dve", lambda E: E.tensor_copy(ye[:, 512:1024], py1[:, 0:512]), r=[py1], w=[ye])
                                sto(YE[e][stl * 128:(stl + 1) * 128, :], ye[:], [ye], [YE[e]])
                k.barrier()
                with ExitStack() as s3:
                    gt = [k.sb(f"gt{i}", [128, D], F32, s3) for i in range(2)]
                    acc = k.sb("acc", [128, D], F32, s3)
                    for g_ in gt:
                        k.op("dve", lambda E, g_=g_: E.memset(g_[:], 0.0), w=[g_])
                    for t in range(NTT):
                        rows = slice(t * 128, (t + 1) * 128)
                        for e in range(4):
                            g_ = gt[e % 2]
                            k.pdma("gs", lambda E, e=e, t=t, g_=g_: E.indirect_dma_start(
                                out=g_[:], out_offset=None, in_=YE[e][:, :], in_offset=bass.IndirectOffsetOnAxis(ap=slot[:, t * 4 + e:t * 4 + e + 1], axis=0),
                                bounds_check=k.preg(E, NS - 1), oob_is_err=False), r=[slot, YE[e]], w=[g_], slot=e % 2, selfwait=False)
                            if e == 0:
                                k.op("dve", lambda E, t=t, g_=g_: E.tensor_scalar(out=acc[:], in0=g_[:], scalar1=gmr[:, t, 0:1], scalar2=None, op0=ALU.mult),
                                     r=[g_, gmr], w=[acc])
                            else:
                                k.op("dve", lambda E, t=t, e=e, g_=g_: E.scalar_tensor_tensor(out=acc[:], in0=g_[:], scalar=gmr[:, t, e:e + 1], in1=acc[:],
                                                                                             op0=ALU.mult, op1=ALU.add), r=[g_, gmr, acc], w=[acc])
                        sto(U[rows, :], acc[:], [acc], [U])
            allreduce_rows(U, UR, TA, D)

        def final_phase(P):
            k.barrier()
            with ExitStack() as s2:
                tmp = k.sb("ftmp", [128, D], F32, s2); g2t = k.sb("fg2", [128, D], F32, s2)
                mod_tile(L - 1, g2t, tmp, 5, 0)
                gf = k.sb("fgf", [128, D], F32, s2)
                ld(gf[:], I["fnorm"][:].partition_broadcast(128), [I["fnorm"]], [gf])
                oi = k.sb("oi", [128, NQ // 128], U32, s2)
                ld(oi[:], I["oidx"][:], [I["oidx"]], [oi])
                xt = k.sb("fxt", [128, D], F32, s2); ut = k.sb("fut", [128, D], F32, s2)
                jk = k.sb("fjk", [128, D], F32, s2); ss = k.sb("fss", [128, 1], F32, s2); rs = k.sb("frs", [128, 1], F32, s2)
                yo = k.sb("fyo", [128, D], F32, s2)
                for t in range(NQ // 128):
                    for dst, src in ((xt, X), (ut, UR)):
                        k.pdma("gs", lambda E, dst=dst, src=src, t=t: E.indirect_dma_start(
                            out=dst[:], out_offset=None, in_=src[:, :], in_offset=bass.IndirectOffsetOnAxis(ap=oi[:, t:t + 1], axis=0),
                            bounds_check=k.preg(E, TA - 1), oob_is_err=False), r=[oi, src], w=[dst], slot=(0 if src is X else 1), selfwait=False)
                    k.op("dve", lambda E: E.tensor_tensor(out=ut[:], in0=ut[:], in1=g2t[:], op=ALU.mult), r=[ut, g2t], w=[ut])
                    k.op("dve", lambda E: E.tensor_tensor(out=xt[:], in0=xt[:], in1=ut[:], op=ALU.add), r=[xt, ut], w=[xt])
                    k.op("act", lambda E: E.activation(out=jk[:], in_=xt[:], func=AF.Square, accum_out=ss[:]), r=[xt], w=[jk, ss])
                    rstd_col(ss, rs, 1.0 / D)
                    k.op("dve", lambda E: E.scalar_tensor_tensor(out=yo[:], in0=xt[:], scalar=rs[:], in1=gf[:], op0=ALU.mult, op1=ALU.mult),
                         r=[xt, rs, gf], w=[yo])
                    sto(out[t * 128:(t + 1) * 128, :], yo[:], [yo], [out])

        P = {n_: k.ps("ps_" + n_, [128, 512], F32) for n_ in "abcde"}
        P["t"] = k.ps("ps_t", [128, 1024], BF16)
        for l in range(L):
            mod_phase(l, P["a"])
            p1_phase(l, None if l == 0 else (l - 1, 5), P)
            attn_phase(l, P)
            merge_phase(l, P)
            router_phase(l, P)
            moe_phase(l, P)
        final_phase(P)
        if dbg:
            dl = dict(X=X, MODG=MODG, FM=FM, VT=VT, VN=VN, YT=YT, SSQR=SSQR, UR=UR, U=U, H2=H2, AFF=AFF, AFFT=AFFT,
                      XE0=XE[0], YE0=YE[0])
            for n_, t_ in dl.items():
                ap = t_.t
                shp = list(ap.shape)
                o_ = nc.dram_tensor("dbg_" + n_, shp, ap.dtype, kind="ExternalOutput").ap()
                fl = "a b c d -> (a b c) d" if len(shp) == 4 else ("a b c -> (a b) c" if len(shp) == 3 else None)
                a2 = ap.rearrange(fl) if fl else ap
                o2 = o_.rearrange(fl) if fl else o_
                R_ = a2.shape[0]
                for r0 in range(0, R_, 1024):
                    r1 = min(R_, r0 + 1024)
                    sto(o2[r0:r1, :], a2[r0:r1, :], [t_], [])
        k.drain("sp", k.rings["st"] + k.rings["ld"] + k.rings["gs"] + k.rings["wt"] + k.rings["cc"])
        k.emit()
        build.ninst = k.ninst
    return nc


def _rope_tables(N):
    t = np.arange(N)
    row = (t // GRID_W).astype(np.float32)
    col = (t % GRID_W).astype(np.float32)
    freqs = (10000.0 ** (-np.arange(16, dtype=np.float32) / 16)).astype(np.float32)
    cos = np.ones((64, N + CTX), np.float32)
    sins = np.zeros((64, N + CTX), np.float32)
    for ax, p in enumerate((row, col)):
        ang = p[None, :] * freqs[:, None]
        c, s = np.cos(ang), np.sin(ang)
        cos[ax * 32:ax * 32 + 16, :N] = c
        cos[ax * 32 + 16:ax * 32 + 32, :N] = c
        sins[ax * 32:ax * 32 + 16, :N] = -s
        sins[ax * 32 + 16:ax * 32 + 32, :N] = s
    return np.stack([cos, sins]).astype(np.float32)


_PARTNER = np.concatenate([np.arange(16, 32), np.arange(0, 16), np.arange(48, 64), np.arange(32, 48)])


def _na_bias(rpb_h, N):
    ROWS = N // GRID_W
    out = np.full((3, 8, 2, 64, 8, 64), NEG, np.float32)
    cols = np.arange(GRID_W)
    c_start = np.clip(cols - 8, 0, GRID_W - 16)
    kc = np.arange(GRID_W)[:, None]
    qc = np.arange(GRID_W)[None, :]
    cmask = (kc >= c_start[None, :]) & (kc < c_start[None, :] + 16)
    coff = np.clip(kc - qc + 15, 0, 30)
    win_r = min(8, ROWS)
    for v, R0 in enumerate((0, 8, ROWS - 8)):
        for i in range(8):
            for krl in range(2):
                KR = R0 - 4 + 2 * i + krl
                if KR < 0 or KR >= ROWS:
                    continue
                for qr in range(8):
                    R = R0 + qr
                    rs = int(np.clip(R - win_r // 2, 0, ROWS - win_r))
                    if not (rs <= KR < rs + win_r):
                        continue
                    ro = KR - R + 7
                    blk = rpb_h[ro][coff]
                    out[v, i, krl, :, qr, :] = np.where(cmask, blk, np.float32(NEG))
    return out.reshape(3, 8, 128, 512)


def _win_masks():
    out = np.full((6, 128, 512), NEG, np.float32)
    kk = np.arange(128)[:, None]
    qq = np.arange(512)[None, :]
    for i in range(6):
        kpos = (i - 1) * 128 + kk
        out[i] = np.where(np.abs(kpos - qq) <= 128, np.float32(0.0), np.float32(NEG))
    return out


def prep_inputs(N, L, FF, x, c, ctx, c_ctx, w_mod, b_mod, norm_mix, norm_ffn, w_in, rpb, q_norm, k_norm, sink,
                sgu_norm, w_sgu, b_sgu, out_norm, w_out, w_router, w_gate, w_up, w_down, final_norm):
    f = lambda a: np.ascontiguousarray(np.asarray(a, dtype=np.float32))
    x, c, ctx, c_ctx, w_mod, b_mod, norm_mix, norm_ffn, w_in, rpb, q_norm, k_norm, sink, sgu_norm, w_sgu, b_sgu, out_norm, \
        w_out, w_router, w_gate, w_up, w_down, final_norm = map(f, (
            x, c, ctx, c_ctx, w_mod, b_mod, norm_mix, norm_ffn, w_in, rpb, q_norm, k_norm, sink, sgu_norm, w_sgu, b_sgu,
            out_norm, w_out, w_router, w_gate, w_up, w_down, final_norm))
    TA = N + CTX
    NQ = N // 4
    cs = _rope_tables(N)
    wm = _win_masks()
    identf = np.eye(128, dtype=np.float32)
    identb = np.eye(128, dtype=np.float32).astype(ml_dtypes.bfloat16)
    pi = np.arange(128)
    segm = (pi[:, None] // 32 == pi[None, :] // 32).astype(np.float32)
    trix = (pi[:, None] < pi[None, :]).astype(np.float32)
    e4 = np.zeros((128, 4), np.float32)
    e4[[0, 32, 64, 96], [0, 1, 2, 3]] = 1.0
    selh = ((pi[:, None] >= 64) & (pi[None, :] >= 64)).astype(np.float32)
    o = dict(qa=0, ka=256, va=512, qb=768, kb=1024, vb=1152, qs=1280, ks=1536, vs=1664, su=1792, sv=2048)
    maps = []
    for core in range(N_CORES):
        b, kk = divmod(core, 4)
        kv = kk // 2
        r64 = np.arange(64)
        qa = o["qa"] + kk * 64 + r64; ka = o["ka"] + kk * 64 + r64; va = o["va"] + kk * 64 + r64
        qb = o["qb"] + kk * 64 + r64; kb = o["kb"] + kv * 64 + r64; vb = o["vb"] + kv * 64 + r64
        qs = o["qs"] + kk * 64 + r64; ks = o["ks"] + kv * 64 + r64; vs = o["vs"] + kv * 64 + r64
        su = o["su"] + kk * 64 + r64
        pad = None
        w1 = np.zeros((L, D, 768), np.float32)
        for j, cols in enumerate((qa, qb, ka, kb, qs, su, ks, pad, qs[_PARTNER], qb[_PARTNER], ks[_PARTNER], kb[_PARTNER])):
            if cols is not None:
                w1[:, :, j * 64:(j + 1) * 64] = w_in[:, :, cols]
        grp = np.concatenate([np.arange(kk * 64, kk * 64 + 64), np.delete(np.arange(256), np.arange(kk * 64, kk * 64 + 64))])
        w2 = np.concatenate([w_in[:, :, va], w_in[:, :, vb], w_in[:, :, vs], w_in[:, :, o["sv"] + grp]], axis=2)
        gqk = np.stack([np.tile(q_norm, (1, 2)), np.tile(q_norm[:, _PARTNER], (1, 2)),
                        np.tile(k_norm, (1, 2)), np.tile(k_norm[:, _PARTNER], (1, 2))], axis=2)
        eperm = np.concatenate([np.arange(4 * kk, 4 * kk + 4), np.delete(np.arange(16), np.arange(4 * kk, 4 * kk + 4))])
        onrm = np.stack([np.tile(out_norm[:, m * 256 + kk * 64:m * 256 + kk * 64 + 64], (1, 2)) for m in range(4)], axis=2)
        m = dict(
            x=np.concatenate([x[b], ctx[b]], axis=0),
            cv=np.ascontiguousarray(np.stack([c[b].reshape(DC, 128).T, c_ctx.reshape(DC, 128).T], axis=2)),
            wmod=np.ascontiguousarray(w_mod[:, :, kk * 1536:(kk + 1) * 1536]), bmod=b_mod, nmix=norm_mix, nffn=norm_ffn,
            fnorm=final_norm, w1=w1, w2=np.ascontiguousarray(w2), cs=cs, gqk=np.ascontiguousarray(gqk),
            sgn=np.ascontiguousarray(sgu_norm[:, kk * 64:(kk + 1) * 64]),
            ws=np.ascontiguousarray(np.transpose(w_sgu[:, kk], (0, 2, 1))), bs=np.ascontiguousarray(b_sgu[:, kk]),
            nab=np.stack([_na_bias(rpb[l, kk], N) for l in range(L)]), wm=wm,
            snk=np.ascontiguousarray(np.broadcast_to(sink[:, kk][:, None, None], (L, 128, 1))), onrm=np.ascontiguousarray(onrm),
            wo=np.ascontiguousarray(np.stack([w_out[:, mm * 256 + kk * 64:mm * 256 + kk * 64 + 64, :] for mm in range(4)], axis=1)),
            wr=np.ascontiguousarray(w_router[:, :, eperm]),
            wg=np.ascontiguousarray(w_gate[:, 4 * kk + 2 * b:4 * kk + 2 * b + 2]).reshape(L, 2, -1),
            wu=np.ascontiguousarray(w_up[:, 4 * kk + 2 * b:4 * kk + 2 * b + 2]).reshape(L, 2, -1),
            wd=np.ascontiguousarray(w_down[:, 4 * kk + 2 * b:4 * kk + 2 * b + 2]).reshape(L, 2, -1),
            identf=identf, identb=identb, segm=segm, trix=trix, e4=e4, selh=selh,
            oidx=np.ascontiguousarray((kk * NQ + np.arange(NQ)).reshape(NQ // 128, 128).T.astype(np.uint32)),
        )
        maps.append(m)
    return maps


_NC_CACHE = {}


def run(N, L, FF, inputs, dbg=False):
    key = (N, L, FF, dbg)
    if key not in _NC_CACHE:
        _NC_CACHE[key] = build(N, L, FF, dbg)
    nc = _NC_CACHE[key]
    maps = prep_inputs(N, L, FF, **inputs)
    res = run_bass_kernel_spmd(nc, maps, core_ids=list(range(N_CORES)))
    run.last = res
    NQ = N // 4
    outp = np.zeros((2, N, D), np.float32)
    for core in range(N_CORES):
        b, kk = divmod(core, 4)
        outp[b, kk * NQ:(kk + 1) * NQ] = res.results[core]["out"]
    return outp


def kernel(x, c, ctx, c_ctx, w_mod, b_mod, norm_mix, norm_ffn, w_in, rpb, q_norm, k_norm, sink,
           sgu_norm, w_sgu, b_sgu, out_norm, w_out, w_router, w_gate, w_up, w_down, final_norm):
    inputs = dict(x=x, c=c, ctx=ctx, c_ctx=c_ctx, w_mod=w_mod, b_mod=b_mod, norm_mix=norm_mix, norm_ffn=norm_ffn,
                  w_in=w_in, rpb=rpb, q_norm=q_norm, k_norm=k_norm, sink=sink, sgu_norm=sgu_norm, w_sgu=w_sgu,
                  b_sgu=b_sgu, out_norm=out_norm, w_out=w_out, w_router=w_router, w_gate=w_gate, w_up=w_up,
                  w_down=w_down, final_norm=final_norm)
    N = np.asarray(x).shape[1]
    L = np.asarray(w_mod).shape[0]
    FF = np.asarray(w_gate).shape[3]
    return run(N, L, FF, inputs)
```
